# Optimizing a Trainium2 kernel written in Bass

```python
import jax
import jax.numpy as jnp
from jax import lax
import numpy as np

D_MODEL = 1024
BATCH = 8
SEQ = 4096
DEPTH = 2

HEAD_DIM = 64
ROPE_DIM = HEAD_DIM // 4
ROPE_THETA = 500000.0
Q_BLOCK = 128
NORM_EPS = 1e-6
N_EVEN = (DEPTH + 1) // 2
N_ODD = DEPTH // 2

DSA_HEADS = D_MODEL // HEAD_DIM
DSA_NOPE = HEAD_DIM - ROPE_DIM
DSA_V_DIM = HEAD_DIM
DSA_Q_LORA = D_MODEL // 4
DSA_KV_LORA = D_MODEL // 8
IDX_HEADS = 8
IDX_DIM = 64
DSA_TOPK = 256
DSA_IN = DSA_Q_LORA + DSA_KV_LORA + ROPE_DIM + IDX_DIM + IDX_HEADS

NSA_HEADS = D_MODEL // HEAD_DIM
NSA_GROUPS = 4
NSA_HPG = NSA_HEADS // NSA_GROUPS
CMP_LEN = 32
CMP_STRIDE = 16
CMP_HIDDEN = 256
SEL_LEN = 64
SEL_BLOCKS = 16
WINDOW = 512
FORCE_SCORE = 1e4
NSA_KV = NSA_GROUPS * HEAD_DIM
NSA_IN = NSA_HEADS * HEAD_DIM + 6 * NSA_KV + 3 * NSA_HEADS

FFN_DIM = 2816
N_EXPERTS = 8
TOP_K = 2
EXPERT_DIM = 3584
MOE_BLOCK = 512

kernel_name = 'hybrid_dsa_nsa_moe_adaln'


def rmsnorm(x, g):
    xf = x.astype(jnp.float32)
    y = xf * lax.rsqrt(jnp.mean(xf * xf, axis=-1, keepdims=True) + NORM_EPS)
    return (y * g.astype(jnp.float32)).astype(x.dtype)


def rope_tables(seq):
    inv = ROPE_THETA ** (-jnp.arange(0, ROPE_DIM, 2, dtype=jnp.float32) / ROPE_DIM)
    ang = jnp.arange(seq, dtype=jnp.float32)[:, None] * inv[None, :]
    return jnp.cos(ang), jnp.sin(ang)


def apply_partial_rope(x, cos, sin):
    shp = (cos.shape[0],) + (1,) * (x.ndim - 3) + (cos.shape[1],)
    co, si = cos.reshape(shp), sin.reshape(shp)
    half = ROPE_DIM // 2
    xr = x[..., :ROPE_DIM].astype(jnp.float32)
    x1, x2 = xr[..., :half], xr[..., half:]
    rot = jnp.concatenate([x1 * co - x2 * si, x2 * co + x1 * si], axis=-1).astype(x.dtype)
    return jnp.concatenate([rot, x[..., ROPE_DIM:]], axis=-1)


def masked_softmax(s, valid):
    s = jnp.where(valid, s.astype(jnp.float32), -jnp.inf)
    m = jnp.max(s, axis=-1, keepdims=True)
    m = jnp.where(jnp.isfinite(m), m, 0.0)
    e = jnp.where(valid, jnp.exp(s - m), 0.0)
    return e / jnp.maximum(jnp.sum(e, axis=-1, keepdims=True), 1e-30)


def modulate(x, c, norm_g, w_ada, b_ada):
    mod = jax.nn.silu(c) @ w_ada + b_ada
    shift, scale, gate = jnp.split(mod, 3, axis=-1)
    h = rmsnorm(x, norm_g) * (1.0 + scale[:, None, :]) + shift[:, None, :]
    return h, gate[:, None, :]


def sweep_query_blocks(fn, batch, seq):
    nq = seq // Q_BLOCK
    out = lax.map(lambda i: fn(i // nq, (i % nq) * Q_BLOCK), jnp.arange(batch * nq))
    return out.reshape((batch, seq) + out.shape[2:])


def dsa_mixer(h, cos, sin, w_in, g_q, w_uq, g_kv, w_uk, w_uv, w_iq, w_o):
    B, S, _ = h.shape
    proj = h @ w_in
    cuts = np.cumsum([DSA_Q_LORA, DSA_KV_LORA, ROPE_DIM, IDX_DIM]).tolist()
    q_lat, kv_lat, k_rope, idx_k, idx_w = jnp.split(proj, cuts, axis=-1)
    q_lat = rmsnorm(q_lat, g_q)
    q = apply_partial_rope((q_lat @ w_uq).reshape(B, S, DSA_HEADS, HEAD_DIM), cos, sin)
    q_rope, q_nope = q[..., :ROPE_DIM], q[..., ROPE_DIM:]
    q_abs = jnp.einsum('bshn,hcn->bshc', q_nope, w_uk)
    q_cat = jnp.concatenate([q_rope, q_abs], axis=-1)
    c_kv = rmsnorm(kv_lat, g_kv)
    k_cat = jnp.concatenate([apply_partial_rope(k_rope, cos, sin), c_kv], axis=-1)
    iq = apply_partial_rope((q_lat @ w_iq).reshape(B, S, IDX_HEADS, IDX_DIM), cos, sin)
    ik = apply_partial_rope(idx_k, cos, sin)
    iw = idx_w.astype(jnp.float32) * (IDX_HEADS ** -0.5)
    k_sel = min(DSA_TOPK, S // 4)
    scale = HEAD_DIM ** -0.5

    def block(b, qs):
        t = qs + jnp.arange(Q_BLOCK)
        causal = jnp.arange(S)[None, :] <= t[:, None]
        iq_b = lax.dynamic_slice_in_dim(iq[b], qs, Q_BLOCK, 0)
        iw_b = lax.dynamic_slice_in_dim(iw[b], qs, Q_BLOCK, 0)
        logits = jnp.einsum('qhd,sd->qhs', iq_b, ik[b]).astype(jnp.float32) * (IDX_DIM ** -0.5)
        score = jnp.einsum('qhs,qh->qs', jax.nn.relu(logits), iw_b)
        score = jnp.where(causal, score, -jnp.inf)
        _, sel = lax.top_k(score, k_sel)
        kv = k_cat[b][sel]
        q_b = lax.dynamic_slice_in_dim(q_cat[b], qs, Q_BLOCK, 0)
        s = jnp.einsum('qhd,qkd->qhk', q_b, kv) * scale
        p = masked_softmax(s, (sel <= t[:, None])[:, None, :]).astype(kv.dtype)
        return jnp.einsum('qhk,qkc->qhc', p, kv[..., ROPE_DIM:])

    o_lat = sweep_query_blocks(block, B, S)
    o = jnp.einsum('bshc,hcv->bshv', o_lat, w_uv).reshape(B, S, DSA_HEADS * DSA_V_DIM)
    return o @ w_o


def nsa_mixer(h, cos, sin, w_in, cmp_pe, cmp_k1, cmp_k2, cmp_v1, cmp_v2, w_o):
    B, S, _ = h.shape
    G, HD = NSA_GROUPS, HEAD_DIM
    proj = h @ w_in
    cuts = np.cumsum([NSA_HEADS * HD] + [NSA_KV] * 6).tolist()
    q, kc, vc, ks, vs, kw, vw, g = jnp.split(proj, cuts, axis=-1)
    q = apply_partial_rope(q.reshape(B, S, NSA_HEADS, HD), cos, sin)
    kc = apply_partial_rope(kc.reshape(B, S, G, HD), cos, sin)
    ks = apply_partial_rope(ks.reshape(B, S, G, HD), cos, sin)
    kw = apply_partial_rope(kw.reshape(B, S, G, HD), cos, sin)
    vc, vs, vw = (v.reshape(B, S, G, HD) for v in (vc, vs, vw))
    gates = jax.nn.sigmoid(g.astype(jnp.float32)).reshape(B, S, NSA_HEADS, 3)

    nc = (S - CMP_LEN) // CMP_STRIDE + 1
    tok = jnp.arange(nc)[:, None] * CMP_STRIDE + jnp.arange(CMP_LEN)[None, :]

    def compress(arr, w1, w2):
        blk = arr[:, tok] + cmp_pe[:, None, :]
        blk = blk.transpose(0, 1, 3, 2, 4).reshape(B, nc, G, CMP_LEN * HD)
        return jax.nn.silu(blk @ w1) @ w2

    kcmp = compress(kc, cmp_k1, cmp_k2)
    vcmp = compress(vc, cmp_v1, cmp_v2)
    cmp_end = jnp.arange(nc) * CMP_STRIDE + CMP_LEN - 1

    nb = S // SEL_LEN
    n_sel = min(SEL_BLOCKS, nb)
    ks_blk = ks.reshape(B, nb, SEL_LEN, G, HD).transpose(0, 3, 1, 2, 4)
    vs_blk = vs.reshape(B, nb, SEL_LEN, G, HD).transpose(0, 3, 1, 2, 4)
    cstart = jnp.arange(nc) * CMP_STRIDE
    bstart = jnp.arange(nb) * SEL_LEN
    cover = ((cstart[:, None] < bstart[None, :] + SEL_LEN)
             & (cstart[:, None] + CMP_LEN > bstart[None, :])).astype(jnp.float32)

    kw_pad = jnp.pad(kw, ((0, 0), (WINDOW, 0), (0, 0), (0, 0)))
    vw_pad = jnp.pad(vw, ((0, 0), (WINDOW, 0), (0, 0), (0, 0)))
    scale = HD ** -0.5
    gidx = jnp.arange(G)[None, :, None]

    def block(b, qs):
        t = qs + jnp.arange(Q_BLOCK)
        qb = lax.dynamic_slice_in_dim(q[b], qs, Q_BLOCK, 0).reshape(Q_BLOCK, G, NSA_HPG, HD)
        sc = jnp.einsum('qghd,ngd->qghn', qb, kcmp[b]) * scale
        pc = masked_softmax(sc, (cmp_end[None, :] <= t[:, None])[:, None, None, :])
        oc = jnp.einsum('qghn,ngd->qghd', pc.astype(qb.dtype), vcmp[b])
        imp = jnp.einsum('qgn,nj->qgj', jnp.sum(pc, axis=2), cover)
        jb = jnp.arange(nb)[None, :]
        cur = (t // SEL_LEN)[:, None]
        forced = (jb == 0) | (jb == cur) | (jb == cur - 1)
        imp = jnp.where(forced[:, None, :], FORCE_SCORE, imp)
        imp = jnp.where((jb <= cur)[:, None, :], imp, -jnp.inf)
        _, sel = lax.top_k(imp, n_sel)
        kb = ks_blk[b][gidx, sel].reshape(Q_BLOCK, G, n_sel * SEL_LEN, HD)
        vb = vs_blk[b][gidx, sel].reshape(Q_BLOCK, G, n_sel * SEL_LEN, HD)
        pos = (sel[..., None] * SEL_LEN + jnp.arange(SEL_LEN)).reshape(Q_BLOCK, G, n_sel * SEL_LEN)
        ss = jnp.einsum('qghd,qgkd->qghk', qb, kb) * scale
        ps = masked_softmax(ss, (pos <= t[:, None, None])[:, :, None, :])
        osel = jnp.einsum('qghk,qgkd->qghd', ps.astype(qb.dtype), vb)
        kwb = lax.dynamic_slice_in_dim(kw_pad[b], qs, WINDOW + Q_BLOCK, 0)
        vwb = lax.dynamic_slice_in_dim(vw_pad[b], qs, WINDOW + Q_BLOCK, 0)
        kpos = qs - WINDOW + jnp.arange(WINDOW + Q_BLOCK)
        wmask = ((kpos[None, :] <= t[:, None]) & (kpos[None, :] > t[:, None] - WINDOW)
                 & (kpos[None, :] >= 0))
        sw = jnp.einsum('qghd,kgd->qghk', qb, kwb) * scale
        pw = masked_softmax(sw, wmask[:, None, None, :])
        ow = jnp.einsum('qghk,kgd->qghd', pw.astype(qb.dtype), vwb)
        gb = lax.dynamic_slice_in_dim(gates[b], qs, Q_BLOCK, 0).reshape(Q_BLOCK, G, NSA_HPG, 3)
        o = gb[..., 0:1] * oc + gb[..., 1:2] * osel + gb[..., 2:3] * ow
        return o.astype(qb.dtype).reshape(Q_BLOCK, NSA_HEADS * HD)

    o = sweep_query_blocks(block, B, S)
    return o @ w_o


def swiglu(h, w1, w3, w2):
    return (jax.nn.silu(h @ w1) * (h @ w3)) @ w2


def moe_swiglu(h, w_router, w1, w3, w2):
    B, S, D = h.shape
    T = B * S
    hf = h.reshape(T, D)
    logits = (hf @ w_router).astype(jnp.float32)
    top_val, top_idx = lax.top_k(logits, TOP_K)
    gate = jax.nn.softmax(top_val, axis=-1)
    flat_e = top_idx.reshape(-1)
    flat_tok = jnp.repeat(jnp.arange(T), TOP_K)
    order = jnp.argsort(flat_e)
    e_sorted = flat_e[order]
    tok_sorted = flat_tok[order]
    gate_sorted = gate.reshape(-1)[order]
    counts = jnp.bincount(flat_e, length=N_EXPERTS)
    padded = (counts + MOE_BLOCK - 1) // MOE_BLOCK * MOE_BLOCK
    ends = jnp.cumsum(padded)
    pstart = ends - padded
    gstart = jnp.cumsum(counts) - counts
    dest = pstart[e_sorted] + (jnp.arange(T * TOP_K) - gstart[e_sorted])
    n_blocks = -(-(T * TOP_K) // MOE_BLOCK) + N_EXPERTS
    slot_tok = jnp.full((n_blocks * MOE_BLOCK,), T, jnp.int32).at[dest].set(tok_sorted)
    block_e = jnp.minimum(jnp.searchsorted(ends, jnp.arange(n_blocks) * MOE_BLOCK, side='right'),
                          N_EXPERTS - 1)
    h_pad = jnp.concatenate([hf, jnp.zeros((1, D), hf.dtype)], axis=0)

    def run(i):
        rows = lax.dynamic_slice_in_dim(slot_tok, i * MOE_BLOCK, MOE_BLOCK)
        e = block_e[i]
        return swiglu(h_pad[rows], w1[e], w3[e], w2[e])

    ys = lax.map(run, jnp.arange(n_blocks)).reshape(-1, D)
    y = ys[dest] * gate_sorted[:, None].astype(ys.dtype)
    return jax.ops.segment_sum(y, tok_sorted, num_segments=T).reshape(B, S, D)


def setup_inputs(seed: int = 0) -> dict:
    key = jax.random.key(seed)
    ks = iter(jax.random.split(key, 40))
    D = D_MODEL

    def w(shape, fan_in):
        return jax.random.normal(next(ks), shape, jnp.float32) * (fan_in ** -0.5)

    def gain(shape):
        return 1.0 + 0.05 * jax.random.normal(next(ks), shape, jnp.float32)

    def small(shape, s):
        return s * jax.random.normal(next(ks), shape, jnp.float32)

    return {
        'x': jax.random.normal(next(ks), (BATCH, SEQ, D), jnp.float32),
        'c': jax.random.normal(next(ks), (BATCH, D), jnp.float32),
        'norm_mix': gain((DEPTH, D)),
        'norm_ffn': gain((DEPTH, D)),
        'ada_w': 0.5 * w((DEPTH, 2, D, 3 * D), D),
        'ada_b': small((DEPTH, 2, 3 * D), 0.02),
        'final_norm': gain((D,)),
        'dsa_w_in': w((N_EVEN, D, DSA_IN), D),
        'dsa_g_q': gain((N_EVEN, DSA_Q_LORA)),
        'dsa_w_uq': w((N_EVEN, DSA_Q_LORA, DSA_HEADS * HEAD_DIM), DSA_Q_LORA),
        'dsa_g_kv': gain((N_EVEN, DSA_KV_LORA)),
        'dsa_w_uk': w((N_EVEN, DSA_HEADS, DSA_KV_LORA, DSA_NOPE), DSA_KV_LORA),
        'dsa_w_uv': w((N_EVEN, DSA_HEADS, DSA_KV_LORA, DSA_V_DIM), DSA_KV_LORA),
        'dsa_w_iq': w((N_EVEN, DSA_Q_LORA, IDX_HEADS * IDX_DIM), DSA_Q_LORA),
        'dsa_w_o': w((N_EVEN, DSA_HEADS * DSA_V_DIM, D), DSA_HEADS * DSA_V_DIM),
        'ffn_w1': w((N_EVEN, D, FFN_DIM), D),
        'ffn_w3': w((N_EVEN, D, FFN_DIM), D),
        'ffn_w2': w((N_EVEN, FFN_DIM, D), FFN_DIM),
        'nsa_w_in': w((N_ODD, D, NSA_IN), D),
        'nsa_cmp_pe': small((N_ODD, CMP_LEN, HEAD_DIM), 0.1),
        'nsa_cmp_k1': w((N_ODD, CMP_LEN * HEAD_DIM, CMP_HIDDEN), CMP_LEN * HEAD_DIM),
        'nsa_cmp_k2': w((N_ODD, CMP_HIDDEN, HEAD_DIM), CMP_HIDDEN),
        'nsa_cmp_v1': w((N_ODD, CMP_LEN * HEAD_DIM, CMP_HIDDEN), CMP_LEN * HEAD_DIM),
        'nsa_cmp_v2': w((N_ODD, CMP_HIDDEN, HEAD_DIM), CMP_HIDDEN),
        'nsa_w_o': w((N_ODD, NSA_HEADS * HEAD_DIM, D), NSA_HEADS * HEAD_DIM),
        'moe_router': w((N_ODD, D, N_EXPERTS), D),
        'moe_w1': w((N_ODD, N_EXPERTS, D, EXPERT_DIM), D),
        'moe_w3': w((N_ODD, N_EXPERTS, D, EXPERT_DIM), D),
        'moe_w2': w((N_ODD, N_EXPERTS, EXPERT_DIM, D), EXPERT_DIM),
    }


def reference(x, c, norm_mix, norm_ffn, ada_w, ada_b, final_norm,
              dsa_w_in, dsa_g_q, dsa_w_uq, dsa_g_kv, dsa_w_uk, dsa_w_uv, dsa_w_iq, dsa_w_o,
              ffn_w1, ffn_w3, ffn_w2,
              nsa_w_in, nsa_cmp_pe, nsa_cmp_k1, nsa_cmp_k2, nsa_cmp_v1, nsa_cmp_v2, nsa_w_o,
              moe_router, moe_w1, moe_w3, moe_w2):
    cos, sin = rope_tables(x.shape[1])
    for i in range(DEPTH):
        j = i // 2
        h, gate = modulate(x, c, norm_mix[i], ada_w[i, 0], ada_b[i, 0])
        if i % 2 == 0:
            mix = dsa_mixer(h, cos, sin, dsa_w_in[j], dsa_g_q[j], dsa_w_uq[j], dsa_g_kv[j],
                            dsa_w_uk[j], dsa_w_uv[j], dsa_w_iq[j], dsa_w_o[j])
        else:
            mix = nsa_mixer(h, cos, sin, nsa_w_in[j], nsa_cmp_pe[j], nsa_cmp_k1[j], nsa_cmp_k2[j],
                            nsa_cmp_v1[j], nsa_cmp_v2[j], nsa_w_o[j])
        x = x + gate * mix
        h, gate = modulate(x, c, norm_ffn[i], ada_w[i, 1], ada_b[i, 1])
        if i % 2 == 0:
            f = swiglu(h, ffn_w1[j], ffn_w3[j], ffn_w2[j])
        else:
            f = moe_swiglu(h, moe_router[j], moe_w1[j], moe_w3[j], moe_w2[j])
        x = x + gate * f
    return rmsnorm(x, final_norm)
```

```python
import numpy as np
import ml_dtypes
from contextlib import ExitStack
import concourse.bass as bass
import concourse.mybir as mybir
from concourse.bass_utils import run_bass_kernel_spmd

F32 = mybir.dt.float32
BF16 = mybir.dt.bfloat16
I32 = mybir.dt.int32
AF = mybir.ActivationFunctionType
ALU = mybir.AluOpType
AX = mybir.AxisListType

T = 4096
D = 1024
NT = 32
EPS = 1e-6
NEG = -30000.0


class Res:
    __slots__ = ("w", "r", "name")

    def __init__(self, name=""):
        self.w = {}
        self.r = []
        self.name = name


class Prog:
    CENG = ["pe", "act", "dve", "pool"]
    NPOOL = 16
    EPOCH = 30000

    def __init__(self, nc):
        self.nc = nc
        self.es = ExitStack()
        self.q = {e: [] for e in ["pe", "act", "dve", "pool", "sp"]}
        self.sems = {}
        self.cnt = {}
        self.seen = {e: {} for e in self.q}
        self.nsem = 0
        self.epoch = {e: 0 for e in self.CENG}
        self.ecount = {e: 0 for e in self.CENG}
        self.cur = {}
        for e in self.CENG:
            self._new_epoch(e)
        self.dpool = {}
        self.dpos = {}
        self.ninstr = 0
        self.pending = {e: [] for e in self.q}
        self.lasttok = {}

    def barrier(self):
        toks = list(self.lasttok.values())
        for k, v in self.cnt.items():
            if k[0] == "dma" and v > 0:
                toks.append((k, v))
        for e in self.q:
            self.pending[e].extend(toks)

    def _mksem(self, key):
        s = self.es.enter_context(self.nc.semaphore("s%d" % self.nsem))
        self.nsem += 1
        self.sems[key] = s
        self.cnt[key] = 0
        return s

    def _new_epoch(self, e):
        key = (e, self.epoch[e])
        self.epoch[e] += 1
        self._mksem(key)
        self.cur[e] = key
        self.ecount[e] = 0

    def sbuf(self, name, shape, dt, stack=None):
        self.nsem += 1
        return (stack or self.es).enter_context(self.nc.sbuf_tensor("%s_u%d" % (name, self.nsem), shape, dt))

    def psum(self, name, shape, dt):
        return self.es.enter_context(self.nc.psum_tensor(name, shape, dt))

    def _waits_for(self, eng, reads, writes):
        toks = self.pending[eng]
        self.pending[eng] = []
        for r in reads:
            toks.extend(r.w.items())
        for w in writes:
            toks.extend(w.w.items())
            toks.extend(w.r)
        need = {}
        seen = self.seen[eng]
        for (k, v) in toks:
            if eng == "pe" and k[0] == "pe":
                continue
            if seen.get(k, 0) >= v:
                continue
            if need.get(k, 0) < v:
                need[k] = v
        for k, v in need.items():
            seen[k] = v
        return list(need.items())

    def _commit(self, tok, reads, writes):
        for r in reads:
            r.r.append(tok)
            if len(r.r) > 16:
                d = {}
                for (k, v) in r.r:
                    if d.get(k, 0) < v:
                        d[k] = v
                r.r = list(d.items())
        for w in writes:
            if w.w.get(tok[0], 0) < tok[1]:
                w.w[tok[0]] = tok[1]
            w.r = []

    def op(self, eng, fn, reads=(), writes=()):
        if self.ecount[eng] >= self.EPOCH:
            self._new_epoch(eng)
        waits = self._waits_for(eng, reads, writes)
        key = self.cur[eng]
        self.cnt[key] += 1
        self.ecount[eng] += 1
        tok = (key, self.cnt[key])
        self.lasttok[eng] = tok
        self.q[eng].append((waits, fn, key, 1))
        self._commit(tok, reads, writes)
        self.ninstr += 1
        return tok

    def op_raw(self, eng, fn, inc, reads=(), writes=()):
        if eng not in self.dpool:
            self.dpool[eng] = []
            for i in range(self.NPOOL):
                self._mksem(("dma", eng, i))
                self.dpool[eng].append(("dma", eng, i))
            self.dpos[eng] = 0
        key = self.dpool[eng][self.dpos[eng] % self.NPOOL]
        self.dpos[eng] += 1
        waits = self._waits_for(eng, reads, writes)
        prev = self.cnt[key]
        if prev > 0 and self.seen[eng].get(key, 0) < prev:
            waits.append((key, prev))
            self.seen[eng][key] = prev
        self.cnt[key] += inc
        tok = (key, self.cnt[key])
        self.q[eng].append((waits, fn, key, inc))
        self._commit(tok, reads, writes)
        self.ninstr += 1
        return tok

    def dma(self, eng, out, in_, reads=(), writes=(), **kw):
        if eng not in self.dpool:
            self.dpool[eng] = []
            for i in range(self.NPOOL):
                self._mksem(("dma", eng, i))
                self.dpool[eng].append(("dma", eng, i))
            self.dpos[eng] = 0
        key = self.dpool[eng][self.dpos[eng] % self.NPOOL]
        self.dpos[eng] += 1
        waits = self._waits_for(eng, reads, writes)
        prev = self.cnt[key]
        if prev > 0 and self.seen[eng].get(key, 0) < prev:
            waits.append((key, prev))
            self.seen[eng][key] = prev
        self.cnt[key] += 16
        tok = (key, self.cnt[key])

        def fn(e, out=out, in_=in_, kw=kw):
            return e.dma_start(out=out, in_=in_, **kw)
        self.q[eng].append((waits, fn, key, 16))
        self._commit(tok, reads, writes)
        self.ninstr += 1
        return tok

    def finish(self, final_tokens):
        nc = self.nc
        sems = self.sems
        q = self.q
        fw = {}
        for (k, v) in final_tokens:
            if fw.get(k, 0) < v:
                fw[k] = v

        def emit(e, lst):
            for (waits, fn, key, inc) in lst:
                for (k, v) in waits:
                    e.wait_ge(sems[k], v)
                fn(e).then_inc(sems[key], inc)

        with nc.Block() as block:
            @block.tensor
            def _(e):
                emit(e, q["pe"])

            @block.scalar
            def _(e):
                emit(e, q["act"])

            @block.vector
            def _(e):
                emit(e, q["dve"])

            @block.gpsimd
            def _(e):
                emit(e, q["pool"])

            @block.sync
            def _(e):
                emit(e, q["sp"])
                for k, v in fw.items():
                    e.wait_ge(sems[k], v)
        self.es.close()


def fbc(ap, pos, n):
    l = [list(x) for x in ap.ap]
    l.insert(1 + pos, [0, n])
    return bass.AP(ap.tensor, ap.offset, l)


def _nsa_consts():
    n = np.arange(128)
    esel = np.zeros((128, 32, 128), np.float32)
    for kt in range(32):
        for s_ in range(2):
            esel[64 * s_ + 2 * kt, kt, 0:64] = 30000.0
            esel[64 * s_ + 2 * kt + 1, kt, 64:128] = 30000.0
    ntri = np.zeros((128, 2, 128), np.float32)
    ntri[:, 0, :] = np.where(n[:, None] > n[None, :], NEG, 0.0)
    ntri[:, 1, :] = np.where(n[:, None] <= n[None, :], NEG, 0.0)
    idx = {}
    tiles = []
    for qt in range(32):
        nvalid = min(255, 8 * qt + 7)
        for nt in range(2):
            if nt * 128 >= nvalid:
                continue
            nn = nt * 128 + n
            tq = qt * 128 + n
            mk = np.where(16 * nn[:, None] + 31 > tq[None, :], NEG, 0.0).astype(np.float32)
            if np.any(mk != 0):
                idx[(qt, nt)] = len(tiles)
                tiles.append(mk)
    ncmp = np.ascontiguousarray(np.stack(tiles, 1))
    fa = np.zeros((128, 32, 64), np.float32)
    j = np.arange(64)
    for qt in range(32):
        cur = 2 * qt + (n >= 64).astype(np.int64)
        forced = (j[None, :] == 0) | (j[None, :] == cur[:, None]) | (j[None, :] == cur[:, None] - 1)
        valid = j[None, :] <= cur[:, None]
        fa[:, qt, :] = np.where(valid, np.where(forced, 1e4, 0.0), -1e30)
    u = np.arange(512)
    cmast = (16 * (u[None, :] - 248) + 31 <= n[:, None]).astype(np.float32)
    return esel, ntri, ncmp, idx, fa, cmast


_NC = _nsa_consts()
NCMP_IDX = _NC[3]
NCMP_N = _NC[2].shape[1]


class Ctx:
    pass


DBG = {"stop": None}


def mod_stage(C, l, s, normg_d):
    P, nc, Dm = C.P, C.nc, C.D
    with ExitStack() as st:
        bb = P.sbuf("mod_bb", [128, 3072], F32, st)
        gb = P.sbuf("mod_gb", [128, 1024], F32, st)
        modt = P.sbuf("mod_t", [128, 2048], F32, st)
        wch = [P.sbuf("mod_w%d" % i, [128, 8, 512], F32, st) for i in range(2)]
        r_bb, r_gb, r_mod = Res(), Res(), Res()
        r_w = [Res(), Res()]
        P.dma("sp", bb[:], bass.AP(Dm["ada_b"].tensor, Dm["ada_b"][l, s, :].offset, [[0, 128], [1, 3072]]), writes=[r_bb])
        P.dma("sp", gb[:], bass.AP(normg_d.tensor, normg_d.offset, [[0, 128], [1, 1024]]), writes=[r_gb])
        wv = Dm["ada_w"][l, s].rearrange("(kc p) n -> p kc n", p=128)
        for n in range(6):
            w = wch[n % 2]
            P.dma("sp", w[:], wv[:, :, n * 512:(n + 1) * 512], writes=[r_w[n % 2]])
            bank = C.bank(6 + (n % 2))
            for kc in range(8):
                P.op("pe", lambda e, w=w, kc=kc, bank=bank: e.matmul(bank.ap[:, :], C.csb[:, kc, :], w[:, kc, :],
                                                                     start=(kc == 0), stop=(kc == 7)),
                     reads=[C.r_csb, r_w[n % 2]], writes=[bank.res])
            if n < 2:
                dst, dres = C.shift[:, n * 512:(n + 1) * 512], C.r_shift
            elif n < 4:
                dst, dres = modt[:, (n - 2) * 512:(n - 1) * 512], r_mod
            else:
                dst, dres = C.gate[:, (n - 4) * 512:(n - 3) * 512], C.r_gate
            P.op("dve", lambda e, dst=dst, bank=bank, n=n: e.tensor_tensor(dst, bank.ap[:, :], bb[:, n * 512:(n + 1) * 512], ALU.add),
                 reads=[bank.res, r_bb], writes=[dres])
        P.op("dve", lambda e: e.scalar_tensor_tensor(C.gs[:], modt[:, 0:1024], 1.0, gb[:], ALU.add, ALU.mult),
             reads=[r_mod, r_gb], writes=[C.r_gs])
        C.fence([r_bb, r_gb, r_mod] + r_w)


class Bank:
    def __init__(self, ap):
        self.ap = ap
        self.res = Res()


class Prenorm:
    def __init__(self, C, x_d, hT_d, st, router=None):
        P = C.P
        self.C, self.x_d, self.router = C, x_d, router
        self.hTv = hT_d.rearrange("kc p t -> p kc t")
        self.xt = P.sbuf("pn_x", [128, 1024], F32, st)
        self.sq = P.sbuf("pn_sq", [128, 1024], BF16, st)
        self.t1 = [P.sbuf("pn_t%d" % i, [128, 1024], F32, st) for i in range(4 if router else 2)]
        self.hb = [P.sbuf("pn_hb%d" % i, [128, 1024], BF16, st) for i in range(4)]
        self.hgp = P.sbuf("pn_hg", [128, 8, 512], BF16, st)
        self.ss = P.sbuf("pn_ss", [128, 64], F32, st)
        self.r_x, self.r_sq, self.r_hgp = Res(), Res(), Res()
        self.r_t1 = [Res() for _ in self.t1]
        self.r_hb = [Res() for _ in self.hb]
        self.r_ss = [Res() for _ in range(32)]
        if router:
            self.identf = P.sbuf("pn_idf", [128, 128], F32, st)
            self.r_idf = Res()
            P.dma("sp", self.identf[:], C.D["ident_in"], writes=[self.r_idf])
            self.hTf = P.sbuf("pn_hTf", [128, 8, 128], F32, st)
            self.r_hTf = Res()
            self.lg = P.sbuf("pn_lg", [128, 32, 8], F32, st)
            self.r_lg = [Res() for _ in range(8)]
            self.m1 = P.sbuf("pn_m1", [128, 4, 8], F32, st)
            self.l2 = P.sbuf("pn_l2", [128, 4, 8], F32, st)
            self.ex = P.sbuf("pn_ex", [128, 4, 8], F32, st)
            self.mx = P.sbuf("pn_mx", [128, 4, 4], F32, st)
            self.r_a = Res()

    def front(self, tg):
        C, P = self.C, self.C.P
        xt, sq, ss = self.xt, self.sq, self.ss
        for j in range(4):
            i = tg * 4 + j
            t1, r_t1 = (self.t1[j], self.r_t1[j]) if self.router else (self.t1[j % 2], self.r_t1[j % 2])
            hb, r_hb = self.hb[j], self.r_hb[j]
            P.dma("sp", xt[:], self.x_d[i * 128:(i + 1) * 128, :], reads=[C.r_xin[i]] if C.r_xin else [], writes=[self.r_x])
            P.op("act", lambda e, i=i: e.activation(sq[:], xt[:], AF.Square, accum_out=ss[:, 2 * i:2 * i + 1]),
                 reads=[self.r_x], writes=[self.r_sq, self.r_ss[i]])
            P.op("act", lambda e, i=i: e.activation(ss[:, 2 * i + 1:2 * i + 2], ss[:, 2 * i:2 * i + 1], AF.Sqrt, bias=C.epsc[:, 0:1], scale=1.0 / D),
                 reads=[self.r_ss[i], C.r_eps], writes=[self.r_ss[i]])
            P.op("dve", lambda e, i=i: e.reciprocal(ss[:, 2 * i:2 * i + 1], ss[:, 2 * i + 1:2 * i + 2]),
                 reads=[self.r_ss[i]], writes=[self.r_ss[i]])
            P.op("dve", lambda e, i=i, t1=t1: e.scalar_tensor_tensor(t1[:], xt[:], ss[:, 2 * i:2 * i + 1], C.gs[:], ALU.mult, ALU.mult),
                 reads=[self.r_x, self.r_ss[i], C.r_gs], writes=[r_t1])
            if not self.router:
                P.op("pool", lambda e, t1=t1, hb=hb: e.tensor_tensor(hb[:], t1[:], C.shift[:], ALU.add),
                     reads=[r_t1, C.r_shift], writes=[r_hb])
            else:
                P.op("pool", lambda e, t1=t1: e.tensor_tensor(t1[:], t1[:], C.shift[:], ALU.add),
                     reads=[r_t1, C.r_shift], writes=[r_t1])
                P.op("act", lambda e, t1=t1, hb=hb: e.copy(hb[:], t1[:]), reads=[r_t1], writes=[r_hb])

    def back(self, tg):
        C, P = self.C, self.C.P
        hgp = self.hgp
        for j in range(4):
            i = tg * 4 + j
            hb, r_hb = self.hb[j], self.r_hb[j]
            bank = C.bank(6)
            pv = bank.ap.bitcast(BF16)
            for kc in range(8):
                P.op("pe", lambda e, hb=hb, kc=kc, pv=pv: e.transpose(pv[:, kc * 128:(kc + 1) * 128], hb[:, kc * 128:(kc + 1) * 128], C.identb[:]),
                     reads=[r_hb, C.r_ident], writes=[bank.res])
            P.op("act", lambda e, j=j, pv=pv: e.copy(hgp[:, :, j * 128:(j + 1) * 128], pv.rearrange("p (kc t) -> p kc t", kc=8)),
                 reads=[bank.res], writes=[self.r_hgp])
            if self.router:
                t1, r_t1 = self.t1[j], self.r_t1[j]
                wrT, r_wr = self.router["wrT"]
                b7 = C.bank(7)
                for half in range(2):
                    for k in range(4):
                        kc = half * 4 + k
                        P.op("pe", lambda e, t1=t1, kc=kc, k=k, b7=b7: e.transpose(b7.ap[:, k * 128:(k + 1) * 128], t1[:, kc * 128:(kc + 1) * 128], self.identf[:]),
                             reads=[r_t1, self.r_idf], writes=[b7.res])
                    P.op("dve", lambda e, half=half, b7=b7: e.tensor_copy(self.hTf[:, half * 4:(half + 1) * 4, :], b7.ap[:, :].rearrange("p (k t) -> p k t", k=4)),
                         reads=[b7.res], writes=[self.r_hTf])
                for kc in range(8):
                    P.op("pe", lambda e, kc=kc, bank=bank: e.matmul(bank.ap[:, 0:8], self.hTf[:, kc, :], wrT[:, kc, :], start=(kc == 0), stop=(kc == 7)),
                         reads=[self.r_hTf, r_wr], writes=[bank.res])
                P.op("dve", lambda e, i=i, bank=bank: e.tensor_copy(self.lg[:, i, :], bank.ap[:, 0:8]), reads=[bank.res], writes=[self.r_lg[tg]])
        P.dma("sp", self.hTv[:, :, tg * 512:(tg + 1) * 512], hgp[:], reads=[self.r_hgp], writes=[C.r_hT[tg]])
        if self.router:
            tokgate, r_tgl = self.router["tokgate"]
            lg = self.lg[:, tg * 4:(tg + 1) * 4, :]
            m1, l2, ex, mx, r_a = self.m1, self.l2, self.ex, self.mx, self.r_a
            P.op("dve", lambda e, lg=lg: e.tensor_reduce(mx[:, :, 0], lg, AX.X, ALU.max), reads=[self.r_lg[tg]], writes=[r_a])
            P.op("dve", lambda e, lg=lg: e.tensor_tensor(m1[:], lg, fbc(mx[:, :, 0], 1, 8), ALU.is_equal), reads=[r_a, self.r_lg[tg]], writes=[r_a])
            P.op("dve", lambda e, lg=lg: e.scalar_tensor_tensor(l2[:], m1[:], -1e30, lg, ALU.mult, ALU.add), reads=[r_a], writes=[r_a])
            P.op("dve", lambda e: e.tensor_reduce(mx[:, :, 1], l2[:], AX.X, ALU.max), reads=[r_a], writes=[r_a])
            P.op("dve", lambda e, lg=lg: e.tensor_tensor(m1[:], lg, fbc(mx[:, :, 1], 1, 8), ALU.is_ge), reads=[r_a], writes=[r_a])
            P.op("dve", lambda e, lg=lg: e.tensor_tensor(l2[:], lg, fbc(mx[:, :, 0], 1, 8), ALU.subtract), reads=[r_a], writes=[r_a])
            P.op("act", lambda e: e.activation(ex[:], l2[:], AF.Exp), reads=[r_a], writes=[r_a])
            P.op("dve", lambda e: e.tensor_tensor(ex[:], ex[:], m1[:], ALU.mult), reads=[r_a], writes=[r_a])
            P.op("dve", lambda e: e.tensor_reduce(mx[:, :, 2], ex[:], AX.X, ALU.add), reads=[r_a], writes=[r_a])
            P.op("dve", lambda e: e.reciprocal(mx[:, :, 3], mx[:, :, 2]), reads=[r_a], writes=[r_a])
            P.op("dve", lambda e, tg=tg: e.tensor_tensor(tokgate[:, tg * 4:(tg + 1) * 4, :], ex[:], fbc(mx[:, :, 3], 1, 8), ALU.mult), reads=[r_a], writes=[r_tgl[tg]])


def prenorm_stage(C, x_d, hT_d, router=None):
    P = C.P
    with ExitStack() as st:
        xt = [P.sbuf("pn_x%d" % i, [128, 1024], F32, st) for i in range(2)]
        sq = P.sbuf("pn_sq", [128, 1024], F32, st)
        t1 = [P.sbuf("pn_t%d" % i, [128, 1024], F32, st) for i in range(2)]
        hb = [P.sbuf("pn_hb%d" % i, [128, 1024], BF16, st) for i in range(2)]
        hg = [P.sbuf("pn_hg%d" % i, [128, 8, 512], BF16, st) for i in range(2)]
        ss = P.sbuf("pn_ss", [128, 64], F32, st)
        r_x = [Res(), Res()]
        r_sq = Res()
        r_t1 = [Res(), Res()]
        r_hb = [Res(), Res()]
        r_hg = [Res(), Res()]
        r_ss = [Res() for _ in range(32)]
        if router is not None:
            wr, r_wr, tokgate, r_tg = router
            lg = P.sbuf("pn_lg", [128, 32, 8], F32, st)
            junk = P.sbuf("pn_junk", [128, 1024], F32, st)
            r_lg = [Res() for _ in range(32)]
            r_junk = Res()
        hTv = hT_d.rearrange("kc p t -> p kc t")
        for i in range(NT):
            b = i % 2
            g = (i // 4) % 2
            P.dma("sp", xt[b][:], x_d[i * 128:(i + 1) * 128, :], writes=[r_x[b]])
            P.op("act", lambda e, b=b, i=i: e.activation(sq[:], xt[b][:], AF.Square, accum_out=ss[:, 2 * i:2 * i + 1]),
                 reads=[r_x[b]], writes=[r_sq, r_ss[i]])
            P.op("act", lambda e, i=i: e.activation(ss[:, 2 * i + 1:2 * i + 2], ss[:, 2 * i:2 * i + 1], AF.Sqrt, bias=C.epsc[:, 0:1], scale=1.0 / D),
                 reads=[r_ss[i], C.r_eps], writes=[r_ss[i]])
            P.op("dve", lambda e, i=i: e.reciprocal(ss[:, 2 * i:2 * i + 1], ss[:, 2 * i + 1:2 * i + 2]),
                 reads=[r_ss[i]], writes=[r_ss[i]])
            P.op("dve", lambda e, b=b, i=i: e.scalar_tensor_tensor(t1[b][:], xt[b][:], ss[:, 2 * i:2 * i + 1], C.gs[:], ALU.mult, ALU.mult),
                 reads=[r_x[b], r_ss[i], C.r_gs], writes=[r_t1[b]])
            if router is None:
                P.op("pool", lambda e, b=b: e.tensor_tensor(hb[b][:], t1[b][:], C.shift[:], ALU.add),
                     reads=[r_t1[b], C.r_shift], writes=[r_hb[b]])
            else:
                P.op("pool", lambda e, b=b: e.tensor_tensor(t1[b][:], t1[b][:], C.shift[:], ALU.add),
                     reads=[r_t1[b], C.r_shift], writes=[r_t1[b]])
                P.op("act", lambda e, b=b: e.copy(hb[b][:], t1[b][:]), reads=[r_t1[b]], writes=[r_hb[b]])
                for ex in range(8):
                    P.op("dve", lambda e, b=b, ex=ex, i=i: e.scalar_tensor_tensor(
                        junk[:], t1[b][:], 1.0, wr[:, ex, :], ALU.mult, ALU.mult, accum_out=lg[:, i, ex:ex + 1]),
                        reads=[r_t1[b], r_wr], writes=[r_junk, r_lg[i]])
            bank = C.bank(6 + (i % 2))
            pv = bank.ap.bitcast(BF16)
            for kc in range(8):
                P.op("pe", lambda e, b=b, kc=kc, pv=pv: e.transpose(pv[:, kc * 128:(kc + 1) * 128], hb[b][:, kc * 128:(kc + 1) * 128], C.identb[:]),
                     reads=[r_hb[b], C.r_ident], writes=[bank.res])
            j = i % 4
            P.op("act", lambda e, g=g, j=j, pv=pv: e.copy(hg[g][:, :, j * 128:(j + 1) * 128], pv.rearrange("p (kc t) -> p kc t", kc=8)),
                 reads=[bank.res], writes=[r_hg[g]])
            if j == 3:
                tg = i // 4
                P.dma("sp", hTv[:, :, tg * 512:(tg + 1) * 512], hg[g][:], reads=[r_hg[g]], writes=[C.r_hT[tg]])
        if router is not None:
            m1 = P.sbuf("pn_m1", [128, 32, 8], F32, st)
            l2 = P.sbuf("pn_l2", [128, 32, 8], F32, st)
            ex = P.sbuf("pn_ex", [128, 32, 8], F32, st)
            mx = P.sbuf("pn_mx", [128, 32, 4], F32, st)
            r_a = Res()
            allr = r_lg
            P.op("dve", lambda e: e.tensor_reduce(mx[:, :, 0], lg[:], AX.X, ALU.max), reads=allr, writes=[r_a])
            P.op("dve", lambda e: e.tensor_tensor(m1[:], lg[:], fbc(mx[:, :, 0], 1, 8), ALU.is_equal), reads=[r_a] + allr, writes=[r_a])
            P.op("dve", lambda e: e.scalar_tensor_tensor(l2[:], m1[:], -1e30, lg[:], ALU.mult, ALU.add), reads=[r_a], writes=[r_a])
            P.op("dve", lambda e: e.tensor_reduce(mx[:, :, 1], l2[:], AX.X, ALU.max), reads=[r_a], writes=[r_a])
            P.op("dve", lambda e: e.tensor_tensor(m1[:], lg[:], fbc(mx[:, :, 1], 1, 8), ALU.is_ge), reads=[r_a], writes=[r_a])
            P.op("dve", lambda e: e.tensor_tensor(l2[:], lg[:], fbc(mx[:, :, 0], 1, 8), ALU.subtract), reads=[r_a], writes=[r_a])
            P.op("act", lambda e: e.activation(ex[:], l2[:], AF.Exp), reads=[r_a], writes=[r_a])
            P.op("dve", lambda e: e.tensor_tensor(ex[:], ex[:], m1[:], ALU.mult), reads=[r_a], writes=[r_a])
            P.op("dve", lambda e: e.tensor_reduce(mx[:, :, 2], ex[:], AX.X, ALU.add), reads=[r_a], writes=[r_a])
            P.op("dve", lambda e: e.reciprocal(mx[:, :, 3], mx[:, :, 2]), reads=[r_a], writes=[r_a])
            P.op("dve", lambda e: e.tensor_tensor(tokgate[:], ex[:], fbc(mx[:, :, 3], 1, 8), ALU.mult), reads=[r_a], writes=[r_tg])
            C.fence([r_a, r_junk] + r_lg)
        C.fence(r_x + [r_sq] + r_t1 + r_hb + r_hg + r_ss)


def ffn_passes(C, hT_d, passes, yacc_d, tokgate=None, pn_args=None, FM=1024):
    P = C.P
    with ExitStack() as st:
        pn = Prenorm(C, pn_args[0], hT_d, st, router=pn_args[1]) if pn_args is not None else None
        W1 = [P.sbuf("ff_w1_%d" % i, [128, 8, FM], BF16, st) for i in range(2)]
        W3 = [P.sbuf("ff_w3_%d" % i, [128, 8, FM], BF16, st) for i in range(2)]
        W2 = [P.sbuf("ff_w2_%d" % i, [128, FM // 128, 1024], BF16, st) for i in range(2)]
        hg = [P.sbuf("ff_hg%d" % i, [128, 8, 512], BF16, st) for i in range(2)]
        gT = [P.sbuf("ff_gT%d" % i, [128, FM // 128, 512], BF16, st) for i in range(2)]
        sS = [P.sbuf("ff_s%d" % i, [128, 512], F32, st) for i in range(2)]
        yS = [P.sbuf("ff_y%d" % i, [128, 1024], F32, st) for i in range(2)]
        r_W = [[Res(), Res(), Res()] for _ in range(2)]
        r_hg = [Res(), Res()]
        r_gT = [Res(), Res()]
        r_s = [Res(), Res()]
        r_y = [Res(), Res()]
        hTv = hT_d.rearrange("kc p t -> p kc t")
        def w_tasks(pi):
            w1a, w3a, w2a, ex = passes[pi]
            F = w1a.shape[1]
            s = pi % 2
            tasks = []
            for kc in range(8):
                tasks.append((W1[s][:, kc, 0:F], w1a[kc * 128:(kc + 1) * 128, :], r_W[s][0]))
            for kc in range(8):
                tasks.append((W3[s][:, kc, 0:F], w3a[kc * 128:(kc + 1) * 128, :], r_W[s][1]))
            for fc in range(F // 128):
                tasks.append((W2[s][:, fc, :], w2a[fc * 128:(fc + 1) * 128, :], r_W[s][2]))
            return tasks

        for t_ in w_tasks(0):
            C.wload(*t_)
        state = {}
        cn = {"cnt": 0, "ycnt": 0, "gcnt": 0}

        def p1(pi, tg):
            w1a, w3a, w2a, ex = passes[pi]
            nf = w1a.shape[1] // 128
            s = pi % 2
            hb = cn["gcnt"] % 2
            cn["gcnt"] += 1
            state[(pi, tg)] = hb
            P.dma("sp", hg[hb][:], hTv[:, :, tg * 512:(tg + 1) * 512], reads=[C.r_hT[tg]], writes=[r_hg[hb]])
            if tg >= 1 and pi + 1 < len(passes):
                for t_ in w_tasks(pi + 1)[(tg - 1) * 4:tg * 4]:
                    C.wload(*t_)
            for fc in range(nf):
                k = cn["cnt"] % 2
                cn["cnt"] += 1
                ba, bb_ = C.bank(k), C.bank(2 + k)
                for kc in range(8):
                    P.op("pe", lambda e, ba=ba, s=s, kc=kc, fc=fc, hb=hb: e.matmul(
                        ba.ap[:, :], W1[s][:, kc, fc * 128:(fc + 1) * 128], hg[hb][:, kc, :], start=(kc == 0), stop=(kc == 7)),
                        reads=[r_W[s][0], r_hg[hb]], writes=[ba.res])
                for kc in range(8):
                    P.op("pe", lambda e, bb_=bb_, s=s, kc=kc, fc=fc, hb=hb: e.matmul(
                        bb_.ap[:, :], W3[s][:, kc, fc * 128:(fc + 1) * 128], hg[hb][:, kc, :], start=(kc == 0), stop=(kc == 7)),
                        reads=[r_W[s][1], r_hg[hb]], writes=[bb_.res])
                P.op("act", lambda e, k=k, ba=ba: e.activation(sS[k][:], ba.ap[:, :], AF.Silu), reads=[ba.res], writes=[r_s[k]])
                P.op("dve", lambda e, k=k, bb_=bb_, hb=hb, fc=fc: e.tensor_tensor(gT[hb][:, fc, :], sS[k][:], bb_.ap[:, :], ALU.mult),
                     reads=[r_s[k], bb_.res], writes=[r_gT[hb]])

        def p2(pi, tg):
            w1a, w3a, w2a, ex = passes[pi]
            nf = w1a.shape[1] // 128
            s = pi % 2
            hb = state[(pi, tg)]
            for j in range(4):
                tile = tg * 4 + j
                yb = cn["ycnt"] % 2
                cn["ycnt"] += 1
                for half in range(2):
                    bo = C.bank(4 + half)
                    for fc in range(nf):
                        P.op("pe", lambda e, bo=bo, hb=hb, fc=fc, j=j, s=s, half=half, nf=nf: e.matmul(
                            bo.ap[:, :], gT[hb][:, fc, j * 128:(j + 1) * 128], W2[s][:, fc, half * 512:(half + 1) * 512],
                            start=(fc == 0), stop=(fc == nf - 1)),
                            reads=[r_gT[hb], r_W[s][2]], writes=[bo.res])
                    if ex is None:
                        P.op("act", lambda e, yb=yb, bo=bo, half=half: e.copy(yS[yb][:, half * 512:(half + 1) * 512], bo.ap[:, :]),
                             reads=[bo.res], writes=[r_y[yb]])
                    else:
                        P.op("act", lambda e, yb=yb, bo=bo, half=half, tile=tile, ex=ex: e.activation(
                            yS[yb][:, half * 512:(half + 1) * 512], bo.ap[:, :], AF.Copy, scale=tokgate[0][:, tile, ex:ex + 1]),
                            reads=[bo.res, tokgate[1][tile // 4]], writes=[r_y[yb]])
                if pi == 0:
                    P.dma("sp", yacc_d[tile * 128:(tile + 1) * 128, :], yS[yb][:], reads=[r_y[yb]], writes=[C.r_yacc[tile]])
                else:
                    P.dma("pool", yacc_d[tile * 128:(tile + 1) * 128, :], yS[yb][:], reads=[r_y[yb]], writes=[C.r_yacc[tile]],
                          accum_op=ALU.add)

        seq = [(pi, tg) for pi in range(len(passes)) for tg in range(8)]
        if pn is not None:
            pn.front(0)
            pn.back(0)
            pn.front(1)
        p1(*seq[0])
        if pn is not None:
            pn.back(1)
        for k, cur in enumerate(seq):
            nx = seq[k + 1] if k + 1 < len(seq) else None
            pnx = pn is not None and nx is not None and nx[0] == 0 and nx[1] + 1 < 8
            if pnx:
                pn.front(nx[1] + 1)
            if nx is not None:
                p1(*nx)
            p2(*cur)
            if pnx:
                pn.back(nx[1] + 1)
        fl = r_hg + r_gT + r_s + r_y
        for a in r_W:
            fl += a
        C.fence(fl)


def combine_stage(C, xin_d, yacc_d, xout_d, final_g=None):
    P = C.P
    with ExitStack() as st:
        xt = [P.sbuf("cb_x%d" % i, [128, 1024], F32, st) for i in range(2)]
        yt = [P.sbuf("cb_y%d" % i, [128, 1024], F32, st) for i in range(2)]
        r_x = [Res(), Res()]
        r_y = [Res(), Res()]
        if final_g is not None:
            fg = P.sbuf("cb_fg", [128, 1024], F32, st)
            sq = P.sbuf("cb_sq", [128, 1024], F32, st)
            ss = P.sbuf("cb_ss", [128, 64], F32, st)
            r_fg, r_sq = Res(), Res()
            r_ss = [Res() for _ in range(32)]
            P.dma("sp", fg[:], bass.AP(final_g.tensor, final_g.offset, [[0, 128], [1, 1024]]), writes=[r_fg])
        for i in range(NT):
            b = i % 2
            P.dma("sp", xt[b][:], xin_d[i * 128:(i + 1) * 128, :], reads=[C.r_xin[i]] if C.r_xin else [], writes=[r_x[b]])
            P.dma("sp", yt[b][:], yacc_d[i * 128:(i + 1) * 128, :], reads=[C.r_yacc[i]], writes=[r_y[b]])
            P.op("pool", lambda e, b=b: e.tensor_tensor(yt[b][:], yt[b][:], C.gate[:], ALU.mult), reads=[r_y[b], C.r_gate], writes=[r_y[b]])
            P.op("dve", lambda e, b=b: e.tensor_tensor(xt[b][:], xt[b][:], yt[b][:], ALU.add), reads=[r_y[b], r_x[b]], writes=[r_x[b]])
            if final_g is not None:
                P.op("act", lambda e, b=b, i=i: e.activation(sq[:], xt[b][:], AF.Square, accum_out=ss[:, 2 * i:2 * i + 1]),
                     reads=[r_x[b]], writes=[r_sq, r_ss[i]])
                P.op("act", lambda e, i=i: e.activation(ss[:, 2 * i + 1:2 * i + 2], ss[:, 2 * i:2 * i + 1], AF.Sqrt, bias=C.epsc[:, 0:1], scale=1.0 / D),
                     reads=[r_ss[i], C.r_eps], writes=[r_ss[i]])
                P.op("dve", lambda e, i=i: e.reciprocal(ss[:, 2 * i:2 * i + 1], ss[:, 2 * i + 1:2 * i + 2]),
                     reads=[r_ss[i]], writes=[r_ss[i]])
                P.op("dve", lambda e, b=b, i=i: e.scalar_tensor_tensor(xt[b][:], xt[b][:], ss[:, 2 * i:2 * i + 1], fg[:], ALU.mult, ALU.mult),
                     reads=[r_x[b], r_ss[i], r_fg], writes=[r_x[b]])
            tok = P.dma("sp", xout_d[i * 128:(i + 1) * 128, :], xt[b][:], reads=[r_x[b]], writes=[C.r_xout[i]])
            C.final.append(tok)
        fl = r_x + r_y
        if final_g is not None:
            fl += [r_fg, r_sq] + r_ss
        C.fence(fl)


def dsa_stage(C, x_d, xout_d):
    P, Dm = C.P, C.D
    KSC = 0.125 / (8.0 ** 0.5)
    with ExitStack() as st:
        CKV_tm = P.sbuf("d_ckvtm", [128, 32, 128], BF16, st)
        CKV_T = P.sbuf("d_ckvT", [128, T], BF16, st)
        IK_T2 = P.sbuf("d_ikT2", [128, T], BF16, st)
        KR_T = P.sbuf("d_krT", [16, T], BF16, st)
        QLN_T = P.sbuf("d_qlnT", [128, 2, T], BF16, st)
        iwabs = P.sbuf("d_iwabs", [128, 32, 8], F32, st)
        iwsgn = P.sbuf("d_iwsgn", [128, 32, 8], F32, st)
        Win = P.sbuf("d_win", [128, 8, 472], BF16, st)
        Wnope = P.sbuf("d_wnope", [128, 2, 768], BF16, st)
        Wrope = P.sbuf("d_wrope", [128, 2, 256], BF16, st)
        Wrsw = P.sbuf("d_wrsw", [128, 2, 256], BF16, st)
        WukT = P.sbuf("d_wukT", [48, 16, 128], BF16, st)
        Wuv2 = P.sbuf("d_wuv2", [128, 16, 128], BF16, st)
        Wiq = P.sbuf("d_wiq", [128, 2, 512], BF16, st)
        Wiqs = P.sbuf("d_wiqs", [128, 2, 512], BF16, st)
        Wo = P.sbuf("d_wo", [128, 8, 1024], BF16, st)
        Ctm = P.sbuf("d_ctm", [128, 32, 16], F32, st)
        Stm = P.sbuf("d_stm", [128, 32, 16], F32, st)
        gq = P.sbuf("d_gq", [128, 256], F32, st)
        gkv = P.sbuf("d_gkv", [128, 128], F32, st)
        onesb = P.sbuf("d_ones", [128, 128], BF16, st)
        r_w = Res()
        r_K = Res()
        r_iw = Res()
        P.op("pool", lambda e: e.memset(onesb[:], 1.0), writes=[r_w])
        for kc in range(8):
            C.wload(Win[:, kc, :], Dm["dsa_w_in"][kc * 128:(kc + 1) * 128, :], r_w)
            C.wload(Wo[:, kc, :], Dm["dsa_w_o"][kc * 128:(kc + 1) * 128, :], r_w)
        for k2 in range(2):
            C.wload(Wnope[:, k2, :], Dm["dsa_wuq_nope"][k2 * 128:(k2 + 1) * 128, :], r_w)
            C.wload(Wrope[:, k2, :], Dm["dsa_wuq_rope"][k2 * 128:(k2 + 1) * 128, :], r_w)
            C.wload(Wrsw[:, k2, :], Dm["dsa_wuq_rsw"][k2 * 128:(k2 + 1) * 128, :], r_w)
            C.wload(Wiq[:, k2, :], Dm["dsa_wiq"][k2 * 128:(k2 + 1) * 128, :], r_w)
            C.wload(Wiqs[:, k2, :], Dm["dsa_wiq_sw"][k2 * 128:(k2 + 1) * 128, :], r_w)
        P.dma("pool", WukT[:], Dm["dsa_wukT"], writes=[r_w])
        P.dma("pool", Wuv2[:], Dm["dsa_wuv2"], writes=[r_w])
        P.dma("sp", Ctm[:], Dm["rope_ctm"].rearrange("(i p) r -> p i r", p=128), writes=[r_w])
        P.dma("sp", Stm[:], Dm["rope_stm"].rearrange("(i p) r -> p i r", p=128), writes=[r_w])
        P.dma("sp", gq[:], bass.AP(Dm["dsa_g_q"].tensor, 0, [[0, 128], [1, 256]]), writes=[r_w])
        P.dma("sp", gkv[:], bass.AP(Dm["dsa_g_kv"].tensor, 0, [[0, 128], [1, 128]]), writes=[r_w])

        with ExitStack() as sa:
            hg = [P.sbuf("da_hg%d" % i, [128, 8, 512], BF16, sa) for i in range(2)]
            pj = [P.sbuf("da_pj%d" % i, [128, 472], F32, sa) for i in range(2)]
            jk = P.sbuf("da_jk", [128, 256], F32, sa)
            stt = P.sbuf("da_st", [128, 32, 4], F32, sa)
            qln = [P.sbuf("da_qln%d" % i, [128, 256], BF16, sa) for i in range(2)]
            ik2 = [P.sbuf("da_ik2%d" % i, [128, 128], BF16, sa) for i in range(2)]
            krb = [P.sbuf("da_kr%d" % i, [128, 16], BF16, sa) for i in range(2)]
            tr = [P.sbuf("da_tr%d" % i, [128, 4, 16], F32, sa) for i in range(2)]
            r_hg = [Res(), Res()]
            r_pj = [Res(), Res()]
            r_jk = Res()
            r_st = [Res() for _ in range(32)]
            r_q = [Res(), Res()]
            r_ik = [Res(), Res()]
            r_kr = [Res(), Res()]
            r_tr = [Res(), Res()]
            hTv = Dm["hT"].rearrange("kc p t -> p kc t")
            pn = Prenorm(C, x_d, Dm["hT"], sa)
            pn.front(0)
            pn.back(0)
            for i in range(NT):
                b = i % 2
                tg, j = i // 4, i % 4
                g = tg % 2
                if j == 0:
                    if tg + 1 < 8:
                        pn.front(tg + 1)
                    P.dma("sp", hg[g][:], hTv[:, :, tg * 512:(tg + 1) * 512], reads=[C.r_hT[tg]], writes=[r_hg[g]])
                bank = C.bank(6 + b)
                for kc in range(8):
                    P.op("pe", lambda e, bank=bank, g=g, kc=kc, j=j: e.matmul(bank.ap[:, 0:472], hg[g][:, kc, j * 128:(j + 1) * 128], Win[:, kc, :],
                                                                           start=(kc == 0), stop=(kc == 7)),
                         reads=[r_hg[g], r_w], writes=[bank.res])
                P.op("act", lambda e, b=b, bank=bank: e.copy(pj[b][:], bank.ap[:, 0:472]), reads=[bank.res], writes=[r_pj[b]])
                P.op("act", lambda e, b=b, i=i: e.activation(jk[:, 0:256], pj[b][:, 0:256], AF.Square, accum_out=stt[:, i, 0:1]),
                     reads=[r_pj[b]], writes=[r_jk, r_st[i]])
                P.op("act", lambda e, b=b, i=i: e.activation(jk[:, 0:128], pj[b][:, 256:384], AF.Square, accum_out=stt[:, i, 1:2]),
                     reads=[r_pj[b]], writes=[r_jk, r_st[i]])
                P.op("act", lambda e, i=i: e.activation(stt[:, i, 2:3], stt[:, i, 0:1], AF.Sqrt, bias=C.epsc[:, 0:1], scale=1.0 / 256),
                     reads=[r_st[i], C.r_eps], writes=[r_st[i]])
                P.op("act", lambda e, i=i: e.activation(stt[:, i, 3:4], stt[:, i, 1:2], AF.Sqrt, bias=C.epsc[:, 0:1], scale=1.0 / 128),
                     reads=[r_st[i], C.r_eps], writes=[r_st[i]])
                P.op("dve", lambda e, i=i: e.reciprocal(stt[:, i, 0:2], stt[:, i, 2:4]), reads=[r_st[i]], writes=[r_st[i]])
                P.op("dve", lambda e, b=b, i=i: e.scalar_tensor_tensor(qln[b][:], pj[b][:, 0:256], stt[:, i, 0:1], gq[:], ALU.mult, ALU.mult),
                     reads=[r_pj[b], r_st[i], r_w], writes=[r_q[b]])
                P.op("dve", lambda e, b=b, i=i: e.scalar_tensor_tensor(CKV_tm[:, i, :], pj[b][:, 256:384], stt[:, i, 1:2], gkv[:], ALU.mult, ALU.mult),
                     reads=[r_pj[b], r_st[i], r_w], writes=[r_K])
                for (c0, which) in [(384, 0), (400, 1)]:
                    P.op("pool", lambda e, b=b, i=i, c0=c0, which=which: e.tensor_tensor(tr[b][:, 2 * which, :], pj[b][:, c0:c0 + 16], Ctm[:, i, :], ALU.mult),
                         reads=[r_pj[b], r_w], writes=[r_tr[b]])
                    P.op("pool", lambda e, b=b, i=i, c0=c0, which=which: e.tensor_tensor(tr[b][:, 2 * which + 1, 0:8], pj[b][:, c0 + 8:c0 + 16], Stm[:, i, 0:8], ALU.mult),
                         reads=[r_pj[b], r_w], writes=[r_tr[b]])
                    P.op("pool", lambda e, b=b, i=i, c0=c0, which=which: e.tensor_tensor(tr[b][:, 2 * which + 1, 8:16], pj[b][:, c0:c0 + 8], Stm[:, i, 8:16], ALU.mult),
                         reads=[r_pj[b], r_w], writes=[r_tr[b]])
                P.op("dve", lambda e, b=b: e.tensor_tensor(krb[b][:], tr[b][:, 0, :], tr[b][:, 1, :], ALU.add), reads=[r_tr[b]], writes=[r_kr[b]])
                P.op("dve", lambda e, b=b: e.tensor_tensor(ik2[b][:, 0:16], tr[b][:, 2, :], tr[b][:, 3, :], ALU.add), reads=[r_tr[b]], writes=[r_ik[b]])
                P.op("act", lambda e, b=b: e.copy(ik2[b][:, 16:64], pj[b][:, 416:464]), reads=[r_pj[b]], writes=[r_ik[b]])
                P.op("pool", lambda e, b=b: e.tensor_copy(ik2[b][:, 64:128], ik2[b][:, 0:64]), reads=[r_ik[b]], writes=[r_ik[b]])
                P.op("act", lambda e, b=b, i=i: e.activation(iwsgn[:, i, :], pj[b][:, 464:472], AF.Sign), reads=[r_pj[b]], writes=[r_iw])
                P.op("dve", lambda e, b=b, i=i: e.scalar_tensor_tensor(iwabs[:, i, :], pj[b][:, 464:472], KSC, iwsgn[:, i, :], ALU.mult, ALU.mult),
                     reads=[r_pj[b], r_iw], writes=[r_iw])
                bank2 = C.bank(4 + b)
                pv = bank2.ap.bitcast(BF16)
                srcs = [(qln[b][:, 0:128], r_q[b], 128), (qln[b][:, 128:256], r_q[b], 128), (CKV_tm[:, i, :], r_K, 128),
                        (ik2[b][:], r_ik[b], 128), (krb[b][:], r_kr[b], 16)]
                for k, (src, rs, n) in enumerate(srcs):
                    P.op("pe", lambda e, pv=pv, k=k, src=src, n=n: e.transpose(pv[0:n, k * 128:(k + 1) * 128], src, C.identb[:]),
                         reads=[rs, C.r_ident], writes=[bank2.res])
                cs = slice(i * 128, (i + 1) * 128)
                P.op("act", lambda e, pv=pv, cs=cs: e.copy(QLN_T[:, :, cs], pv[:, 0:256].rearrange("p (k t) -> p k t", k=2)), reads=[bank2.res], writes=[r_K])
                P.op("dve", lambda e, pv=pv, cs=cs: e.tensor_copy(CKV_T[:, cs], pv[:, 256:384]), reads=[bank2.res], writes=[r_K])
                P.op("act", lambda e, pv=pv, cs=cs: e.copy(IK_T2[:, cs], pv[:, 384:512]), reads=[bank2.res], writes=[r_K])
                P.op("dve", lambda e, pv=pv, cs=cs: e.tensor_copy(KR_T[:, cs], pv[0:16, 512:640]), reads=[bank2.res], writes=[r_K])
                if j == 3 and tg + 1 < 8:
                    pn.back(tg + 1)
            P.barrier()

        SC = P.sbuf("d_sc", [128, T], F32, st)
        MK = P.sbuf("d_mk", [128, T], BF16, st)
        MT = P.sbuf("d_mt", [128, 32, 128], BF16, st)
        Rr = [P.sbuf("d_r%d" % i, [128, 512], F32, st) for i in range(2)]
        NE = 4
        Eb = [P.sbuf("d_e%d" % i, [128, 512], BF16, st) for i in range(NE)]
        rec = P.sbuf("d_rec", [128, 512], F32, st)
        ON = P.sbuf("d_on", [128, 16, 128], BF16, st)
        QABS = [P.sbuf("d_qabs%d" % i, [128, 16, 128], BF16, st) for i in range(2)]
        QN = P.sbuf("d_qn", [48, 16, 128], BF16, st)
        QR = [P.sbuf("d_qr%d" % i, [16, 16, 128], BF16, st) for i in range(2)]
        IQ = P.sbuf("d_iq", [128, 4, 128], BF16, st)
        OV = P.sbuf("d_ov", [128, 8, 128], BF16, st)
        Cq = P.sbuf("d_cq", [128, 128], F32, st)
        Sq = P.sbuf("d_sq", [128, 128], F32, st)
        tq = [P.sbuf("d_tq%d" % i, [128, 512], F32, st) for i in range(2)]
        xs = P.sbuf("d_xs", [128, 1024], F32, st)
        ys = P.sbuf("d_ys", [128, 1024], F32, st)
        bs = P.sbuf("d_bs", [128, 8], F32, st)
        thrneg = P.sbuf("d_thrneg", [128, 1], F32, st)
        identN = P.sbuf("d_identN", [128, 128], BF16, st)
        r_SC, r_MK, r_MT, r_rec, r_ON, r_QN, r_IQ, r_OV, r_cs, r_xs, r_ys, r_bs = [Res() for _ in range(12)]
        r_QABS = [Res(), Res()]
        r_QR = [Res(), Res()]
        r_R = [Res(), Res()]
        r_E = [Res() for _ in range(NE)]
        r_tq = [Res(), Res()]
        r_thrneg = Res()
        r_idn = Res()
        P.op("pool", lambda e: e.memset(thrneg[:], -1e29), writes=[r_thrneg])
        P.op("act", lambda e: e.activation(identN[:], C.identb[:], AF.Copy, scale=30000.0), reads=[C.r_ident], writes=[r_idn])
        SB = [C.bank(0), C.bank(1), C.bank(4), C.bank(5)]
        st_ = {"ecnt": 0, "scnt": 0}

        def stage_b1(qt):
            qs = slice(qt * 128, (qt + 1) * 128)
            qp = qt % 2
            P.dma("sp", Cq[:], Dm["rope_cfm"][:, qs], writes=[r_cs])
            P.dma("sp", Sq[:], Dm["rope_sfm"][:, qs], writes=[r_cs])
            for hq in range(4):
                bank = C.bank(6 + (hq % 2))
                for hh in range(4):
                    h = hq * 4 + hh
                    for k2 in range(2):
                        P.op("pe", lambda e, bank=bank, hh=hh, h=h, k2=k2, qs=qs: e.matmul(
                            bank.ap[0:48, hh * 128:(hh + 1) * 128], Wnope[:, k2, h * 48:(h + 1) * 48], QLN_T[:, k2, qs], start=(k2 == 0), stop=(k2 == 1)),
                            reads=[r_w, r_K], writes=[bank.res])
                P.op("act", lambda e, bank=bank, hq=hq: e.copy(QN[:, hq * 4:(hq + 1) * 4, :], bank.ap[0:48, :].rearrange("p (h t) -> p h t", h=4)),
                     reads=[bank.res], writes=[r_QN])
            for hq in range(4):
                bank = C.bank(6 + (hq % 2))
                for hh in range(4):
                    h = hq * 4 + hh
                    P.op("pe", lambda e, bank=bank, hh=hh, h=h: e.matmul(bank.ap[:, hh * 128:(hh + 1) * 128], WukT[:, h, :], QN[:, h, :], start=True, stop=True),
                         reads=[r_w, r_QN], writes=[bank.res])
                P.op("act", lambda e, bank=bank, hq=hq, qp=qp: e.copy(QABS[qp][:, hq * 4:(hq + 1) * 4, :], bank.ap[:, :].rearrange("p (h t) -> p h t", h=4)),
                     reads=[bank.res], writes=[r_QABS[qp]])
            for hq in range(4):
                b0, b1 = C.bank(6), C.bank(7)
                for (bank, Wt) in [(b0, Wrope), (b1, Wrsw)]:
                    for hh in range(4):
                        h = hq * 4 + hh
                        for k2 in range(2):
                            P.op("pe", lambda e, bank=bank, Wt=Wt, hh=hh, h=h, k2=k2, qs=qs: e.matmul(
                                bank.ap[0:16, hh * 128:(hh + 1) * 128], Wt[:, k2, h * 16:(h + 1) * 16], QLN_T[:, k2, qs], start=(k2 == 0), stop=(k2 == 1)),
                                reads=[r_w, r_K], writes=[bank.res])
                P.op("dve", lambda e: e.tensor_tensor(tq[0][0:16, :].rearrange("p (h t) -> p h t", h=4), C.bank(6).ap[0:16, :].rearrange("p (h t) -> p h t", h=4),
                                                      fbc(Cq[0:16, :], 0, 4), ALU.mult), reads=[b0.res, r_cs], writes=[r_tq[0]])
                P.op("dve", lambda e: e.tensor_tensor(tq[1][0:16, :].rearrange("p (h t) -> p h t", h=4), C.bank(7).ap[0:16, :].rearrange("p (h t) -> p h t", h=4),
                                                      fbc(Sq[0:16, :], 0, 4), ALU.mult), reads=[b1.res, r_cs], writes=[r_tq[1]])
                P.op("pool", lambda e, hq=hq, qp=qp: e.tensor_tensor(QR[qp][:, hq * 4:(hq + 1) * 4, :], tq[0][0:16, :].rearrange("p (h t) -> p h t", h=4),
                                                                     tq[1][0:16, :].rearrange("p (h t) -> p h t", h=4), ALU.add),
                     reads=r_tq, writes=[r_QR[qp]])
            b0, b1 = C.bank(6), C.bank(7)
            for (bank, Wt) in [(b0, Wiq), (b1, Wiqs)]:
                for ch in range(4):
                    for k2 in range(2):
                        P.op("pe", lambda e, bank=bank, Wt=Wt, ch=ch, k2=k2, qs=qs: e.matmul(
                            bank.ap[:, ch * 128:(ch + 1) * 128], Wt[:, k2, ch * 128:(ch + 1) * 128], QLN_T[:, k2, qs], start=(k2 == 0), stop=(k2 == 1)),
                            reads=[r_w, r_K], writes=[bank.res])
            P.op("dve", lambda e: e.tensor_tensor(tq[0][:].rearrange("p (h t) -> p h t", h=4), C.bank(6).ap[:, :].rearrange("p (h t) -> p h t", h=4),
                                                  fbc(Cq[:], 0, 4), ALU.mult), reads=[b0.res, r_cs], writes=[r_tq[0]])
            P.op("dve", lambda e: e.tensor_tensor(tq[1][:].rearrange("p (h t) -> p h t", h=4), C.bank(7).ap[:, :].rearrange("p (h t) -> p h t", h=4),
                                                  fbc(Sq[:], 0, 4), ALU.mult), reads=[b1.res, r_cs], writes=[r_tq[1]])
            P.op("pool", lambda e: e.tensor_tensor(IQ[:].rearrange("p h t -> p (h t)"), tq[0][:], tq[1][:], ALU.add), reads=r_tq, writes=[r_IQ])

        def stage_b2(qt):
            qs = slice(qt * 128, (qt + 1) * 128)
            nk = (qt + 1) * 128
            ng = (nk + 511) // 512
            for kg in range(ng):
                ncol = min(512, nk - kg * 512)
                ks = slice(kg * 512, kg * 512 + ncol)
                for h in range(8):
                    bank = C.bank(6 + (h % 2))
                    rb = st_["ecnt"] % 2
                    st_["ecnt"] += 1
                    pb = (h % 2) * 64
                    P.op("pe", lambda e, bank=bank, pb=pb, h=h, ks=ks, ncol=ncol: e.matmul(
                        bank.ap[:, 0:ncol], IQ[pb:pb + 64, h // 2, :], IK_T2[pb:pb + 64, ks], start=True, stop=True),
                        reads=[r_IQ, r_K], writes=[bank.res])
                    P.op("act", lambda e, bank=bank, rb=rb, ncol=ncol, qt=qt, h=h: e.activation(
                        Rr[rb][:, 0:ncol], bank.ap[:, 0:ncol], AF.Relu, scale=iwabs[:, qt, h:h + 1]),
                        reads=[bank.res, r_iw], writes=[r_R[rb]])
                    if h == 0:
                        P.op("dve", lambda e, rb=rb, ncol=ncol, ks=ks, qt=qt, h=h: e.tensor_scalar(
                            SC[:, ks], Rr[rb][:, 0:ncol], iwsgn[:, qt, h:h + 1], None, ALU.mult),
                            reads=[r_R[rb], r_iw], writes=[r_SC])
                    else:
                        P.op("dve", lambda e, rb=rb, ncol=ncol, ks=ks, qt=qt, h=h: e.scalar_tensor_tensor(
                            SC[:, ks], Rr[rb][:, 0:ncol], iwsgn[:, qt, h:h + 1], SC[:, ks], ALU.mult, ALU.add),
                            reads=[r_R[rb], r_iw, r_SC], writes=[r_SC])
            P.op("pool", lambda e, qs=qs: e.affine_select(SC[:, qs], SC[:, qs], [[-1, 128]], ALU.is_ge, -1e30, base=0, channel_multiplier=1),
                 reads=[r_SC], writes=[r_SC])
            if qt >= 2:
                P.op("dve", lambda e, nk=nk: e.tensor_reduce(bs[:, 0:1], SC[:, 0:nk], AX.X, ALU.max), reads=[r_SC], writes=[r_bs])
                P.op("dve", lambda e, qt=qt: e.tensor_reduce(bs[:, 1:2], SC[:, 0:qt * 128], AX.X, ALU.min), reads=[r_SC], writes=[r_bs])
                P.op("dve", lambda e: e.tensor_tensor(bs[:, 2:3], bs[:, 0:1], bs[:, 1:2], ALU.subtract), reads=[r_bs], writes=[r_bs])

        def stage_b4(qt, k0, k1):
            nk = (qt + 1) * 128
            if qt < 2:
                return
            for k in range(k0, k1):
                f = 2.0 ** (-k)
                P.op("dve", lambda e, f=f: e.scalar_tensor_tensor(bs[:, 3:4], bs[:, 2:3], f, bs[:, 1:2], ALU.mult, ALU.add), reads=[r_bs], writes=[r_bs])
                P.op("dve", lambda e, nk=nk: e.tensor_scalar(MK[:, 0:nk], SC[:, 0:nk], bs[:, 3:4], 0.0, ALU.is_ge, ALU.add, accum_out=bs[:, 4:5]),
                     reads=[r_SC, r_bs], writes=[r_MK, r_bs])
                P.op("dve", lambda e: e.scalar_tensor_tensor(bs[:, 5:6], bs[:, 4:5], 255.5, bs[:, 2:3], ALU.is_ge, ALU.mult), reads=[r_bs], writes=[r_bs])
                P.op("dve", lambda e, f=f: e.scalar_tensor_tensor(bs[:, 1:2], bs[:, 5:6], f, bs[:, 1:2], ALU.mult, ALU.add), reads=[r_bs], writes=[r_bs])

        def stage_b5(qt):
            nk = (qt + 1) * 128
            if qt >= 2:
                thr_ap, thr_res = bs[:, 1:2], r_bs
            else:
                thr_ap, thr_res = thrneg[:, 0:1], r_thrneg
            P.op("dve", lambda e, nk=nk, thr_ap=thr_ap: e.tensor_scalar(MK[:, 0:nk], SC[:, 0:nk], thr_ap, 1.0, ALU.is_ge, ALU.subtract),
                 reads=[r_SC, thr_res], writes=[r_MK])
            for k4 in range((qt + 4) // 4):
                bank = C.bank(6 + (k4 % 2))
                pv = bank.ap.bitcast(BF16)
                nn = min(4, qt + 1 - k4 * 4)
                for kk in range(nn):
                    kt = k4 * 4 + kk
                    P.op("pe", lambda e, pv=pv, kk=kk, kt=kt: e.transpose(pv[:, kk * 128:(kk + 1) * 128], MK[:, kt * 128:(kt + 1) * 128], C.identb[:]),
                         reads=[r_MK, C.r_ident], writes=[bank.res])
                P.op("act", lambda e, pv=pv, k4=k4, nn=nn: e.copy(MT[:, k4 * 4:k4 * 4 + nn, :], pv[:, 0:nn * 128].rearrange("p (k t) -> p k t", k=nn)),
                     reads=[bank.res], writes=[r_MT])

        def stage_b6_cg(qt, cg):
            qp = qt % 2
            bo, br = C.bank(2 + 4 * (cg % 2)), C.bank(3 + 4 * (cg % 2))
            qa = QABS[qp][:, cg * 4:(cg + 1) * 4, :].rearrange("p h t -> p (h t)")
            qr = QR[qp][:, cg * 4:(cg + 1) * 4, :].rearrange("p h t -> p (h t)")
            LA = 3
            slots = {}

            def emit_s(kt):
                i = st_["scnt"]
                st_["scnt"] += 1
                sb = SB[i % 4]
                eb = i % NE
                slots[kt] = (sb, eb)
                kss = slice(kt * 128, (kt + 1) * 128)
                P.op("pe", lambda e, sb=sb, kss=kss, qa=qa: e.matmul(sb.ap[:, :], CKV_T[:, kss], qa, start=True, stop=False),
                     reads=[r_K, r_QABS[qp]], writes=[sb.res])
                P.op("pe", lambda e, sb=sb, kss=kss, qr=qr: e.matmul(sb.ap[:, :], KR_T[:, kss], qr, start=False, stop=False),
                     reads=[r_K, r_QR[qp]], writes=[sb.res])
                P.op("pe", lambda e, sb=sb, kt=kt: e.matmul(sb.ap[:, :].rearrange("p (h t) -> p h t", h=4), identN[:], fbc(MT[:, kt, :], 0, 4), start=False, stop=True),
                     reads=[r_idn, r_MT], writes=[sb.res])
                P.op("act", lambda e, sb=sb, eb=eb: e.activation(Eb[eb][:], sb.ap[:, :], AF.Exp, scale=0.125), reads=[sb.res], writes=[r_E[eb]])

            for kt in range(min(LA, qt + 1)):
                emit_s(kt)
            for kt in range(qt + 1):
                sb, eb = slots[kt]
                P.op("pe", lambda e, bo=bo, kt=kt, eb=eb, qt=qt: e.matmul(bo.ap[:, :], CKV_tm[:, kt, :], Eb[eb][:], start=(kt == 0), stop=(kt == qt)),
                     reads=[r_K, r_E[eb]], writes=[bo.res])
                P.op("pe", lambda e, br=br, kt=kt, eb=eb, qt=qt: e.matmul(br.ap[:, :], onesb[:], Eb[eb][:], start=(kt == 0), stop=(kt == qt)),
                     reads=[r_w, r_E[eb]], writes=[br.res])
                if kt + LA <= qt:
                    emit_s(kt + LA)
            P.op("dve", lambda e, br=br: e.reciprocal(rec[:], br.ap[:, :]), reads=[br.res], writes=[r_rec])
            P.op("dve", lambda e, bo=bo, cg=cg: e.tensor_tensor(ON[:, cg * 4:(cg + 1) * 4, :].rearrange("p h t -> p (h t)"), bo.ap[:, :], rec[:], ALU.mult),
                 reads=[bo.res, r_rec], writes=[r_ON])

        def stage_b7(qt):
            qs = slice(qt * 128, (qt + 1) * 128)
            P.dma("sp", xs[:], x_d[qs, :], writes=[r_xs])
            for hf in range(2):
                bank = C.bank(6 + hf)
                for jj in range(4):
                    j = hf * 4 + jj
                    for u in range(2):
                        h = 2 * j + u
                        P.op("pe", lambda e, bank=bank, jj=jj, h=h, u=u: e.matmul(bank.ap[:, jj * 128:(jj + 1) * 128], Wuv2[:, h, :], ON[:, h, :], start=(u == 0), stop=(u == 1)),
                             reads=[r_w, r_ON], writes=[bank.res])
                P.op("act", lambda e, bank=bank, hf=hf: e.copy(OV[:, hf * 4:(hf + 1) * 4, :], bank.ap[:, :].rearrange("p (j t) -> p j t", j=4)),
                     reads=[bank.res], writes=[r_OV])
            for half in range(2):
                bank = C.bank(6 + half)
                for j in range(8):
                    P.op("pe", lambda e, bank=bank, j=j, half=half: e.matmul(bank.ap[:, :], OV[:, j, :], Wo[:, j, half * 512:(half + 1) * 512], start=(j == 0), stop=(j == 7)),
                         reads=[r_OV, r_w], writes=[bank.res])
                hs = slice(half * 512, (half + 1) * 512)
                P.op("dve", lambda e, bank=bank, hs=hs: e.tensor_tensor(ys[:, hs], bank.ap[:, :], C.gate[:, hs], ALU.mult), reads=[bank.res, C.r_gate], writes=[r_ys])
                P.op("pool", lambda e, hs=hs: e.tensor_tensor(ys[:, hs], ys[:, hs], xs[:, hs], ALU.add), reads=[r_ys, r_xs], writes=[r_ys])
            tok = P.dma("sp", xout_d[qs, :], ys[:], reads=[r_ys], writes=[C.r_xout[qt]])
            C.final.append(tok)

        nq = NT
        if DBG["stop"] == "dsa_qt":
            nq = DBG.get("nqt", 3) + 1
        stage_b1(0)
        stage_b2(0)
        stage_b4(0, 1, 17)
        stage_b5(0)
        for qt in range(nq):
            nx = qt + 1
            has = nx < nq
            if has:
                stage_b1(nx)
                stage_b2(nx)
            for cg in range(4):
                stage_b6_cg(qt, cg)
                if has:
                    stage_b4(nx, 1 + 4 * cg, 5 + 4 * cg)
            stage_b7(qt)
            if has:
                stage_b5(nx)
        P.barrier()


def nsa_stage(C, x_d, xout_d):
    P, Dm = C.P, C.D
    with ExitStack() as st:
        KS_T = P.sbuf("n_ksT", [128, 2, T], BF16, st)
        KW_T = P.sbuf("n_kwT", [128, 2, T], BF16, st)
        VS = P.sbuf("n_vs", [128, 32, 4, 65], BF16, st)
        VW = P.sbuf("n_vw", [128, 32, 4, 65], BF16, st)
        KCM = P.sbuf("n_kcm", [128, 2, 256], BF16, st)
        VCM = P.sbuf("n_vcm", [128, 2, 4, 65], BF16, st)
        GT = P.sbuf("n_gt", [128, 32, 48], F32, st)
        r_w, r_K, r_V, r_G, r_cm = Res(), Res(), Res(), Res(), Res()
        P.op("pool", lambda e: e.memset(VS[:, :, :, 64:65], 1.0), writes=[r_V])
        P.op("pool", lambda e: e.memset(VW[:, :, :, 64:65], 1.0), writes=[r_V])
        P.op("pool", lambda e: e.memset(VCM[:], 0.0), writes=[r_cm])
        P.op("pool", lambda e: e.memset(VCM[:, :, :, 64:65], 1.0), writes=[r_cm])
        P.op("pool", lambda e: e.memset(KCM[:], 0.0), writes=[r_cm])
        hTv = Dm["hT"].rearrange("kc p t -> p kc t")
        qTv = Dm["qT"].rearrange("b p t -> p b t")
        r_qT = [Res() for _ in range(8)]

        with ExitStack() as sa:
            KC_T = P.sbuf("na_kcT", [128, 2, T], BF16, sa)
            VC_T = P.sbuf("na_vcT", [128, 2, T], BF16, sa)
            r_kc = Res()
            cnt = 0
            for ppass in range(2):
              with ExitStack() as sp_:
                if ppass == 0:
                    Wk = P.sbuf("na_wk", [128, 4, 8, 256], BF16, sp_)
                    Wks = P.sbuf("na_wks", [128, 3, 8, 256], BF16, sp_)
                    Wvg = P.sbuf("na_wvg", [128, 8, 560], BF16, sp_)
                else:
                    Wq = P.sbuf("na_wq", [128, 8, 1024], BF16, sp_)
                    Wqs = P.sbuf("na_wqs", [128, 8, 1024], BF16, sp_)
                    qg = P.sbuf("na_qg", [128, 8, 512], BF16, sp_)
                hg = [P.sbuf("na_hg%d_%d" % (i, ppass), [128, 8, 512], BF16, sp_) for i in range(2)]
                Cg = P.sbuf("na_cg%d" % ppass, [128, 512], F32, sp_)
                Sg = P.sbuf("na_sg%d" % ppass, [128, 512], F32, sp_)
                t0 = [P.sbuf("na_t0%d_%d" % (i, ppass), [128, 512], F32, sp_) for i in range(2)]
                t1 = [P.sbuf("na_t1%d_%d" % (i, ppass), [128, 512], F32, sp_) for i in range(2)]
                r_wa = Res()
                r_hg = [Res(), Res()]
                r_cs = Res()
                r_t0 = [Res(), Res()]
                r_t1 = [Res(), Res()]
                r_qg = Res()
                for kc in range(8):
                    if ppass == 0:
                        for t in range(4):
                            C.wload(Wk[:, t, kc, :], Dm["nsa_wk"][t, kc * 128:(kc + 1) * 128, :], r_wa)
                        for t in range(3):
                            C.wload(Wks[:, t, kc, :], Dm["nsa_wk_sw"][t, kc * 128:(kc + 1) * 128, :], r_wa)
                        C.wload(Wvg[:, kc, :], Dm["nsa_wvg"][kc * 128:(kc + 1) * 128, :], r_wa)
                    else:
                        C.wload(Wq[:, kc, :], Dm["nsa_wq"][kc * 128:(kc + 1) * 128, :], r_wa)
                        C.wload(Wqs[:, kc, :], Dm["nsa_wq_sw"][kc * 128:(kc + 1) * 128, :], r_wa)
                for tg in range(8):
                    g2 = tg % 2
                    ts = slice(tg * 512, (tg + 1) * 512)
                    P.dma("sp", hg[g2][:], hTv[:, :, ts], reads=[C.r_hT[tg]], writes=[r_hg[g2]])
                    P.dma("sp", Cg[:], Dm["rope_cfm"][:, ts], writes=[r_cs])
                    P.dma("sp", Sg[:], Dm["rope_sfm"][:, ts], writes=[r_cs])

                    hgt, r_hgt = hg[g2], r_hg[g2]

                    def proj_pair(wp, ws, dst, dres, rope, hgt=hgt, r_hgt=r_hgt, Cg=Cg, Sg=Sg, t0=t0, t1=t1, r_t0=r_t0, r_t1=r_t1, r_cs=r_cs, r_wa=r_wa):
                        nonlocal cnt
                        k = cnt % 2
                        cnt += 1
                        bp, bsw = C.bank(k), C.bank(2 + k)
                        for kc in range(8):
                            P.op("pe", lambda e, hgt=hgt, bp=bp, wp=wp, kc=kc: e.matmul(bp.ap[:, :], wp(kc), hgt[:, kc, :], start=(kc == 0), stop=(kc == 7)),
                                 reads=[r_wa, r_hgt], writes=[bp.res])
                        if not rope:
                            P.op("act", lambda e, bp=bp, dst=dst: e.copy(dst, bp.ap[:, :]), reads=[bp.res], writes=[dres])
                            return
                        for kc in range(8):
                            P.op("pe", lambda e, hgt=hgt, bsw=bsw, ws=ws, kc=kc: e.matmul(bsw.ap[:, :], ws(kc), hgt[:, kc, :], start=(kc == 0), stop=(kc == 7)),
                                 reads=[r_wa, r_hgt], writes=[bsw.res])
                        P.op("dve", lambda e, bp=bp, k=k: e.tensor_tensor(t0[k][:], bp.ap[:, :], Cg[:], ALU.mult), reads=[bp.res, r_cs], writes=[r_t0[k]])
                        P.op("dve", lambda e, bsw=bsw, k=k: e.tensor_tensor(t1[k][:], bsw.ap[:, :], Sg[:], ALU.mult), reads=[bsw.res, r_cs], writes=[r_t1[k]])
                        P.op("pool", lambda e, k=k, dst=dst: e.tensor_tensor(dst, t0[k][:], t1[k][:], ALU.add), reads=[r_t0[k], r_t1[k]], writes=[dres])

                    for a in (range(2) if ppass == 0 else []):
                        acs = slice(a * 128, (a + 1) * 128)
                        for t, dstT, dres in [(0, KC_T, r_kc), (1, KS_T, r_K), (2, KW_T, r_K)]:
                            proj_pair(lambda kc, t=t, acs=acs: Wk[:, t, kc, acs], lambda kc, t=t, acs=acs: Wks[:, t, kc, acs], dstT[:, a, ts], dres, True)
                        proj_pair(lambda kc, acs=acs: Wk[:, 3, kc, acs], None, VC_T[:, a, ts], r_kc, False)
                    for blk in (range(8) if ppass == 1 else []):
                        bcs = slice(blk * 128, (blk + 1) * 128)
                        proj_pair(lambda kc, bcs=bcs: Wq[:, kc, bcs], lambda kc, bcs=bcs: Wqs[:, kc, bcs], qg[:, blk, :], r_qg, True)
                    if ppass == 1:
                        P.dma("sp", qTv[:, :, ts], qg[:], reads=[r_qg], writes=[r_qT[tg]])
                    for j in (range(4) if ppass == 0 else []):
                        i = tg * 4 + j
                        b1, b2 = C.bank(4 + (j % 2)), C.bank(6 + (j % 2))
                        for kc in range(8):
                            P.op("pe", lambda e, hgt=hgt, b1=b1, kc=kc, j=j: e.matmul(b1.ap[:, :], hgt[:, kc, j * 128:(j + 1) * 128], Wvg[:, kc, 0:512], start=(kc == 0), stop=(kc == 7)),
                                 reads=[r_wa, r_hgt], writes=[b1.res])
                        for kc in range(8):
                            P.op("pe", lambda e, hgt=hgt, b2=b2, kc=kc, j=j: e.matmul(b2.ap[:, 0:48], hgt[:, kc, j * 128:(j + 1) * 128], Wvg[:, kc, 512:560], start=(kc == 0), stop=(kc == 7)),
                                 reads=[r_wa, r_hgt], writes=[b2.res])
                        P.op("act", lambda e, b1=b1, i=i: e.copy(VS[:, i, :, 0:64], b1.ap[:, 0:256].rearrange("p (g d) -> p g d", g=4)), reads=[b1.res], writes=[r_V])
                        P.op("dve", lambda e, b1=b1, i=i: e.tensor_copy(VW[:, i, :, 0:64], b1.ap[:, 256:512].rearrange("p (g d) -> p g d", g=4)), reads=[b1.res], writes=[r_V])
                        P.op("act", lambda e, b2=b2, i=i: e.activation(GT[:, i, :], b2.ap[:, 0:48], AF.Sigmoid), reads=[b2.res], writes=[r_G])
                P.barrier()
            if DBG["stop"] == "nsa_A":
                C.final.append(P.dma("sp", xout_d[0:128, :], x_d[0:128, :], writes=[Res()]))
                P.barrier()
                return
            W1s = [P.sbuf("na_w1s%d" % i, [128, 32, 256], BF16, sa) for i in range(2)]
            peT = P.sbuf("na_peT", [128, 32], BF16, sa)
            W2k2 = P.sbuf("na_w2k2", [128, 2, 2, 128], BF16, sa)
            W2v = P.sbuf("na_w2v", [128, 2, 64], BF16, sa)
            cb = P.sbuf("na_cb", [128, 4], F32, sa)
            hidT = [P.sbuf("na_hid%d" % i, [128, 2, 256], BF16, sa) for i in range(2)]
            r_cw, r_cb = Res(), Res()
            r_hid = [Res(), Res()]
            P.dma("pool", W1s[0][:], Dm["nsa_w1k"], writes=[r_cw])
            P.dma("pool", W1s[1][:], Dm["nsa_w1v"], writes=[r_cw])
            P.dma("pool", peT[:], Dm["nsa_peT"], writes=[r_cw])
            P.dma("pool", W2k2[:], Dm["nsa_w2k2"].rearrange("(c p) s m -> p c s m", p=128), writes=[r_cw])
            P.dma("pool", W2v[:], Dm["nsa_w2v"].rearrange("(c p) m -> p c m", p=128), writes=[r_cw])
            for kv in range(2):
                for ch in range(2):
                    bank = C.bank(6 + ch)
                    for l in range(32):
                        P.op("pe", lambda e, bank=bank, kv=kv, ch=ch, l=l: e.matmul(bank.ap[:, 0:1], W1s[kv][0:64, l, ch * 128:(ch + 1) * 128], peT[0:64, l:l + 1],
                                                                                    start=(l == 0), stop=(l == 31)),
                             reads=[r_cw], writes=[bank.res])
                    P.op("act", lambda e, bank=bank, kv=kv, ch=ch: e.copy(cb[:, kv * 2 + ch:kv * 2 + ch + 1], bank.ap[:, 0:1]), reads=[bank.res], writes=[r_cb])
            hc = 0
            for kv in range(2):
                srcT = KC_T if kv == 0 else VC_T
                for g in range(4):
                    a, s = g // 2, g % 2
                    pb = s * 64
                    hb = hc % 2
                    hc += 1
                    for ch in range(2):
                        bank = C.bank(4 + ch)
                        for l in range(32):
                            base = srcT[pb:pb + 64, a, 0:16]
                            rhs = bass.AP(base.tensor, base.offset + l, [list(base.ap[0]), [16, 255]])
                            P.op("pe", lambda e, bank=bank, kv=kv, ch=ch, l=l, pb=pb, rhs=rhs: e.matmul(
                                bank.ap[:, 0:255], W1s[kv][pb:pb + 64, l, ch * 128:(ch + 1) * 128], rhs, start=(l == 0), stop=(l == 31)),
                                reads=[r_cw, r_kc], writes=[bank.res])
                        P.op("act", lambda e, bank=bank, kv=kv, ch=ch, hb=hb: e.activation(hidT[hb][:, ch, 0:255], bank.ap[:, 0:255], AF.Silu,
                                                                                            bias=cb[:, kv * 2 + ch:kv * 2 + ch + 1]),
                             reads=[bank.res, r_cb], writes=[r_hid[hb]])
                    if kv == 0:
                        bank = C.bank(6)
                        for ch in range(2):
                            P.op("pe", lambda e, bank=bank, ch=ch, s=s, hb=hb: e.matmul(bank.ap[:, 0:255], W2k2[:, ch, s, :], hidT[hb][:, ch, 0:255], start=(ch == 0), stop=(ch == 1)),
                                 reads=[r_cw, r_hid[hb]], writes=[bank.res])
                        P.op("dve", lambda e, bank=bank, pb=pb, a=a: e.tensor_copy(KCM[pb:pb + 64, a, 0:255], bank.ap[pb:pb + 64, 0:255]), reads=[bank.res], writes=[r_cm])
                    else:
                        for nt in range(2):
                            nn = 128 if nt == 0 else 127
                            bank = C.bank(6 + nt)
                            for ch in range(2):
                                P.op("pe", lambda e, bank=bank, ch=ch, nt=nt, nn=nn, hb=hb: e.matmul(bank.ap[0:nn, 0:64], hidT[hb][:, ch, nt * 128:nt * 128 + nn], W2v[:, ch, :],
                                                                                                   start=(ch == 0), stop=(ch == 1)),
                                     reads=[r_cw, r_hid[hb]], writes=[bank.res])
                            P.op("dve", lambda e, bank=bank, nt=nt, nn=nn, g=g: e.tensor_copy(VCM[0:nn, nt, g, 0:64], bank.ap[0:nn, 0:64]), reads=[bank.res], writes=[r_cm])
            P.barrier()

        if DBG["stop"] == "nsa_C":
            C.final.append(P.dma("sp", xout_d[0:128, :], x_d[0:128, :], writes=[Res()]))
            P.barrier()
            return
        Wo = P.sbuf("n_wo", [128, 8, 1024], BF16, st)
        esel = P.sbuf("n_esel", [128, 32, 128], BF16, st)
        ntri = P.sbuf("n_ntri", [128, 2, 128], BF16, st)
        ncmp = P.sbuf("n_ncmp", [128, NCMP_N, 128], BF16, st)
        fa = P.sbuf("n_fa", [128, 32, 64], F32, st)
        cmast = P.sbuf("n_cmast", [128, 512], F32, st)
        for kc in range(8):
            C.wload(Wo[:, kc, :], Dm["nsa_w_o"][kc * 128:(kc + 1) * 128, :], r_w)
        P.dma("sp", esel[:], Dm["nsa_esel"], writes=[r_w])
        P.dma("sp", ntri[:], Dm["nsa_ntri"], writes=[r_w])
        P.dma("sp", ncmp[:], Dm["nsa_ncmp"], writes=[r_w])
        P.dma("sp", fa[:], Dm["nsa_fa"], writes=[r_w])
        P.dma("sp", cmast[:], Dm["nsa_cmast"], writes=[r_w])
        qgB = [P.sbuf("n_qg%d" % i, [128, 8, 512], BF16, st) for i in range(2)]
        NPM = 6
        PM = [P.sbuf("n_pm%d" % i, [128, 4, 128], BF16, st) for i in range(NPM)]
        ee = [P.sbuf("n_ee%d" % i, [128, 2, 256], F32, st) for i in range(2)]
        em = [P.sbuf("n_em%d" % i, [128, 256], F32, st) for i in range(2)]
        pcs = P.sbuf("n_pcs", [128, 4, 256], F32, st)
        imp = P.sbuf("n_imp", [128, 4, 64], F32, st)
        imp3 = P.sbuf("n_imp3", [128, 64], F32, st)
        m8 = P.sbuf("n_m8", [128, 4, 16], F32, st)
        sm = P.sbuf("n_sm", [128, 64], F32, st)
        nsel = P.sbuf("n_nsel", [128, 2, 128], BF16, st)
        nselT = P.sbuf("n_nselT", [128, 2, 128], BF16, st)
        oacc = P.sbuf("n_oacc", [128, 16, 64], F32, st)
        otmp = P.sbuf("n_otmp", [128, 4, 64], F32, st)
        fac = P.sbuf("n_fac", [128, 16], F32, st)
        otmp2 = [P.sbuf("n_otmp2_%d" % i, [128, 4, 64], F32, st) for i in range(2)]
        obf = P.sbuf("n_obf", [128, 1024], BF16, st)
        OT = P.sbuf("n_oT", [128, 8, 128], BF16, st)
        xs = P.sbuf("n_xs", [128, 1024], F32, st)
        ys = P.sbuf("n_ys", [128, 1024], F32, st)
        r_qg = [Res(), Res()]
        r_PM = [Res() for _ in range(6)]
        r_ee = [Res(), Res()]
        r_em = [Res(), Res()]
        r_pcs, r_imp, r_imp3, r_m8, r_sm, r_nsel, r_nselT, r_oacc, r_otmp, r_fac, r_obf, r_OT, r_xs, r_ys = [Res() for _ in range(14)]
        P.op("pool", lambda e: e.memset(pcs[:], 0.0), writes=[r_pcs])
        SB = [C.bank(0), C.bank(1), C.bank(4), C.bank(5)]
        cst = {"pm": 0, "sb": 0, "ob": 0}

        def qbuf(qt):
            return (qt // 4) % 2

        def load_q(qt):
            tg = qt // 4
            qb = qbuf(qt)
            P.dma("sp", qgB[qb][:], qTv[:, :, tg * 512:(tg + 1) * 512], reads=[r_qT[tg]], writes=[r_qg[qb]])

        def sel_scores(qt, hps):
            qb = qbuf(qt)
            ql = slice((qt % 4) * 128, (qt % 4 + 1) * 128)
            u0 = 248 - 8 * qt
            for hp in hps:
                a, u = hp // 4, hp % 4
                eb = hp % 2
                for s in range(2):
                    bank = C.bank(4 + s)
                    qap = qgB[qb][s * 64:(s + 1) * 64, a * 4 + u, ql]
                    P.op("pe", lambda e, bank=bank, s=s, a=a, qap=qap: e.matmul(bank.ap[:, 0:255], qap,
                                                                                KCM[s * 64:(s + 1) * 64, a, 0:255], start=True, stop=True),
                         reads=[r_qg[qb], r_cm], writes=[bank.res])
                    P.op("act", lambda e, bank=bank, eb=eb, s=s: e.activation(ee[eb][:, s, 0:255], bank.ap[:, 0:255], AF.Exp, scale=0.125),
                         reads=[bank.res], writes=[r_ee[eb]])
                for s in range(2):
                    g = 2 * a + s
                    k = s
                    c0 = 2 * (hp * 2 + s)
                    P.op("dve", lambda e, eb=eb, s=s, k=k, u0=u0, c0=c0: e.scalar_tensor_tensor(em[k][:, 0:255], ee[eb][:, s, 0:255], 1.0, cmast[:, u0:u0 + 255], ALU.mult, ALU.mult,
                                                                                                accum_out=sm[:, c0:c0 + 1]),
                         reads=[r_ee[eb], r_w], writes=[r_em[k], r_sm])
                    P.op("dve", lambda e, c0=c0: e.tensor_scalar(sm[:, c0 + 1:c0 + 2], sm[:, c0:c0 + 1], 1e-30, None, ALU.max), reads=[r_sm], writes=[r_sm])
                    P.op("dve", lambda e, c0=c0: e.reciprocal(sm[:, c0:c0 + 1], sm[:, c0 + 1:c0 + 2]), reads=[r_sm], writes=[r_sm])
                    if u == 0:
                        P.op("dve", lambda e, k=k, g=g, c0=c0: e.tensor_scalar(pcs[:, g, 0:255], em[k][:, 0:255], sm[:, c0:c0 + 1], None, ALU.mult),
                             reads=[r_em[k], r_sm], writes=[r_pcs])
                    else:
                        P.op("dve", lambda e, k=k, g=g, c0=c0: e.scalar_tensor_tensor(pcs[:, g, 0:255], em[k][:, 0:255], sm[:, c0:c0 + 1], pcs[:, g, 0:255], ALU.mult, ALU.add),
                             reads=[r_em[k], r_sm, r_pcs], writes=[r_pcs])

        def sel_top(qt):
            P.op("dve", lambda e: e.tensor_reduce(imp[:], pcs[:].rearrange("p g (j r) -> p g j r", r=4), AX.X, ALU.add), reads=[r_pcs], writes=[r_imp])
            sh = pcs[:, :, 0:252]
            shv = bass.AP(sh.tensor, sh.offset + 3, [list(sh.ap[0]), list(sh.ap[1]), [4, 63]])
            P.op("dve", lambda e, shv=shv: e.tensor_tensor(imp[:, :, 1:64], imp[:, :, 1:64], shv, ALU.add), reads=[r_pcs, r_imp], writes=[r_imp])
            P.op("dve", lambda e, qt=qt: e.tensor_tensor(imp[:], imp[:], fbc(fa[:, qt, :], 0, 4), ALU.add), reads=[r_imp, r_w], writes=[r_imp])
            for g in range(4):
                P.op("dve", lambda e, g=g: e.max(m8[:, g, 0:8], imp[:, g, :]), reads=[r_imp], writes=[r_m8])
                P.op("dve", lambda e, g=g: e.match_replace(imp3[:], m8[:, g, 0:8], imp[:, g, :], -1e30), reads=[r_imp, r_m8], writes=[r_imp3])
                P.op("dve", lambda e, g=g: e.max(m8[:, g, 8:16], imp3[:]), reads=[r_imp3], writes=[r_m8])
                P.op("dve", lambda e, g=g: e.tensor_scalar(m8[:, g, 0:1], m8[:, g, 15:16], -1.0, None, ALU.max), reads=[r_m8], writes=[r_m8])
                P.op("dve", lambda e, g=g: e.tensor_scalar(nsel[:, g // 2, (g % 2) * 64:(g % 2) * 64 + 64], imp[:, g, :], m8[:, g, 0:1], 1.0, ALU.is_ge, ALU.subtract),
                     reads=[r_imp, r_m8], writes=[r_nsel])
            bank = C.bank(4)
            pv = bank.ap.bitcast(BF16)
            for a2 in range(2):
                P.op("pe", lambda e, pv=pv, a2=a2: e.transpose(pv[:, a2 * 128:(a2 + 1) * 128], nsel[:, a2, :], C.identb[:]), reads=[r_nsel, C.r_ident], writes=[bank.res])
            P.op("act", lambda e, pv=pv: e.copy(nselT[:], pv[:, 0:256].rearrange("p (g t) -> p g t", g=2)), reads=[bank.res], writes=[r_nselT])

        OB = [C.bank(2), C.bank(3), C.bank(6), C.bank(7)]

        def branches_pair(qt, a):
            qb = qbuf(qt)
            ql = slice((qt % 4) * 128, (qt % 4 + 1) * 128)
            nvalid = min(255, 8 * qt + 7)
            work = []
            for br in range(3):
                if br == 0:
                    items = [("c", nt, 128 if nt == 0 else 127) for nt in range(2) if nt * 128 < nvalid]
                elif br == 1:
                    items = [("s", kt, 128) for kt in range(qt + 1)]
                else:
                    items = [("w", kt, 128) for kt in range(max(0, qt - 4), qt + 1)]
                for ii, it in enumerate(items):
                    work.append((br, ii, len(items)) + it)
            obanks = {}
            for br in range(3):
                k = cst["ob"] % 2
                cst["ob"] += 1
                obanks[br] = (OB[2 * k], OB[2 * k + 1])
            slots = {}

            def stage1(w):
                br, ii, nitems, kind, kt, nn = work[w]
                sbs, pis, mms = [], [], []
                for s in range(2):
                    pb = s * 64
                    g = 2 * a + s
                    qrh = qgB[qb][pb:pb + 64, a * 4:(a + 1) * 4, ql]
                    sb = SB[cst["sb"] % 4]
                    cst["sb"] += 1
                    pi = cst["pm"] % NPM
                    cst["pm"] += 1
                    sbs.append(sb)
                    pis.append(pi)
                    mm = []
                    if kind == "c":
                        mm.append((KCM[pb:pb + 64, a, kt * 128:kt * 128 + nn], qrh, [r_cm, r_qg[qb]]))
                        ci = NCMP_IDX.get((qt, kt))
                        if ci is not None:
                            mm.append((C.identb[0:nn, 0:nn], fbc(ncmp[0:nn, ci, :], 0, 4), [C.r_ident, r_w]))
                    elif kind == "s":
                        mm.append((KS_T[pb:pb + 64, a, kt * 128:(kt + 1) * 128], qrh, [r_K, r_qg[qb]]))
                        mm.append((esel[pb:pb + 64, kt, :], fbc(nselT[pb:pb + 64, a, :], 0, 4), [r_w, r_nselT]))
                        if kt == qt:
                            mm.append((C.identb[:], fbc(ntri[:, 0, :], 0, 4), [C.r_ident, r_w]))
                    else:
                        mm.append((KW_T[pb:pb + 64, a, kt * 128:(kt + 1) * 128], qrh, [r_K, r_qg[qb]]))
                        if kt == qt:
                            mm.append((C.identb[:], fbc(ntri[:, 0, :], 0, 4), [C.r_ident, r_w]))
                        if kt == qt - 4:
                            mm.append((C.identb[:], fbc(ntri[:, 1, :], 0, 4), [C.r_ident, r_w]))
                    mms.append(mm)
                slots[w] = pis
                nm = len(mms[0])
                for mi in range(nm):
                    for s in range(2):
                        l_, r_, rs_ = mms[s][mi]
                        sb = sbs[s]
                        P.op("pe", lambda e, sb=sb, nn=nn, l_=l_, r_=r_, mi=mi, last=(mi == nm - 1): e.matmul(
                            sb.ap[0:nn, :].rearrange("p (h t) -> p h t", h=4), l_, r_, start=(mi == 0), stop=last),
                            reads=rs_, writes=[sb.res])
                for s in range(2):
                    sb, pi = sbs[s], pis[s]
                    P.op("act", lambda e, sb=sb, nn=nn, pi=pi: e.activation(PM[pi][0:nn, :, :], sb.ap[0:nn, :].rearrange("p (h t) -> p h t", h=4), AF.Exp, scale=0.125),
                         reads=[sb.res], writes=[r_PM[pi]])

            def stage2(w):
                br, ii, nitems, kind, kt, nn = work[w]
                for s in range(2):
                    g = 2 * a + s
                    pi = slots[w][s]
                    ob = obanks[br][s]
                    oview = ob.ap[:, 0:260].rearrange("p (h c) -> p h c", h=4)
                    if kind == "c":
                        vsrc, vres = VCM[0:nn, kt, g, :], r_cm
                    elif kind == "s":
                        vsrc, vres = VS[:, kt, g, :], r_V
                    else:
                        vsrc, vres = VW[:, kt, g, :], r_V
                    for hh in range(4):
                        P.op("pe", lambda e, oview=oview, hh=hh, pi=pi, nn=nn, vsrc=vsrc, first=(ii == 0 and hh == 0), last=(ii == nitems - 1 and hh == 3): e.matmul(
                            oview[:, hh, :], PM[pi][0:nn, hh, :], vsrc, start=first, stop=last),
                            reads=[r_PM[pi], vres], writes=[ob.res])
                    if ii == nitems - 1:
                        fs = fac[:, s * 8:(s + 1) * 8]
                        rsum = oview[:, :, 64]
                        P.op("dve", lambda e, rsum=rsum, fs=fs: e.tensor_scalar(fs[:, 0:4], rsum, 1e-30, None, ALU.max), reads=[ob.res], writes=[r_fac])
                        P.op("dve", lambda e, fs=fs: e.reciprocal(fs[:, 4:8], fs[:, 0:4]), reads=[r_fac], writes=[r_fac])
                        gsl = GT[:, qt, g * 12:(g + 1) * 12]
                        gv = bass.AP(gsl.tensor, gsl.offset + br, [list(gsl.ap[0]), [3, 4]])
                        P.op("dve", lambda e, gv=gv, fs=fs: e.tensor_tensor(fs[:, 0:4], fs[:, 4:8], gv, ALU.mult), reads=[r_fac, r_G], writes=[r_fac])
                        if br == 0:
                            P.op("dve", lambda e, oview=oview, g=g, fs=fs: e.tensor_tensor(oacc[:, g * 4:(g + 1) * 4, :], oview[:, :, 0:64], fbc(fs[:, 0:4], 1, 64), ALU.mult),
                                 reads=[ob.res, r_fac], writes=[r_oacc])
                        else:
                            ot = otmp2[s]
                            P.op("dve", lambda e, oview=oview, fs=fs, ot=ot: e.tensor_tensor(ot[:], oview[:, :, 0:64], fbc(fs[:, 0:4], 1, 64), ALU.mult),
                                 reads=[ob.res, r_fac], writes=[r_otmp])
                            P.op("pool", lambda e, g=g, ot=ot: e.tensor_tensor(oacc[:, g * 4:(g + 1) * 4, :], oacc[:, g * 4:(g + 1) * 4, :], ot[:], ALU.add),
                                 reads=[r_otmp, r_oacc], writes=[r_oacc])

            LA = 1
            nw = len(work)
            for w in range(min(LA, nw)):
                stage1(w)
            for w in range(nw):
                if w + LA < nw:
                    stage1(w + LA)
                stage2(w)

        def outproj(qt):
            qs = slice(qt * 128, (qt + 1) * 128)
            P.dma("sp", xs[:], x_d[qs, :], writes=[r_xs])
            P.op("act", lambda e: e.copy(obf[:], oacc[:].rearrange("p h d -> p (h d)")), reads=[r_oacc], writes=[r_obf])
            bank = C.bank(4)
            pv = bank.ap.bitcast(BF16)
            for j in range(8):
                P.op("pe", lambda e, pv=pv, j=j: e.transpose(pv[:, j * 128:(j + 1) * 128], obf[:, j * 128:(j + 1) * 128], C.identb[:]), reads=[r_obf, C.r_ident], writes=[bank.res])
            P.op("act", lambda e, pv=pv: e.copy(OT[:], pv.rearrange("p (j t) -> p j t", j=8)), reads=[bank.res], writes=[r_OT])
            for half in range(2):
                bank = C.bank(4 + half)
                for j in range(8):
                    P.op("pe", lambda e, bank=bank, j=j, half=half: e.matmul(bank.ap[:, :], OT[:, j, :], Wo[:, j, half * 512:(half + 1) * 512], start=(j == 0), stop=(j == 7)),
                         reads=[r_OT, r_w], writes=[bank.res])
                hs = slice(half * 512, (half + 1) * 512)
                P.op("dve", lambda e, bank=bank, hs=hs: e.tensor_tensor(ys[:, hs], bank.ap[:, :], C.gate[:, hs], ALU.mult), reads=[bank.res, C.r_gate], writes=[r_ys])
                P.op("pool", lambda e, hs=hs: e.tensor_tensor(ys[:, hs], ys[:, hs], xs[:, hs], ALU.add), reads=[r_ys, r_xs], writes=[r_ys])
            tok = P.dma("sp", xout_d[qs, :], ys[:], reads=[r_ys], writes=[C.r_xout[qt]])
            C.final.append(tok)

        nq = NT
        if DBG["stop"] == "nsa_qt":
            nq = DBG.get("nqt", 3) + 1
        load_q(0)
        sel_scores(0, range(8))
        sel_top(0)
        for qt in range(nq):
            nx = qt + 1
            has = nx < nq
            if has and nx % 4 == 0:
                load_q(nx)
            for a in range(2):
                branches_pair(qt, a)
                if has:
                    sel_scores(nx, [4 * a, 4 * a + 1, 4 * a + 2, 4 * a + 3])
            outproj(qt)
            if has:
                sel_top(nx)
        P.barrier()


def build(stages, ncores=8):
    nc = bass.Bass("TRN2", target_bir_lowering=False)
    P = Prog(nc)
    C = Ctx()
    C.P, C.nc = P, nc
    Dm = {}
    C.D = Dm
    C.final = []

    def din(name, shape, dt=F32):
        Dm[name] = nc.dram_tensor(name, list(shape), dt, kind="ExternalInput").ap()
        return Dm[name]

    def dout(name, shape, dt=F32):
        Dm[name] = nc.dram_tensor(name, list(shape), dt, kind="ExternalOutput").ap()
        return Dm[name]

    def dtmp(name, shape, dt=F32):
        Dm[name] = nc.dram_tensor(name, list(shape), dt).ap()
        return Dm[name]

    C.fence_res = Res()
    C.pending_fence = []

    def fence(rl):
        P.barrier()
    C.fence = fence

    banks = [Bank(P.psum("bank%d" % i, [128, 512], F32)) for i in range(8)]
    C.bank = lambda i: banks[i]
    NSTG = 3
    stg = [P.sbuf("stg%d" % i, [128, 1024], F32) for i in range(NSTG)]
    r_stg = [Res() for _ in range(NSTG)]
    stg_i = [0]

    def wload(dst, src, dres):
        p, n = dst.shape[0], dst.shape[-1]
        i = stg_i[0] % NSTG
        stg_i[0] += 1
        P.dma("sp", stg[i][0:p, 0:n], src, writes=[r_stg[i]])
        P.op("pool", lambda e, i=i, p=p, n=n, dst=dst: e.tensor_copy(dst, stg[i][0:p, 0:n]), reads=[r_stg[i]], writes=[dres])
    C.wload = wload

    order = ["dsa", "ffn", "nsa", "moe"]
    xnames = {"dsa": ("x", "x1"), "ffn": ("x1", "x2"), "nsa": ("x2", "x3"), "moe": ("x3", "out")}
    first, last = stages[0], stages[-1]
    for sname in stages:
        a, b = xnames[sname]
        if a not in Dm:
            din(a, [T, D])
        if sname == last:
            dout(b, [T, D])
        else:
            dtmp(b, [T, D])
    din("csil_in", [128, 8])
    din("ada_w", [2, 2, 1024, 3072])
    din("ada_b", [2, 2, 3072])
    din("norm_mix", [2, 1024])
    din("norm_ffn", [2, 1024])
    din("ident_in", [128, 128])
    dtmp("hT", [8, 128, T], BF16)
    dtmp("yacc", [T, D])
    C.r_hT = [Res() for _ in range(8)]
    C.r_yacc = [Res() for _ in range(NT)]
    rx = {n: [Res() for _ in range(NT)] for n in ["x", "x1", "x2", "x3", "out"]}

    C.identb = P.sbuf("identb", [128, 128], BF16)
    C.r_ident = Res()
    P.dma("pool", C.identb[:], Dm["ident_in"], writes=[C.r_ident])
    csl = P.sbuf("csl", [128, 8], F32)
    C.csb = P.sbuf("csb", [128, 8, 128], F32)
    C.r_csb = Res()
    r_csl = Res()
    P.dma("sp", csl[:], Dm["csil_in"], writes=[r_csl])
    P.op("act", lambda e: e.activation(csl[:], csl[:], AF.Silu), reads=[r_csl], writes=[r_csl])
    P.op("dve", lambda e: e.tensor_copy(C.csb[:], fbc(csl[:], 1, 128)), reads=[r_csl], writes=[C.r_csb])
    C.epsc = P.sbuf("epsc", [128, 2], F32)
    C.r_eps = Res()
    P.op("pool", lambda e: e.memset(C.epsc[:], EPS), writes=[C.r_eps])
    C.gs = P.sbuf("gs", [128, 1024], F32)
    C.shift = P.sbuf("shift", [128, 1024], F32)
    C.gate = P.sbuf("gate", [128, 1024], F32)
    C.r_gs, C.r_shift, C.r_gate = Res(), Res(), Res()

    for sname in stages:
        a, b = xnames[sname]
        C.r_xin = rx[a]
        C.r_xout = rx[b]
        C.final = []
        if sname == "dsa":
            for nm, shp in [("dsa_w_in", [1024, 472]), ("dsa_w_o", [1024, 1024]), ("dsa_wuq_nope", [256, 768]), ("dsa_wuq_rope", [256, 256]),
                            ("dsa_wuq_rsw", [256, 256]), ("dsa_wiq", [256, 512]), ("dsa_wiq_sw", [256, 512]), ("dsa_wukT", [48, 16, 128]),
                            ("dsa_wuv2", [128, 16, 128]), ("dsa_g_q", [256]), ("dsa_g_kv", [128]), ("rope_ctm", [T, 16]), ("rope_stm", [T, 16]),
                            ("rope_cfm", [128, T]), ("rope_sfm", [128, T])]:
                if nm not in Dm:
                    din(nm, shp)
            mod_stage(C, 0, 0, Dm["norm_mix"][0])
            dsa_stage(C, Dm[a], Dm[b])
        elif sname == "nsa":
            for ent in [("nsa_w_o", [1024, 1024]), ("nsa_wk", [4, 1024, 256]), ("nsa_wk_sw", [3, 1024, 256]), ("nsa_wq", [1024, 1024]),
                            ("nsa_wq_sw", [1024, 1024]), ("nsa_wvg", [1024, 560]), ("nsa_w1k", [128, 32, 256]), ("nsa_w1v", [128, 32, 256]),
                            ("nsa_peT", [128, 32]), ("nsa_w2k2", [256, 2, 128]), ("nsa_w2v", [256, 64]), ("nsa_esel", [128, 32, 128], BF16),
                            ("nsa_ntri", [128, 2, 128], BF16), ("nsa_ncmp", [128, NCMP_N, 128], BF16), ("nsa_fa", [128, 32, 64]), ("nsa_cmast", [128, 512]),
                            ("rope_cfm", [128, T]), ("rope_sfm", [128, T])]:
                nm, shp = ent[0], ent[1]
                if nm not in Dm:
                    din(nm, shp, ent[2] if len(ent) > 2 else F32)
            dtmp("qT", [8, 128, T], BF16)
            mod_stage(C, 1, 0, Dm["norm_mix"][1])
            prenorm_stage(C, Dm[a], Dm["hT"])
            nsa_stage(C, Dm[a], Dm[b])
        elif sname == "ffn":
            din("ffn_w1", [1024, 2816])
            din("ffn_w3", [1024, 2816])
            din("ffn_w2", [2816, 1024])
            mod_stage(C, 0, 1, Dm["norm_ffn"][0])
            if DBG["stop"] == "mod":
                dout("dbg_mod", [3, 128, 1024])
                for i, t in enumerate([C.gs, C.shift, C.gate]):
                    C.final.append(P.dma("sp", Dm["dbg_mod"][i], t[:], reads=[C.r_gs, C.r_shift, C.r_gate], writes=[Res()]))
                break
            if DBG["stop"] == "prenorm":
                prenorm_stage(C, Dm[a], Dm["hT"])
                dout("dbg_hT", [8, 128, T], BF16)
                hsb = P.sbuf("dbg_hsb", [128, 8, 512], BF16)
                rr = Res()
                for tg in range(8):
                    P.dma("sp", hsb[:], Dm["hT"].rearrange("kc p t -> p kc t")[:, :, tg * 512:(tg + 1) * 512], reads=[C.r_hT[tg]], writes=[rr])
                    C.final.append(P.dma("sp", Dm["dbg_hT"].rearrange("kc p t -> p kc t")[:, :, tg * 512:(tg + 1) * 512], hsb[:], reads=[rr], writes=[Res()]))
                break
            passes = []
            for (f0, F) in [(0, 1024), (1024, 896), (1920, 896)]:
                passes.append((Dm["ffn_w1"][:, f0:f0 + F], Dm["ffn_w3"][:, f0:f0 + F], Dm["ffn_w2"][f0:f0 + F, :], None))
            ffn_passes(C, Dm["hT"], passes, Dm["yacc"], pn_args=(Dm[a], None))
            combine_stage(C, Dm[a], Dm["yacc"], Dm[b])
        elif sname == "moe":
            din("moe_router", [1024, 8])
            din("moe_w1", [8, 1024, 3584])
            din("moe_w3", [8, 1024, 3584])
            din("moe_w2", [8, 3584, 1024])
            din("final_norm", [1024])
            mod_stage(C, 1, 1, Dm["norm_ffn"][1])
            wrT = P.sbuf("moe_wrT", [128, 8, 8], F32)
            tokgate = P.sbuf("moe_tokgate", [128, 32, 8], F32)
            r_wr = Res()
            r_tgl = [Res() for _ in range(8)]
            P.dma("sp", wrT[:], Dm["moe_router"].rearrange("(kc p) e -> p kc e", p=128), writes=[r_wr])
            passes = []
            for ex in range(8):
                for qf in range(4):
                    f0 = qf * 896
                    passes.append((Dm["moe_w1"][ex, :, f0:f0 + 896], Dm["moe_w3"][ex, :, f0:f0 + 896],
                                   Dm["moe_w2"][ex, f0:f0 + 896, :], ex))
            ffn_passes(C, Dm["hT"], passes, Dm["yacc"], tokgate=(tokgate, r_tgl),
                       pn_args=(Dm[a], dict(wrT=(wrT, r_wr), tokgate=(tokgate, r_tgl))), FM=896)
            combine_stage(C, Dm[a], Dm["yacc"], Dm[b], final_g=Dm["final_norm"])
    P.finish(C.final)
    return nc


def host_prep(inputs, b, stages):
    m = {}
    m["csil_in"] = np.ascontiguousarray(inputs["c"][b].reshape(8, 128).T)
    m["ada_w"] = inputs["ada_w"]
    m["ada_b"] = inputs["ada_b"]
    m["norm_mix"] = inputs["norm_mix"]
    m["norm_ffn"] = inputs["norm_ffn"]
    m["ident_in"] = np.eye(128, dtype=np.float32)
    if "dsa" in stages or "nsa" in stages:
        inv = (500000.0 ** (-np.arange(0, 16, 2, dtype=np.float32) / 16)).astype(np.float32)
        ang = np.arange(T, dtype=np.float32)[:, None] * inv[None, :]
        co, si = np.cos(ang).astype(np.float32), np.sin(ang).astype(np.float32)
        m["rope_ctm"] = np.ascontiguousarray(np.concatenate([co, co], 1))
        m["rope_stm"] = np.ascontiguousarray(np.concatenate([-si, si], 1))
        cf = np.ones((64, T), np.float32)
        sf = np.zeros((64, T), np.float32)
        cf[0:8] = co.T
        cf[8:16] = co.T
        sf[0:8] = -si.T
        sf[8:16] = si.T
        m["rope_cfm"] = np.ascontiguousarray(np.concatenate([cf, cf], 0))
        m["rope_sfm"] = np.ascontiguousarray(np.concatenate([sf, sf], 0))
    if "dsa" in stages:
        m["dsa_w_in"] = inputs["dsa_w_in"][0]
        m["dsa_w_o"] = inputs["dsa_w_o"][0]
        wuq = inputs["dsa_w_uq"][0].reshape(256, 16, 64)
        m["dsa_wuq_nope"] = np.ascontiguousarray(wuq[:, :, 16:64].reshape(256, 768))
        m["dsa_wuq_rope"] = np.ascontiguousarray(wuq[:, :, 0:16].reshape(256, 256))
        m["dsa_wuq_rsw"] = np.ascontiguousarray(np.concatenate([wuq[:, :, 8:16], wuq[:, :, 0:8]], 2).reshape(256, 256))
        wiq = inputs["dsa_w_iq"][0].reshape(256, 8, 64)
        m["dsa_wiq"] = np.ascontiguousarray(wiq.reshape(256, 512))
        m["dsa_wiq_sw"] = np.ascontiguousarray(np.concatenate([wiq[:, :, 8:16], wiq[:, :, 0:8], wiq[:, :, 16:64]], 2).reshape(256, 512))
        m["dsa_wukT"] = np.ascontiguousarray(inputs["dsa_w_uk"][0].transpose(2, 0, 1))
        wuv2 = np.zeros((128, 16, 128), np.float32)
        for h in range(16):
            wuv2[:, h, (h % 2) * 64:(h % 2) * 64 + 64] = inputs["dsa_w_uv"][0][h]
        m["dsa_wuv2"] = wuv2
        m["dsa_g_q"] = inputs["dsa_g_q"][0]
        m["dsa_g_kv"] = inputs["dsa_g_kv"][0]
    if "nsa" in stages:
        w = inputs["nsa_w_in"][0]

        def sw(wc, nh):
            x = wc.reshape(1024, nh, 64)
            return np.concatenate([x[:, :, 8:16], x[:, :, 0:8], x[:, :, 16:64]], 2).reshape(1024, nh * 64)

        def blk(wc):
            x = wc.reshape(1024, 16, 64)
            cols = []
            for a_ in range(2):
                for u_ in range(4):
                    cols += [x[:, 8 * a_ + u_], x[:, 8 * a_ + 4 + u_]]
            return np.ascontiguousarray(np.concatenate(cols, 1))
        wq = w[:, 0:1024]
        m["nsa_wq"] = blk(wq)
        m["nsa_wq_sw"] = blk(sw(wq, 16))
        kc, vc, ks, vs, kw, vw = [w[:, 1024 + 256 * i_:1280 + 256 * i_] for i_ in range(6)]
        m["nsa_wk"] = np.ascontiguousarray(np.stack([kc, ks, kw, vc], 0))
        m["nsa_wk_sw"] = np.ascontiguousarray(np.stack([sw(kc, 4), sw(ks, 4), sw(kw, 4)], 0))
        m["nsa_wvg"] = np.ascontiguousarray(np.concatenate([vs, vw, w[:, 2560:2608]], 1))
        m["nsa_w_o"] = inputs["nsa_w_o"][0]
        for nm, src in [("nsa_w1k", "nsa_cmp_k1"), ("nsa_w1v", "nsa_cmp_v1")]:
            x = inputs[src][0].reshape(32, 64, 256).transpose(1, 0, 2)
            m[nm] = np.ascontiguousarray(np.concatenate([x, x], 0))
        pe = inputs["nsa_cmp_pe"][0].T
        m["nsa_peT"] = np.ascontiguousarray(np.concatenate([pe, pe], 0))
        w2k2 = np.zeros((256, 2, 128), np.float32)
        w2k2[:, 0, 0:64] = inputs["nsa_cmp_k2"][0]
        w2k2[:, 1, 64:128] = inputs["nsa_cmp_k2"][0]
        m["nsa_w2k2"] = w2k2
        m["nsa_w2v"] = inputs["nsa_cmp_v2"][0]
        m["nsa_esel"], m["nsa_ntri"], m["nsa_ncmp"] = [np.asarray(a_).astype(ml_dtypes.bfloat16) for a_ in _NC[0:3]]
        m["nsa_fa"], m["nsa_cmast"] = _NC[4], _NC[5]
    if "ffn" in stages:
        m["ffn_w1"] = inputs["ffn_w1"][0]
        m["ffn_w3"] = inputs["ffn_w3"][0]
        m["ffn_w2"] = inputs["ffn_w2"][0]
    if "moe" in stages:
        m["moe_router"] = inputs["moe_router"][0]
        m["moe_w1"] = inputs["moe_w1"][0]
        m["moe_w3"] = inputs["moe_w3"][0]
        m["moe_w2"] = inputs["moe_w2"][0]
        m["final_norm"] = inputs["final_norm"]
    return m


_CACHE = {}


def kernel(**inputs):
    inputs = {k: np.asarray(v) for k, v in inputs.items()}
    stages = ["dsa", "ffn", "nsa", "moe"]
    if "nc" not in _CACHE:
        _CACHE["nc"] = build(stages)
    nc = _CACHE["nc"]
    shared = host_prep(inputs, 0, stages)
    in_maps = []
    for b in range(8):
        m = dict(shared)
        m["csil_in"] = np.ascontiguousarray(inputs["c"][b].reshape(8, 128).T.astype(np.float32))
        m["x"] = np.ascontiguousarray(inputs["x"][b].astype(np.float32))
        in_maps.append(m)
    res = run_bass_kernel_spmd(nc, in_maps, core_ids=list(range(8)))
    out = np.stack([np.asarray(res.results[b]["out"]) for b in range(8)], 0).astype(np.float32)
    return out
```

```python
import numpy as np
import ml_dtypes
from contextlib import ExitStack
import concourse.bass as bass
import concourse.mybir as mybir
from concourse.bass_utils import run_bass_kernel_spmd

F32 = mybir.dt.float32
BF16 = mybir.dt.bfloat16
I32 = mybir.dt.int32
AF = mybir.ActivationFunctionType
ALU = mybir.AluOpType
AX = mybir.AxisListType

T = 4096
D = 1024
NT = 32
EPS = 1e-6
NEG = -30000.0


class Res:
    __slots__ = ("w", "r", "name")

    def __init__(self, name=""):
        self.w = {}
        self.r = []
        self.name = name


class Prog:
    CENG = ["pe", "act", "dve", "pool"]
    NPOOL = 16
    EPOCH = 30000

    def __init__(self, nc):
        self.nc = nc
        self.es = ExitStack()
        self.q = {e: [] for e in ["pe", "act", "dve", "pool", "sp"]}
        self.sems = {}
        self.cnt = {}
        self.seen = {e: {} for e in self.q}
        self.nsem = 0
        self.epoch = {e: 0 for e in self.CENG}
        self.ecount = {e: 0 for e in self.CENG}
        self.cur = {}
        for e in self.CENG:
            self._new_epoch(e)
        self.dpool = {}
        self.dpos = {}
        self.ninstr = 0
        self.pending = {e: [] for e in self.q}
        self.lasttok = {}

    def barrier(self):
        toks = list(self.lasttok.values())
        for k, v in self.cnt.items():
            if k[0] == "dma" and v > 0:
                toks.append((k, v))
        for e in self.q:
            self.pending[e].extend(toks)

    def _mksem(self, key):
        s = self.es.enter_context(self.nc.semaphore("s%d" % self.nsem))
        self.nsem += 1
        self.sems[key] = s
        self.cnt[key] = 0
        return s

    def _new_epoch(self, e):
        key = (e, self.epoch[e])
        self.epoch[e] += 1
        self._mksem(key)
        self.cur[e] = key
        self.ecount[e] = 0

    def sbuf(self, name, shape, dt, stack=None):
        self.nsem += 1
        return (stack or self.es).enter_context(self.nc.sbuf_tensor("%s_u%d" % (name, self.nsem), shape, dt))

    def psum(self, name, shape, dt):
        return self.es.enter_context(self.nc.psum_tensor(name, shape, dt))

    def _waits_for(self, eng, reads, writes):
        toks = self.pending[eng]
        self.pending[eng] = []
        for r in reads:
            toks.extend(r.w.items())
        for w in writes:
            toks.extend(w.w.items())
            toks.extend(w.r)
        need = {}
        seen = self.seen[eng]
        for (k, v) in toks:
            if eng == "pe" and k[0] == "pe":
                continue
            if seen.get(k, 0) >= v:
                continue
            if need.get(k, 0) < v:
                need[k] = v
        for k, v in need.items():
            seen[k] = v
        return list(need.items())

    def _commit(self, tok, reads, writes):
        for r in reads:
            r.r.append(tok)
            if len(r.r) > 16:
                d = {}
                for (k, v) in r.r:
                    if d.get(k, 0) < v:
                        d[k] = v
                r.r = list(d.items())
        for w in writes:
            if w.w.get(tok[0], 0) < tok[1]:
                w.w[tok[0]] = tok[1]
            w.r = []

    def op(self, eng, fn, reads=(), writes=()):
        if self.ecount[eng] >= self.EPOCH:
            self._new_epoch(eng)
        waits = self._waits_for(eng, reads, writes)
        key = self.cur[eng]
        self.cnt[key] += 1
        self.ecount[eng] += 1
        tok = (key, self.cnt[key])
        self.lasttok[eng] = tok
        self.q[eng].append((waits, fn, key, 1))
        self._commit(tok, reads, writes)
        self.ninstr += 1
        return tok

    def op_raw(self, eng, fn, inc, reads=(), writes=()):
        if eng not in self.dpool:
            self.dpool[eng] = []
            for i in range(self.NPOOL):
                self._mksem(("dma", eng, i))
                self.dpool[eng].append(("dma", eng, i))
            self.dpos[eng] = 0
        key = self.dpool[eng][self.dpos[eng] % self.NPOOL]
        self.dpos[eng] += 1
        waits = self._waits_for(eng, reads, writes)
        prev = self.cnt[key]
        if prev > 0 and self.seen[eng].get(key, 0) < prev:
            waits.append((key, prev))
            self.seen[eng][key] = prev
        self.cnt[key] += inc
        tok = (key, self.cnt[key])
        self.q[eng].append((waits, fn, key, inc))
        self._commit(tok, reads, writes)
        self.ninstr += 1
        return tok

    def dma(self, eng, out, in_, reads=(), writes=(), **kw):
        if eng not in self.dpool:
            self.dpool[eng] = []
            for i in range(self.NPOOL):
                self._mksem(("dma", eng, i))
                self.dpool[eng].append(("dma", eng, i))
            self.dpos[eng] = 0
        key = self.dpool[eng][self.dpos[eng] % self.NPOOL]
        self.dpos[eng] += 1
        waits = self._waits_for(eng, reads, writes)
        prev = self.cnt[key]
        if prev > 0 and self.seen[eng].get(key, 0) < prev:
            waits.append((key, prev))
            self.seen[eng][key] = prev
        self.cnt[key] += 16
        tok = (key, self.cnt[key])

        def fn(e, out=out, in_=in_, kw=kw):
            return e.dma_start(out=out, in_=in_, **kw)
        self.q[eng].append((waits, fn, key, 16))
        self._commit(tok, reads, writes)
        self.ninstr += 1
        return tok

    def finish(self, final_tokens):
        nc = self.nc
        sems = self.sems
        q = self.q
        fw = {}
        for (k, v) in final_tokens:
            if fw.get(k, 0) < v:
                fw[k] = v

        def emit(e, lst):
            for (waits, fn, key, inc) in lst:
                for (k, v) in waits:
                    e.wait_ge(sems[k], v)
                fn(e).then_inc(sems[key], inc)

        with nc.Block() as block:
            @block.tensor
            def _(e):
                emit(e, q["pe"])

            @block.scalar
            def _(e):
                emit(e, q["act"])

            @block.vector
            def _(e):
                emit(e, q["dve"])

            @block.gpsimd
            def _(e):
                emit(e, q["pool"])

            @block.sync
            def _(e):
                emit(e, q["sp"])
                for k, v in fw.items():
                    e.wait_ge(sems[k], v)
        self.es.close()


def fbc(ap, pos, n):
    l = [list(x) for x in ap.ap]
    l.insert(1 + pos, [0, n])
    return bass.AP(ap.tensor, ap.offset, l)


def _nsa_consts():
    n = np.arange(128)
    esel = np.zeros((128, 32, 128), np.float32)
    for kt in range(32):
        for s_ in range(2):
            esel[64 * s_ + 2 * kt, kt, 0:64] = 30000.0
            esel[64 * s_ + 2 * kt + 1, kt, 64:128] = 30000.0
    ntri = np.zeros((128, 2, 128), np.float32)
    ntri[:, 0, :] = np.where(n[:, None] > n[None, :], NEG, 0.0)
    ntri[:, 1, :] = np.where(n[:, None] <= n[None, :], NEG, 0.0)
    idx = {}
    tiles = []
    for qt in range(32):
        nvalid = min(255, 8 * qt + 7)
        for nt in range(2):
            if nt * 128 >= nvalid:
                continue
            nn = nt * 128 + n
            tq = qt * 128 + n
            mk = np.where(16 * nn[:, None] + 31 > tq[None, :], NEG, 0.0).astype(np.float32)
            if np.any(mk != 0):
                idx[(qt, nt)] = len(tiles)
                tiles.append(mk)
    ncmp = np.ascontiguousarray(np.stack(tiles, 1))
    fa = np.zeros((128, 32, 64), np.float32)
    j = np.arange(64)
    for qt in range(32):
        cur = 2 * qt + (n >= 64).astype(np.int64)
        forced = (j[None, :] == 0) | (j[None, :] == cur[:, None]) | (j[None, :] == cur[:, None] - 1)
        valid = j[None, :] <= cur[:, None]
        fa[:, qt, :] = np.where(valid, np.where(forced, 1e4, 0.0), -1e30)
    u = np.arange(512)
    cmast = (16 * (u[None, :] - 248) + 31 <= n[:, None]).astype(np.float32)
    return esel, ntri, ncmp, idx, fa, cmast


_NC = _nsa_consts()
NCMP_IDX = _NC[3]
NCMP_N = _NC[2].shape[1]


class Ctx:
    pass


DBG = {"stop": None}


def mod_stage(C, l, s, normg_d):
    P, nc, Dm = C.P, C.nc, C.D
    with ExitStack() as st:
        bb = P.sbuf("mod_bb", [128, 3072], F32, st)
        gb = P.sbuf("mod_gb", [128, 1024], F32, st)
        modt = P.sbuf("mod_t", [128, 2048], F32, st)
        wch = [P.sbuf("mod_w%d" % i, [128, 8, 512], F32, st) for i in range(2)]
        r_bb, r_gb, r_mod = Res(), Res(), Res()
        r_w = [Res(), Res()]
        P.dma("sp", bb[:], bass.AP(Dm["ada_b"].tensor, Dm["ada_b"][l, s, :].offset, [[0, 128], [1, 3072]]), writes=[r_bb])
        P.dma("sp", gb[:], bass.AP(normg_d.tensor, normg_d.offset, [[0, 128], [1, 1024]]), writes=[r_gb])
        wv = Dm["ada_w"][l, s].rearrange("(kc p) n -> p kc n", p=128)
        for n in range(6):
            w = wch[n % 2]
            P.dma("sp", w[:], wv[:, :, n * 512:(n + 1) * 512], writes=[r_w[n % 2]])
            bank = C.bank(6 + (n % 2))
            for kc in range(8):
                P.op("pe", lambda e, w=w, kc=kc, bank=bank: e.matmul(bank.ap[:, :], C.csb[:, kc, :], w[:, kc, :],
                                                                     start=(kc == 0), stop=(kc == 7)),
                     reads=[C.r_csb, r_w[n % 2]], writes=[bank.res])
            if n < 2:
                dst, dres = C.shift[:, n * 512:(n + 1) * 512], C.r_shift
            elif n < 4:
                dst, dres = modt[:, (n - 2) * 512:(n - 1) * 512], r_mod
            else:
                dst, dres = C.gate[:, (n - 4) * 512:(n - 3) * 512], C.r_gate
            P.op("dve", lambda e, dst=dst, bank=bank, n=n: e.tensor_tensor(dst, bank.ap[:, :], bb[:, n * 512:(n + 1) * 512], ALU.add),
                 reads=[bank.res, r_bb], writes=[dres])
        P.op("dve", lambda e: e.scalar_tensor_tensor(C.gs[:], modt[:, 0:1024], 1.0, gb[:], ALU.add, ALU.mult),
             reads=[r_mod, r_gb], writes=[C.r_gs])
        C.fence([r_bb, r_gb, r_mod] + r_w)


class Bank:
    def __init__(self, ap):
        self.ap = ap
        self.res = Res()


class Prenorm:
    def __init__(self, C, x_d, hT_d, st, router=None):
        P = C.P
        self.C, self.x_d, self.router = C, x_d, router
        self.hTv = hT_d.rearrange("kc p t -> p kc t")
        self.xt = P.sbuf("pn_x", [128, 1024], F32, st)
        self.sq = P.sbuf("pn_sq", [128, 1024], BF16, st)
        self.t1 = [P.sbuf("pn_t%d" % i, [128, 1024], F32, st) for i in range(4 if router else 2)]
        self.hb = [P.sbuf("pn_hb%d" % i, [128, 1024], BF16, st) for i in range(4)]
        self.hgp = P.sbuf("pn_hg", [128, 8, 512], BF16, st)
        self.ss = P.sbuf("pn_ss", [128, 64], F32, st)
        self.r_x, self.r_sq, self.r_hgp = Res(), Res(), Res()
        self.r_t1 = [Res() for _ in self.t1]
        self.r_hb = [Res() for _ in self.hb]
        self.r_ss = [Res() for _ in range(32)]
        if router:
            self.identf = P.sbuf("pn_idf", [128, 128], F32, st)
            self.r_idf = Res()
            P.dma("sp", self.identf[:], C.D["ident_in"], writes=[self.r_idf])
            self.hTf = P.sbuf("pn_hTf", [128, 8, 128], F32, st)
            self.r_hTf = Res()
            self.lg = P.sbuf("pn_lg", [128, 32, 8], F32, st)
            self.r_lg = [Res() for _ in range(8)]
            self.m1 = P.sbuf("pn_m1", [128, 4, 8], F32, st)
            self.l2 = P.sbuf("pn_l2", [128, 4, 8], F32, st)
            self.ex = P.sbuf("pn_ex", [128, 4, 8], F32, st)
            self.mx = P.sbuf("pn_mx", [128, 4, 4], F32, st)
            self.r_a = Res()

    def front(self, tg):
        C, P = self.C, self.C.P
        xt, sq, ss = self.xt, self.sq, self.ss
        for j in range(4):
            i = tg * 4 + j
            t1, r_t1 = (self.t1[j], self.r_t1[j]) if self.router else (self.t1[j % 2], self.r_t1[j % 2])
            hb, r_hb = self.hb[j], self.r_hb[j]
            P.dma("sp", xt[:], self.x_d[i * 128:(i + 1) * 128, :], reads=[C.r_xin[i]] if C.r_xin else [], writes=[self.r_x])
            P.op("act", lambda e, i=i: e.activation(sq[:], xt[:], AF.Square, accum_out=ss[:, 2 * i:2 * i + 1]),
                 reads=[self.r_x], writes=[self.r_sq, self.r_ss[i]])
            P.op("act", lambda e, i=i: e.activation(ss[:, 2 * i + 1:2 * i + 2], ss[:, 2 * i:2 * i + 1], AF.Sqrt, bias=C.epsc[:, 0:1], scale=1.0 / D),
                 reads=[self.r_ss[i], C.r_eps], writes=[self.r_ss[i]])
            P.op("dve", lambda e, i=i: e.reciprocal(ss[:, 2 * i:2 * i + 1], ss[:, 2 * i + 1:2 * i + 2]),
                 reads=[self.r_ss[i]], writes=[self.r_ss[i]])
            P.op("dve", lambda e, i=i, t1=t1: e.scalar_tensor_tensor(t1[:], xt[:], ss[:, 2 * i:2 * i + 1], C.gs[:], ALU.mult, ALU.mult),
                 reads=[self.r_x, self.r_ss[i], C.r_gs], writes=[r_t1])
            if not self.router:
                P.op("pool", lambda e, t1=t1, hb=hb: e.tensor_tensor(hb[:], t1[:], C.shift[:], ALU.add),
                     reads=[r_t1, C.r_shift], writes=[r_hb])
            else:
                P.op("pool", lambda e, t1=t1: e.tensor_tensor(t1[:], t1[:], C.shift[:], ALU.add),
                     reads=[r_t1, C.r_shift], writes=[r_t1])
                P.op("act", lambda e, t1=t1, hb=hb: e.copy(hb[:], t1[:]), reads=[r_t1], writes=[r_hb])

    def back(self, tg):
        C, P = self.C, self.C.P
        hgp = self.hgp
        for j in range(4):
            i = tg * 4 + j
            hb, r_hb = self.hb[j], self.r_hb[j]
            bank = C.bank(6)
            pv = bank.ap.bitcast(BF16)
            for kc in range(8):
                P.op("pe", lambda e, hb=hb, kc=kc, pv=pv: e.transpose(pv[:, kc * 128:(kc + 1) * 128], hb[:, kc * 128:(kc + 1) * 128], C.identb[:]),
                     reads=[r_hb, C.r_ident], writes=[bank.res])
            P.op("act", lambda e, j=j, pv=pv: e.copy(hgp[:, :, j * 128:(j + 1) * 128], pv.rearrange("p (kc t) -> p kc t", kc=8)),
                 reads=[bank.res], writes=[self.r_hgp])
            if self.router:
                t1, r_t1 = self.t1[j], self.r_t1[j]
                wrT, r_wr = self.router["wrT"]
                b7 = C.bank(7)
                for half in range(2):
                    for k in range(4):
                        kc = half * 4 + k
                        P.op("pe", lambda e, t1=t1, kc=kc, k=k, b7=b7: e.transpose(b7.ap[:, k * 128:(k + 1) * 128], t1[:, kc * 128:(kc + 1) * 128], self.identf[:]),
                             reads=[r_t1, self.r_idf], writes=[b7.res])
                    P.op("dve", lambda e, half=half, b7=b7: e.tensor_copy(self.hTf[:, half * 4:(half + 1) * 4, :], b7.ap[:, :].rearrange("p (k t) -> p k t", k=4)),
                         reads=[b7.res], writes=[self.r_hTf])
                for kc in range(8):
                    P.op("pe", lambda e, kc=kc, bank=bank: e.matmul(bank.ap[:, 0:8], self.hTf[:, kc, :], wrT[:, kc, :], start=(kc == 0), stop=(kc == 7)),
                         reads=[self.r_hTf, r_wr], writes=[bank.res])
                P.op("dve", lambda e, i=i, bank=bank: e.tensor_copy(self.lg[:, i, :], bank.ap[:, 0:8]), reads=[bank.res], writes=[self.r_lg[tg]])
        P.dma("sp", self.hTv[:, :, tg * 512:(tg + 1) * 512], hgp[:], reads=[self.r_hgp], writes=[C.r_hT[tg]])
        if self.router:
            tokgate, r_tgl = self.router["tokgate"]
            lg = self.lg[:, tg * 4:(tg + 1) * 4, :]
            m1, l2, ex, mx, r_a = self.m1, self.l2, self.ex, self.mx, self.r_a
            P.op("dve", lambda e, lg=lg: e.tensor_reduce(mx[:, :, 0], lg, AX.X, ALU.max), reads=[self.r_lg[tg]], writes=[r_a])
            P.op("dve", lambda e, lg=lg: e.tensor_tensor(m1[:], lg, fbc(mx[:, :, 0], 1, 8), ALU.is_equal), reads=[r_a, self.r_lg[tg]], writes=[r_a])
            P.op("dve", lambda e, lg=lg: e.scalar_tensor_tensor(l2[:], m1[:], -1e30, lg, ALU.mult, ALU.add), reads=[r_a], writes=[r_a])
            P.op("dve", lambda e: e.tensor_reduce(mx[:, :, 1], l2[:], AX.X, ALU.max), reads=[r_a], writes=[r_a])
            P.op("dve", lambda e, lg=lg: e.tensor_tensor(m1[:], lg, fbc(mx[:, :, 1], 1, 8), ALU.is_ge), reads=[r_a], writes=[r_a])
            P.op("dve", lambda e, lg=lg: e.tensor_tensor(l2[:], lg, fbc(mx[:, :, 0], 1, 8), ALU.subtract), reads=[r_a], writes=[r_a])
            P.op("act", lambda e: e.activation(ex[:], l2[:], AF.Exp), reads=[r_a], writes=[r_a])
            P.op("dve", lambda e: e.tensor_tensor(ex[:], ex[:], m1[:], ALU.mult), reads=[r_a], writes=[r_a])
            P.op("dve", lambda e: e.tensor_reduce(mx[:, :, 2], ex[:], AX.X, ALU.add), reads=[r_a], writes=[r_a])
            P.op("dve", lambda e: e.reciprocal(mx[:, :, 3], mx[:, :, 2]), reads=[r_a], writes=[r_a])
            P.op("dve", lambda e, tg=tg: e.tensor_tensor(tokgate[:, tg * 4:(tg + 1) * 4, :], ex[:], fbc(mx[:, :, 3], 1, 8), ALU.mult), reads=[r_a], writes=[r_tgl[tg]])


def prenorm_stage(C, x_d, hT_d, router=None):
    P = C.P
    with ExitStack() as st:
        xt = [P.sbuf("pn_x%d" % i, [128, 1024], F32, st) for i in range(2)]
        sq = P.sbuf("pn_sq", [128, 1024], F32, st)
        t1 = [P.sbuf("pn_t%d" % i, [128, 1024], F32, st) for i in range(2)]
        hb = [P.sbuf("pn_hb%d" % i, [128, 1024], BF16, st) for i in range(2)]
        hg = [P.sbuf("pn_hg%d" % i, [128, 8, 512], BF16, st) for i in range(2)]
        ss = P.sbuf("pn_ss", [128, 64], F32, st)
        r_x = [Res(), Res()]
        r_sq = Res()
        r_t1 = [Res(), Res()]
        r_hb = [Res(), Res()]
        r_hg = [Res(), Res()]
        r_ss = [Res() for _ in range(32)]
        if router is not None:
            wr, r_wr, tokgate, r_tg = router
            lg = P.sbuf("pn_lg", [128, 32, 8], F32, st)
            junk = P.sbuf("pn_junk", [128, 1024], F32, st)
            r_lg = [Res() for _ in range(32)]
            r_junk = Res()
        hTv = hT_d.rearrange("kc p t -> p kc t")
        for i in range(NT):
            b = i % 2
            g = (i // 4) % 2
            P.dma("sp", xt[b][:], x_d[i * 128:(i + 1) * 128, :], writes=[r_x[b]])
            P.op("act", lambda e, b=b, i=i: e.activation(sq[:], xt[b][:], AF.Square, accum_out=ss[:, 2 * i:2 * i + 1]),
                 reads=[r_x[b]], writes=[r_sq, r_ss[i]])
            P.op("act", lambda e, i=i: e.activation(ss[:, 2 * i + 1:2 * i + 2], ss[:, 2 * i:2 * i + 1], AF.Sqrt, bias=C.epsc[:, 0:1], scale=1.0 / D),
                 reads=[r_ss[i], C.r_eps], writes=[r_ss[i]])
            P.op("dve", lambda e, i=i: e.reciprocal(ss[:, 2 * i:2 * i + 1], ss[:, 2 * i + 1:2 * i + 2]),
                 reads=[r_ss[i]], writes=[r_ss[i]])
            P.op("dve", lambda e, b=b, i=i: e.scalar_tensor_tensor(t1[b][:], xt[b][:], ss[:, 2 * i:2 * i + 1], C.gs[:], ALU.mult, ALU.mult),
                 reads=[r_x[b], r_ss[i], C.r_gs], writes=[r_t1[b]])
            if router is None:
                P.op("pool", lambda e, b=b: e.tensor_tensor(hb[b][:], t1[b][:], C.shift[:], ALU.add),
                     reads=[r_t1[b], C.r_shift], writes=[r_hb[b]])
            else:
                P.op("pool", lambda e, b=b: e.tensor_tensor(t1[b][:], t1[b][:], C.shift[:], ALU.add),
                     reads=[r_t1[b], C.r_shift], writes=[r_t1[b]])
                P.op("act", lambda e, b=b: e.copy(hb[b][:], t1[b][:]), reads=[r_t1[b]], writes=[r_hb[b]])
                for ex in range(8):
                    P.op("dve", lambda e, b=b, ex=ex, i=i: e.scalar_tensor_tensor(
                        junk[:], t1[b][:], 1.0, wr[:, ex, :], ALU.mult, ALU.mult, accum_out=lg[:, i, ex:ex + 1]),
                        reads=[r_t1[b], r_wr], writes=[r_junk, r_lg[i]])
            bank = C.bank(6 + (i % 2))
            pv = bank.ap.bitcast(BF16)
            for kc in range(8):
                P.op("pe", lambda e, b=b, kc=kc, pv=pv: e.transpose(pv[:, kc * 128:(kc + 1) * 128], hb[b][:, kc * 128:(kc + 1) * 128], C.identb[:]),
                     reads=[r_hb[b], C.r_ident], writes=[bank.res])
            j = i % 4
            P.op("act", lambda e, g=g, j=j, pv=pv: e.copy(hg[g][:, :, j * 128:(j + 1) * 128], pv.rearrange("p (kc t) -> p kc t", kc=8)),
                 reads=[bank.res], writes=[r_hg[g]])
            if j == 3:
                tg = i // 4
                P.dma("sp", hTv[:, :, tg * 512:(tg + 1) * 512], hg[g][:], reads=[r_hg[g]], writes=[C.r_hT[tg]])
        if router is not None:
            m1 = P.sbuf("pn_m1", [128, 32, 8], F32, st)
            l2 = P.sbuf("pn_l2", [128, 32, 8], F32, st)
            ex = P.sbuf("pn_ex", [128, 32, 8], F32, st)
            mx = P.sbuf("pn_mx", [128, 32, 4], F32, st)
            r_a = Res()
            allr = r_lg
            P.op("dve", lambda e: e.tensor_reduce(mx[:, :, 0], lg[:], AX.X, ALU.max), reads=allr, writes=[r_a])
            P.op("dve", lambda e: e.tensor_tensor(m1[:], lg[:], fbc(mx[:, :, 0], 1, 8), ALU.is_equal), reads=[r_a] + allr, writes=[r_a])
            P.op("dve", lambda e: e.scalar_tensor_tensor(l2[:], m1[:], -1e30, lg[:], ALU.mult, ALU.add), reads=[r_a], writes=[r_a])
            P.op("dve", lambda e: e.tensor_reduce(mx[:, :, 1], l2[:], AX.X, ALU.max), reads=[r_a], writes=[r_a])
            P.op("dve", lambda e: e.tensor_tensor(m1[:], lg[:], fbc(mx[:, :, 1], 1, 8), ALU.is_ge), reads=[r_a], writes=[r_a])
            P.op("dve", lambda e: e.tensor_tensor(l2[:], lg[:], fbc(mx[:, :, 0], 1, 8), ALU.subtract), reads=[r_a], writes=[r_a])
            P.op("act", lambda e: e.activation(ex[:], l2[:], AF.Exp), reads=[r_a], writes=[r_a])
            P.op("dve", lambda e: e.tensor_tensor(ex[:], ex[:], m1[:], ALU.mult), reads=[r_a], writes=[r_a])
            P.op("dve", lambda e: e.tensor_reduce(mx[:, :, 2], ex[:], AX.X, ALU.add), reads=[r_a], writes=[r_a])
            P.op("dve", lambda e: e.reciprocal(mx[:, :, 3], mx[:, :, 2]), reads=[r_a], writes=[r_a])
            P.op("dve", lambda e: e.tensor_tensor(tokgate[:], ex[:], fbc(mx[:, :, 3], 1, 8), ALU.mult), reads=[r_a], writes=[r_tg])
            C.fence([r_a, r_junk] + r_lg)
        C.fence(r_x + [r_sq] + r_t1 + r_hb + r_hg + r_ss)


def ffn_passes(C, hT_d, passes, yacc_d, tokgate=None, pn_args=None, FM=1024):
    P = C.P
    with ExitStack() as st:
        pn = Prenorm(C, pn_args[0], hT_d, st, router=pn_args[1]) if pn_args is not None else None
        W1 = [P.sbuf("ff_w1_%d" % i, [128, 8, FM], BF16, st) for i in range(2)]
        W3 = [P.sbuf("ff_w3_%d" % i, [128, 8, FM], BF16, st) for i in range(2)]
        W2 = [P.sbuf("ff_w2_%d" % i, [128, FM // 128, 1024], BF16, st) for i in range(2)]
        hg = [P.sbuf("ff_hg%d" % i, [128, 8, 512], BF16, st) for i in range(2)]
        gT = [P.sbuf("ff_gT%d" % i, [128, FM // 128, 512], BF16, st) for i in range(2)]
        sS = [P.sbuf("ff_s%d" % i, [128, 512], F32, st) for i in range(2)]
        yS = [P.sbuf("ff_y%d" % i, [128, 1024], F32, st) for i in range(2)]
        r_W = [[Res(), Res(), Res()] for _ in range(2)]
        r_hg = [Res(), Res()]
        r_gT = [Res(), Res()]
        r_s = [Res(), Res()]
        r_y = [Res(), Res()]
        hTv = hT_d.rearrange("kc p t -> p kc t")
        def w_tasks(pi):
            w1a, w3a, w2a, ex = passes[pi]
            F = w1a.shape[1]
            s = pi % 2
            tasks = []
            for kc in range(8):
                tasks.append((W1[s][:, kc, 0:F], w1a[kc * 128:(kc + 1) * 128, :], r_W[s][0]))
            for kc in range(8):
                tasks.append((W3[s][:, kc, 0:F], w3a[kc * 128:(kc + 1) * 128, :], r_W[s][1]))
            for fc in range(F // 128):
                tasks.append((W2[s][:, fc, :], w2a[fc * 128:(fc + 1) * 128, :], r_W[s][2]))
            return tasks

        for t_ in w_tasks(0):
            C.wload(*t_)
        state = {}
        cn = {"cnt": 0, "ycnt": 0, "gcnt": 0}

        def p1(pi, tg):
            w1a, w3a, w2a, ex = passes[pi]
            nf = w1a.shape[1] // 128
            s = pi % 2
            hb = cn["gcnt"] % 2
            cn["gcnt"] += 1
            state[(pi, tg)] = hb
            P.dma("sp", hg[hb][:], hTv[:, :, tg * 512:(tg + 1) * 512], reads=[C.r_hT[tg]], writes=[r_hg[hb]])
            if tg >= 1 and pi + 1 < len(passes):
                for t_ in w_tasks(pi + 1)[(tg - 1) * 4:tg * 4]:
                    C.wload(*t_)
            for fc in range(nf):
                k = cn["cnt"] % 2
                cn["cnt"] += 1
                ba, bb_ = C.bank(k), C.bank(2 + k)
                for kc in range(8):
                    P.op("pe", lambda e, ba=ba, s=s, kc=kc, fc=fc, hb=hb: e.matmul(
                        ba.ap[:, :], W1[s][:, kc, fc * 128:(fc + 1) * 128], hg[hb][:, kc, :], start=(kc == 0), stop=(kc == 7)),
                        reads=[r_W[s][0], r_hg[hb]], writes=[ba.res])
                for kc in range(8):
                    P.op("pe", lambda e, bb_=bb_, s=s, kc=kc, fc=fc, hb=hb: e.matmul(
                        bb_.ap[:, :], W3[s][:, kc, fc * 128:(fc + 1) * 128], hg[hb][:, kc, :], start=(kc == 0), stop=(kc == 7)),
                        reads=[r_W[s][1], r_hg[hb]], writes=[bb_.res])
                P.op("act", lambda e, k=k, ba=ba: e.activation(sS[k][:], ba.ap[:, :], AF.Silu), reads=[ba.res], writes=[r_s[k]])
                P.op("dve", lambda e, k=k, bb_=bb_, hb=hb, fc=fc: e.tensor_tensor(gT[hb][:, fc, :], sS[k][:], bb_.ap[:, :], ALU.mult),
                     reads=[r_s[k], bb_.res], writes=[r_gT[hb]])

        def p2(pi, tg):
            w1a, w3a, w2a, ex = passes[pi]
            nf = w1a.shape[1] // 128
            s = pi % 2
            hb = state[(pi, tg)]
            for j in range(4):
                tile = tg * 4 + j
                yb = cn["ycnt"] % 2
                cn["ycnt"] += 1
                for half in range(2):
                    bo = C.bank(4 + half)
                    for fc in range(nf):
                        P.op("pe", lambda e, bo=bo, hb=hb, fc=fc, j=j, s=s, half=half, nf=nf: e.matmul(
                            bo.ap[:, :], gT[hb][:, fc, j * 128:(j + 1) * 128], W2[s][:, fc, half * 512:(half + 1) * 512],
                            start=(fc == 0), stop=(fc == nf - 1)),
                            reads=[r_gT[hb], r_W[s][2]], writes=[bo.res])
                    if ex is None:
                        P.op("act", lambda e, yb=yb, bo=bo, half=half: e.copy(yS[yb][:, half * 512:(half + 1) * 512], bo.ap[:, :]),
                             reads=[bo.res], writes=[r_y[yb]])
                    else:
                        P.op("act", lambda e, yb=yb, bo=bo, half=half, tile=tile, ex=ex: e.activation(
                            yS[yb][:, half * 512:(half + 1) * 512], bo.ap[:, :], AF.Copy, scale=tokgate[0][:, tile, ex:ex + 1]),
                            reads=[bo.res, tokgate[1][tile // 4]], writes=[r_y[yb]])
                if pi == 0:
                    P.dma("sp", yacc_d[tile * 128:(tile + 1) * 128, :], yS[yb][:], reads=[r_y[yb]], writes=[C.r_yacc[tile]])
                else:
                    P.dma("pool", yacc_d[tile * 128:(tile + 1) * 128, :], yS[yb][:], reads=[r_y[yb]], writes=[C.r_yacc[tile]],
                          accum_op=ALU.add)

        seq = [(pi, tg) for pi in range(len(passes)) for tg in range(8)]
        if pn is not None:
            pn.front(0)
            pn.back(0)
            pn.front(1)
        p1(*seq[0])
        if pn is not None:
            pn.back(1)
        for k, cur in enumerate(seq):
            nx = seq[k + 1] if k + 1 < len(seq) else None
            pnx = pn is not None and nx is not None and nx[0] == 0 and nx[1] + 1 < 8
            if pnx:
                pn.front(nx[1] + 1)
            if nx is not None:
                p1(*nx)
            p2(*cur)
            if pnx:
                pn.back(nx[1] + 1)
        fl = r_hg + r_gT + r_s + r_y
        for a in r_W:
            fl += a
        C.fence(fl)


def combine_stage(C, xin_d, yacc_d, xout_d, final_g=None):
    P = C.P
    with ExitStack() as st:
        xt = [P.sbuf("cb_x%d" % i, [128, 1024], F32, st) for i in range(2)]
        yt = [P.sbuf("cb_y%d" % i, [128, 1024], F32, st) for i in range(2)]
        r_x = [Res(), Res()]
        r_y = [Res(), Res()]
        if final_g is not None:
            fg = P.sbuf("cb_fg", [128, 1024], F32, st)
            sq = P.sbuf("cb_sq", [128, 1024], F32, st)
            ss = P.sbuf("cb_ss", [128, 64], F32, st)
            r_fg, r_sq = Res(), Res()
            r_ss = [Res() for _ in range(32)]
            P.dma("sp", fg[:], bass.AP(final_g.tensor, final_g.offset, [[0, 128], [1, 1024]]), writes=[r_fg])
        for i in range(NT):
            b = i % 2
            P.dma("sp", xt[b][:], xin_d[i * 128:(i + 1) * 128, :], reads=[C.r_xin[i]] if C.r_xin else [], writes=[r_x[b]])
            P.dma("sp", yt[b][:], yacc_d[i * 128:(i + 1) * 128, :], reads=[C.r_yacc[i]], writes=[r_y[b]])
            P.op("pool", lambda e, b=b: e.tensor_tensor(yt[b][:], yt[b][:], C.gate[:], ALU.mult), reads=[r_y[b], C.r_gate], writes=[r_y[b]])
            P.op("dve", lambda e, b=b: e.tensor_tensor(xt[b][:], xt[b][:], yt[b][:], ALU.add), reads=[r_y[b], r_x[b]], writes=[r_x[b]])
            if final_g is not None:
                P.op("act", lambda e, b=b, i=i: e.activation(sq[:], xt[b][:], AF.Square, accum_out=ss[:, 2 * i:2 * i + 1]),
                     reads=[r_x[b]], writes=[r_sq, r_ss[i]])
                P.op("act", lambda e, i=i: e.activation(ss[:, 2 * i + 1:2 * i + 2], ss[:, 2 * i:2 * i + 1], AF.Sqrt, bias=C.epsc[:, 0:1], scale=1.0 / D),
                     reads=[r_ss[i], C.r_eps], writes=[r_ss[i]])
                P.op("dve", lambda e, i=i: e.reciprocal(ss[:, 2 * i:2 * i + 1], ss[:, 2 * i + 1:2 * i + 2]),
                     reads=[r_ss[i]], writes=[r_ss[i]])
                P.op("dve", lambda e, b=b, i=i: e.scalar_tensor_tensor(xt[b][:], xt[b][:], ss[:, 2 * i:2 * i + 1], fg[:], ALU.mult, ALU.mult),
                     reads=[r_x[b], r_ss[i], r_fg], writes=[r_x[b]])
            tok = P.dma("sp", xout_d[i * 128:(i + 1) * 128, :], xt[b][:], reads=[r_x[b]], writes=[C.r_xout[i]])
            C.final.append(tok)
        fl = r_x + r_y
        if final_g is not None:
            fl += [r_fg, r_sq] + r_ss
        C.fence(fl)


def dsa_stage(C, x_d, xout_d):
    P, Dm = C.P, C.D
    KSC = 0.125 / (8.0 ** 0.5)
    with ExitStack() as st:
        CKV_tm = P.sbuf("d_ckvtm", [128, 32, 128], BF16, st)
        CKV_T = P.sbuf("d_ckvT", [128, T], BF16, st)
        IK_T2 = P.sbuf("d_ikT2", [128, T], BF16, st)
        KR_T = P.sbuf("d_krT", [16, T], BF16, st)
        QLN_T = P.sbuf("d_qlnT", [128, 2, T], BF16, st)
        iwabs = P.sbuf("d_iwabs", [128, 32, 8], F32, st)
        iwsgn = P.sbuf("d_iwsgn", [128, 32, 8], F32, st)
        Win = P.sbuf("d_win", [128, 8, 472], BF16, st)
        Wnope = P.sbuf("d_wnope", [128, 2, 768], BF16, st)
        Wrope = P.sbuf("d_wrope", [128, 2, 256], BF16, st)
        Wrsw = P.sbuf("d_wrsw", [128, 2, 256], BF16, st)
        WukT = P.sbuf("d_wukT", [48, 16, 128], BF16, st)
        Wuv2 = P.sbuf("d_wuv2", [128, 16, 128], BF16, st)
        Wiq = P.sbuf("d_wiq", [128, 2, 512], BF16, st)
        Wiqs = P.sbuf("d_wiqs", [128, 2, 512], BF16, st)
        Wo = P.sbuf("d_wo", [128, 8, 1024], BF16, st)
        Ctm = P.sbuf("d_ctm", [128, 32, 16], F32, st)
        Stm = P.sbuf("d_stm", [128, 32, 16], F32, st)
        gq = P.sbuf("d_gq", [128, 256], F32, st)
        gkv = P.sbuf("d_gkv", [128, 128], F32, st)
        onesb = P.sbuf("d_ones", [128, 128], BF16, st)
        r_w = Res()
        r_K = Res()
        r_iw = Res()
        P.op("pool", lambda e: e.memset(onesb[:], 1.0), writes=[r_w])
        for kc in range(8):
            C.wload(Win[:, kc, :], Dm["dsa_w_in"][kc * 128:(kc + 1) * 128, :], r_w)
            C.wload(Wo[:, kc, :], Dm["dsa_w_o"][kc * 128:(kc + 1) * 128, :], r_w)
        for k2 in range(2):
            C.wload(Wnope[:, k2, :], Dm["dsa_wuq_nope"][k2 * 128:(k2 + 1) * 128, :], r_w)
            C.wload(Wrope[:, k2, :], Dm["dsa_wuq_rope"][k2 * 128:(k2 + 1) * 128, :], r_w)
            C.wload(Wrsw[:, k2, :], Dm["dsa_wuq_rsw"][k2 * 128:(k2 + 1) * 128, :], r_w)
            C.wload(Wiq[:, k2, :], Dm["dsa_wiq"][k2 * 128:(k2 + 1) * 128, :], r_w)
            C.wload(Wiqs[:, k2, :], Dm["dsa_wiq_sw"][k2 * 128:(k2 + 1) * 128, :], r_w)
        P.dma("pool", WukT[:], Dm["dsa_wukT"], writes=[r_w])
        P.dma("pool", Wuv2[:], Dm["dsa_wuv2"], writes=[r_w])
        P.dma("sp", Ctm[:], Dm["rope_ctm"].rearrange("(i p) r -> p i r", p=128), writes=[r_w])
        P.dma("sp", Stm[:], Dm["rope_stm"].rearrange("(i p) r -> p i r", p=128), writes=[r_w])
        P.dma("sp", gq[:], bass.AP(Dm["dsa_g_q"].tensor, 0, [[0, 128], [1, 256]]), writes=[r_w])
        P.dma("sp", gkv[:], bass.AP(Dm["dsa_g_kv"].tensor, 0, [[0, 128], [1, 128]]), writes=[r_w])

        with ExitStack() as sa:
            hg = [P.sbuf("da_hg%d" % i, [128, 8, 512], BF16, sa) for i in range(2)]
            pj = [P.sbuf("da_pj%d" % i, [128, 472], F32, sa) for i in range(2)]
            jk = P.sbuf("da_jk", [128, 256], F32, sa)
            stt = P.sbuf("da_st", [128, 32, 4], F32, sa)
            qln = [P.sbuf("da_qln%d" % i, [128, 256], BF16, sa) for i in range(2)]
            ik2 = [P.sbuf("da_ik2%d" % i, [128, 128], BF16, sa) for i in range(2)]
            krb = [P.sbuf("da_kr%d" % i, [128, 16], BF16, sa) for i in range(2)]
            tr = [P.sbuf("da_tr%d" % i, [128, 4, 16], F32, sa) for i in range(2)]
            r_hg = [Res(), Res()]
            r_pj = [Res(), Res()]
            r_jk = Res()
            r_st = [Res() for _ in range(32)]
            r_q = [Res(), Res()]
            r_ik = [Res(), Res()]
            r_kr = [Res(), Res()]
            r_tr = [Res(), Res()]
            hTv = Dm["hT"].rearrange("kc p t -> p kc t")
            pn = Prenorm(C, x_d, Dm["hT"], sa)
            pn.front(0)
            pn.back(0)
            for i in range(NT):
                b = i % 2
                tg, j = i // 4, i % 4
                g = tg % 2
                if j == 0:
                    if tg + 1 < 8:
                        pn.front(tg + 1)
                    P.dma("sp", hg[g][:], hTv[:, :, tg * 512:(tg + 1) * 512], reads=[C.r_hT[tg]], writes=[r_hg[g]])
                bank = C.bank(6 + b)
                for kc in range(8):
                    P.op("pe", lambda e, bank=bank, g=g, kc=kc, j=j: e.matmul(bank.ap[:, 0:472], hg[g][:, kc, j * 128:(j + 1) * 128], Win[:, kc, :],
                                                                           start=(kc == 0), stop=(kc == 7)),
                         reads=[r_hg[g], r_w], writes=[bank.res])
                P.op("act", lambda e, b=b, bank=bank: e.copy(pj[b][:], bank.ap[:, 0:472]), reads=[bank.res], writes=[r_pj[b]])
                P.op("act", lambda e, b=b, i=i: e.activation(jk[:, 0:256], pj[b][:, 0:256], AF.Square, accum_out=stt[:, i, 0:1]),
                     reads=[r_pj[b]], writes=[r_jk, r_st[i]])
                P.op("act", lambda e, b=b, i=i: e.activation(jk[:, 0:128], pj[b][:, 256:384], AF.Square, accum_out=stt[:, i, 1:2]),
                     reads=[r_pj[b]], writes=[r_jk, r_st[i]])
                P.op("act", lambda e, i=i: e.activation(stt[:, i, 2:3], stt[:, i, 0:1], AF.Sqrt, bias=C.epsc[:, 0:1], scale=1.0 / 256),
                     reads=[r_st[i], C.r_eps], writes=[r_st[i]])
                P.op("act", lambda e, i=i: e.activation(stt[:, i, 3:4], stt[:, i, 1:2], AF.Sqrt, bias=C.epsc[:, 0:1], scale=1.0 / 128),
                     reads=[r_st[i], C.r_eps], writes=[r_st[i]])
                P.op("dve", lambda e, i=i: e.reciprocal(stt[:, i, 0:2], stt[:, i, 2:4]), reads=[r_st[i]], writes=[r_st[i]])
                P.op("dve", lambda e, b=b, i=i: e.scalar_tensor_tensor(qln[b][:], pj[b][:, 0:256], stt[:, i, 0:1], gq[:], ALU.mult, ALU.mult),
                     reads=[r_pj[b], r_st[i], r_w], writes=[r_q[b]])
                P.op("dve", lambda e, b=b, i=i: e.scalar_tensor_tensor(CKV_tm[:, i, :], pj[b][:, 256:384], stt[:, i, 1:2], gkv[:], ALU.mult, ALU.mult),
                     reads=[r_pj[b], r_st[i], r_w], writes=[r_K])
                for (c0, which) in [(384, 0), (400, 1)]:
                    P.op("pool", lambda e, b=b, i=i, c0=c0, which=which: e.tensor_tensor(tr[b][:, 2 * which, :], pj[b][:, c0:c0 + 16], Ctm[:, i, :], ALU.mult),
                         reads=[r_pj[b], r_w], writes=[r_tr[b]])
                    P.op("pool", lambda e, b=b, i=i, c0=c0, which=which: e.tensor_tensor(tr[b][:, 2 * which + 1, 0:8], pj[b][:, c0 + 8:c0 + 16], Stm[:, i, 0:8], ALU.mult),
                         reads=[r_pj[b], r_w], writes=[r_tr[b]])
                    P.op("pool", lambda e, b=b, i=i, c0=c0, which=which: e.tensor_tensor(tr[b][:, 2 * which + 1, 8:16], pj[b][:, c0:c0 + 8], Stm[:, i, 8:16], ALU.mult),
                         reads=[r_pj[b], r_w], writes=[r_tr[b]])
                P.op("dve", lambda e, b=b: e.tensor_tensor(krb[b][:], tr[b][:, 0, :], tr[b][:, 1, :], ALU.add), reads=[r_tr[b]], writes=[r_kr[b]])
                P.op("dve", lambda e, b=b: e.tensor_tensor(ik2[b][:, 0:16], tr[b][:, 2, :], tr[b][:, 3, :], ALU.add), reads=[r_tr[b]], writes=[r_ik[b]])
                P.op("act", lambda e, b=b: e.copy(ik2[b][:, 16:64], pj[b][:, 416:464]), reads=[r_pj[b]], writes=[r_ik[b]])
                P.op("pool", lambda e, b=b: e.tensor_copy(ik2[b][:, 64:128], ik2[b][:, 0:64]), reads=[r_ik[b]], writes=[r_ik[b]])
                P.op("act", lambda e, b=b, i=i: e.activation(iwsgn[:, i, :], pj[b][:, 464:472], AF.Sign), reads=[r_pj[b]], writes=[r_iw])
                P.op("dve", lambda e, b=b, i=i: e.scalar_tensor_tensor(iwabs[:, i, :], pj[b][:, 464:472], KSC, iwsgn[:, i, :], ALU.mult, ALU.mult),
                     reads=[r_pj[b], r_iw], writes=[r_iw])
                bank2 = C.bank(4 + b)
                pv = bank2.ap.bitcast(BF16)
                srcs = [(qln[b][:, 0:128], r_q[b], 128), (qln[b][:, 128:256], r_q[b], 128), (CKV_tm[:, i, :], r_K, 128),
                        (ik2[b][:], r_ik[b], 128), (krb[b][:], r_kr[b], 16)]
                for k, (src, rs, n) in enumerate(srcs):
                    P.op("pe", lambda e, pv=pv, k=k, src=src, n=n: e.transpose(pv[0:n, k * 128:(k + 1) * 128], src, C.identb[:]),
                         reads=[rs, C.r_ident], writes=[bank2.res])
                cs = slice(i * 128, (i + 1) * 128)
                P.op("act", lambda e, pv=pv, cs=cs: e.copy(QLN_T[:, :, cs], pv[:, 0:256].rearrange("p (k t) -> p k t", k=2)), reads=[bank2.res], writes=[r_K])
                P.op("dve", lambda e, pv=pv, cs=cs: e.tensor_copy(CKV_T[:, cs], pv[:, 256:384]), reads=[bank2.res], writes=[r_K])
                P.op("act", lambda e, pv=pv, cs=cs: e.copy(IK_T2[:, cs], pv[:, 384:512]), reads=[bank2.res], writes=[r_K])
                P.op("dve", lambda e, pv=pv, cs=cs: e.tensor_copy(KR_T[:, cs], pv[0:16, 512:640]), reads=[bank2.res], writes=[r_K])
                if j == 3 and tg + 1 < 8:
                    pn.back(tg + 1)
            P.barrier()

        SC = P.sbuf("d_sc", [128, T], F32, st)
        MK = P.sbuf("d_mk", [128, T], BF16, st)
        MT = P.sbuf("d_mt", [128, 32, 128], BF16, st)
        Rr = [P.sbuf("d_r%d" % i, [128, 512], F32, st) for i in range(2)]
        NE = 4
        Eb = [P.sbuf("d_e%d" % i, [128, 512], BF16, st) for i in range(NE)]
        rec = P.sbuf("d_rec", [128, 512], F32, st)
        ON = P.sbuf("d_on", [128, 16, 128], BF16, st)
        QABS = [P.sbuf("d_qabs%d" % i, [128, 16, 128], BF16, st) for i in range(2)]
        QN = P.sbuf("d_qn", [48, 16, 128], BF16, st)
        QR = [P.sbuf("d_qr%d" % i, [16, 16, 128], BF16, st) for i in range(2)]
        IQ = P.sbuf("d_iq", [128, 4, 128], BF16, st)
        OV = P.sbuf("d_ov", [128, 8, 128], BF16, st)
        Cq = P.sbuf("d_cq", [128, 128], F32, st)
        Sq = P.sbuf("d_sq", [128, 128], F32, st)
        tq = [P.sbuf("d_tq%d" % i, [128, 512], F32, st) for i in range(2)]
        xs = P.sbuf("d_xs", [128, 1024], F32, st)
        ys = P.sbuf("d_ys", [128, 1024], F32, st)
        bs = P.sbuf("d_bs", [128, 8], F32, st)
        thrneg = P.sbuf("d_thrneg", [128, 1], F32, st)
        identN = P.sbuf("d_identN", [128, 128], BF16, st)
        r_SC, r_MK, r_MT, r_rec, r_ON, r_QN, r_IQ, r_OV, r_cs, r_xs, r_ys, r_bs = [Res() for _ in range(12)]
        r_QABS = [Res(), Res()]
        r_QR = [Res(), Res()]
        r_R = [Res(), Res()]
        r_E = [Res() for _ in range(NE)]
        r_tq = [Res(), Res()]
        r_thrneg = Res()
        r_idn = Res()
        P.op("pool", lambda e: e.memset(thrneg[:], -1e29), writes=[r_thrneg])
        P.op("act", lambda e: e.activation(identN[:], C.identb[:], AF.Copy, scale=30000.0), reads=[C.r_ident], writes=[r_idn])
        SB = [C.bank(0), C.bank(1), C.bank(4), C.bank(5)]
        st_ = {"ecnt": 0, "scnt": 0}

        def stage_b1(qt):
            qs = slice(qt * 128, (qt + 1) * 128)
            qp = qt % 2
            P.dma("sp", Cq[:], Dm["rope_cfm"][:, qs], writes=[r_cs])
            P.dma("sp", Sq[:], Dm["rope_sfm"][:, qs], writes=[r_cs])
            for hq in range(4):
                bank = C.bank(6 + (hq % 2))
                for hh in range(4):
                    h = hq * 4 + hh
                    for k2 in range(2):
                        P.op("pe", lambda e, bank=bank, hh=hh, h=h, k2=k2, qs=qs: e.matmul(
                            bank.ap[0:48, hh * 128:(hh + 1) * 128], Wnope[:, k2, h * 48:(h + 1) * 48], QLN_T[:, k2, qs], start=(k2 == 0), stop=(k2 == 1)),
                            reads=[r_w, r_K], writes=[bank.res])
                P.op("act", lambda e, bank=bank, hq=hq: e.copy(QN[:, hq * 4:(hq + 1) * 4, :], bank.ap[0:48, :].rearrange("p (h t) -> p h t", h=4)),
                     reads=[bank.res], writes=[r_QN])
            for hq in range(4):
                bank = C.bank(6 + (hq % 2))
                for hh in range(4):
                    h = hq * 4 + hh
                    P.op("pe", lambda e, bank=bank, hh=hh, h=h: e.matmul(bank.ap[:, hh * 128:(hh + 1) * 128], WukT[:, h, :], QN[:, h, :], start=True, stop=True),
                         reads=[r_w, r_QN], writes=[bank.res])
                P.op("act", lambda e, bank=bank, hq=hq, qp=qp: e.copy(QABS[qp][:, hq * 4:(hq + 1) * 4, :], bank.ap[:, :].rearrange("p (h t) -> p h t", h=4)),
                     reads=[bank.res], writes=[r_QABS[qp]])
            for hq in range(4):
                b0, b1 = C.bank(6), C.bank(7)
                for (bank, Wt) in [(b0, Wrope), (b1, Wrsw)]:
                    for hh in range(4):
                        h = hq * 4 + hh
                        for k2 in range(2):
                            P.op("pe", lambda e, bank=bank, Wt=Wt, hh=hh, h=h, k2=k2, qs=qs: e.matmul(
                                bank.ap[0:16, hh * 128:(hh + 1) * 128], Wt[:, k2, h * 16:(h + 1) * 16], QLN_T[:, k2, qs], start=(k2 == 0), stop=(k2 == 1)),
                                reads=[r_w, r_K], writes=[bank.res])
                P.op("dve", lambda e: e.tensor_tensor(tq[0][0:16, :].rearrange("p (h t) -> p h t", h=4), C.bank(6).ap[0:16, :].rearrange("p (h t) -> p h t", h=4),
                                                      fbc(Cq[0:16, :], 0, 4), ALU.mult), reads=[b0.res, r_cs], writes=[r_tq[0]])
                P.op("dve", lambda e: e.tensor_tensor(tq[1][0:16, :].rearrange("p (h t) -> p h t", h=4), C.bank(7).ap[0:16, :].rearrange("p (h t) -> p h t", h=4),
                                                      fbc(Sq[0:16, :], 0, 4), ALU.mult), reads=[b1.res, r_cs], writes=[r_tq[1]])
                P.op("pool", lambda e, hq=hq, qp=qp: e.tensor_tensor(QR[qp][:, hq * 4:(hq + 1) * 4, :], tq[0][0:16, :].rearrange("p (h t) -> p h t", h=4),
                                                                     tq[1][0:16, :].rearrange("p (h t) -> p h t", h=4), ALU.add),
                     reads=r_tq, writes=[r_QR[qp]])
            b0, b1 = C.bank(6), C.bank(7)
            for (bank, Wt) in [(b0, Wiq), (b1, Wiqs)]:
                for ch in range(4):
                    for k2 in range(2):
                        P.op("pe", lambda e, bank=bank, Wt=Wt, ch=ch, k2=k2, qs=qs: e.matmul(
                            bank.ap[:, ch * 128:(ch + 1) * 128], Wt[:, k2, ch * 128:(ch + 1) * 128], QLN_T[:, k2, qs], start=(k2 == 0), stop=(k2 == 1)),
                            reads=[r_w, r_K], writes=[bank.res])
            P.op("dve", lambda e: e.tensor_tensor(tq[0][:].rearrange("p (h t) -> p h t", h=4), C.bank(6).ap[:, :].rearrange("p (h t) -> p h t", h=4),
                                                  fbc(Cq[:], 0, 4), ALU.mult), reads=[b0.res, r_cs], writes=[r_tq[0]])
            P.op("dve", lambda e: e.tensor_tensor(tq[1][:].rearrange("p (h t) -> p h t", h=4), C.bank(7).ap[:, :].rearrange("p (h t) -> p h t", h=4),
                                                  fbc(Sq[:], 0, 4), ALU.mult), reads=[b1.res, r_cs], writes=[r_tq[1]])
            P.op("pool", lambda e: e.tensor_tensor(IQ[:].rearrange("p h t -> p (h t)"), tq[0][:], tq[1][:], ALU.add), reads=r_tq, writes=[r_IQ])

        def stage_b2(qt):
            qs = slice(qt * 128, (qt + 1) * 128)
            nk = (qt + 1) * 128
            ng = (nk + 511) // 512
            for kg in range(ng):
                ncol = min(512, nk - kg * 512)
                ks = slice(kg * 512, kg * 512 + ncol)
                for h in range(8):
                    bank = C.bank(6 + (h % 2))
                    rb = st_["ecnt"] % 2
                    st_["ecnt"] += 1
                    pb = (h % 2) * 64
                    P.op("pe", lambda e, bank=bank, pb=pb, h=h, ks=ks, ncol=ncol: e.matmul(
                        bank.ap[:, 0:ncol], IQ[pb:pb + 64, h // 2, :], IK_T2[pb:pb + 64, ks], start=True, stop=True),
                        reads=[r_IQ, r_K], writes=[bank.res])
                    P.op("act", lambda e, bank=bank, rb=rb, ncol=ncol, qt=qt, h=h: e.activation(
                        Rr[rb][:, 0:ncol], bank.ap[:, 0:ncol], AF.Relu, scale=iwabs[:, qt, h:h + 1]),
                        reads=[bank.res, r_iw], writes=[r_R[rb]])
                    if h == 0:
                        P.op("dve", lambda e, rb=rb, ncol=ncol, ks=ks, qt=qt, h=h: e.tensor_scalar(
                            SC[:, ks], Rr[rb][:, 0:ncol], iwsgn[:, qt, h:h + 1], None, ALU.mult),
                            reads=[r_R[rb], r_iw], writes=[r_SC])
                    else:
                        P.op("dve", lambda e, rb=rb, ncol=ncol, ks=ks, qt=qt, h=h: e.scalar_tensor_tensor(
                            SC[:, ks], Rr[rb][:, 0:ncol], iwsgn[:, qt, h:h + 1], SC[:, ks], ALU.mult, ALU.add),
                            reads=[r_R[rb], r_iw, r_SC], writes=[r_SC])
            P.op("pool", lambda e, qs=qs: e.affine_select(SC[:, qs], SC[:, qs], [[-1, 128]], ALU.is_ge, -1e30, base=0, channel_multiplier=1),
                 reads=[r_SC], writes=[r_SC])
            if qt >= 2:
                P.op("dve", lambda e, nk=nk: e.tensor_reduce(bs[:, 0:1], SC[:, 0:nk], AX.X, ALU.max), reads=[r_SC], writes=[r_bs])
                P.op("dve", lambda e, qt=qt: e.tensor_reduce(bs[:, 1:2], SC[:, 0:qt * 128], AX.X, ALU.min), reads=[r_SC], writes=[r_bs])
                P.op("dve", lambda e: e.tensor_tensor(bs[:, 2:3], bs[:, 0:1], bs[:, 1:2], ALU.subtract), reads=[r_bs], writes=[r_bs])

        def stage_b4(qt, k0, k1):
            nk = (qt + 1) * 128
            if qt < 2:
                return
            for k in range(k0, k1):
                f = 2.0 ** (-k)
                P.op("dve", lambda e, f=f: e.scalar_tensor_tensor(bs[:, 3:4], bs[:, 2:3], f, bs[:, 1:2], ALU.mult, ALU.add), reads=[r_bs], writes=[r_bs])
                P.op("dve", lambda e, nk=nk: e.tensor_scalar(MK[:, 0:nk], SC[:, 0:nk], bs[:, 3:4], 0.0, ALU.is_ge, ALU.add, accum_out=bs[:, 4:5]),
                     reads=[r_SC, r_bs], writes=[r_MK, r_bs])
                P.op("dve", lambda e: e.scalar_tensor_tensor(bs[:, 5:6], bs[:, 4:5], 255.5, bs[:, 2:3], ALU.is_ge, ALU.mult), reads=[r_bs], writes=[r_bs])
                P.op("dve", lambda e, f=f: e.scalar_tensor_tensor(bs[:, 1:2], bs[:, 5:6], f, bs[:, 1:2], ALU.mult, ALU.add), reads=[r_bs], writes=[r_bs])

        def stage_b5(qt):
            nk = (qt + 1) * 128
            if qt >= 2:
                thr_ap, thr_res = bs[:, 1:2], r_bs
            else:
                thr_ap, thr_res = thrneg[:, 0:1], r_thrneg
            P.op("dve", lambda e, nk=nk, thr_ap=thr_ap: e.tensor_scalar(MK[:, 0:nk], SC[:, 0:nk], thr_ap, 1.0, ALU.is_ge, ALU.subtract),
                 reads=[r_SC, thr_res], writes=[r_MK])
            for k4 in range((qt + 4) // 4):
                bank = C.bank(6 + (k4 % 2))
                pv = bank.ap.bitcast(BF16)
                nn = min(4, qt + 1 - k4 * 4)
                for kk in range(nn):
                    kt = k4 * 4 + kk
                    P.op("pe", lambda e, pv=pv, kk=kk, kt=kt: e.transpose(pv[:, kk * 128:(kk + 1) * 128], MK[:, kt * 128:(kt + 1) * 128], C.identb[:]),
                         reads=[r_MK, C.r_ident], writes=[bank.res])
                P.op("act", lambda e, pv=pv, k4=k4, nn=nn: e.copy(MT[:, k4 * 4:k4 * 4 + nn, :], pv[:, 0:nn * 128].rearrange("p (k t) -> p k t", k=nn)),
                     reads=[bank.res], writes=[r_MT])

        def stage_b6_cg(qt, cg):
            qp = qt % 2
            bo, br = C.bank(2 + 4 * (cg % 2)), C.bank(3 + 4 * (cg % 2))
            qa = QABS[qp][:, cg * 4:(cg + 1) * 4, :].rearrange("p h t -> p (h t)")
            qr = QR[qp][:, cg * 4:(cg + 1) * 4, :].rearrange("p h t -> p (h t)")
            LA = 3
            slots = {}

            def emit_s(kt):
                i = st_["scnt"]
                st_["scnt"] += 1
                sb = SB[i % 4]
                eb = i % NE
                slots[kt] = (sb, eb)
                kss = slice(kt * 128, (kt + 1) * 128)
                P.op("pe", lambda e, sb=sb, kss=kss, qa=qa: e.matmul(sb.ap[:, :], CKV_T[:, kss], qa, start=True, stop=False),
                     reads=[r_K, r_QABS[qp]], writes=[sb.res])
                P.op("pe", lambda e, sb=sb, kss=kss, qr=qr: e.matmul(sb.ap[:, :], KR_T[:, kss], qr, start=False, stop=False),
                     reads=[r_K, r_QR[qp]], writes=[sb.res])
                P.op("pe", lambda e, sb=sb, kt=kt: e.matmul(sb.ap[:, :].rearrange("p (h t) -> p h t", h=4), identN[:], fbc(MT[:, kt, :], 0, 4), start=False, stop=True),
                     reads=[r_idn, r_MT], writes=[sb.res])
                P.op("act", lambda e, sb=sb, eb=eb: e.activation(Eb[eb][:], sb.ap[:, :], AF.Exp, scale=0.125), reads=[sb.res], writes=[r_E[eb]])

            for kt in range(min(LA, qt + 1)):
                emit_s(kt)
            for kt in range(qt + 1):
                sb, eb = slots[kt]
                P.op("pe", lambda e, bo=bo, kt=kt, eb=eb, qt=qt: e.matmul(bo.ap[:, :], CKV_tm[:, kt, :], Eb[eb][:], start=(kt == 0), stop=(kt == qt)),
                     reads=[r_K, r_E[eb]], writes=[bo.res])
                P.op("pe", lambda e, br=br, kt=kt, eb=eb, qt=qt: e.matmul(br.ap[:, :], onesb[:], Eb[eb][:], start=(kt == 0), stop=(kt == qt)),
                     reads=[r_w, r_E[eb]], writes=[br.res])
                if kt + LA <= qt:
                    emit_s(kt + LA)
            P.op("dve", lambda e, br=br: e.reciprocal(rec[:], br.ap[:, :]), reads=[br.res], writes=[r_rec])
            P.op("dve", lambda e, bo=bo, cg=cg: e.tensor_tensor(ON[:, cg * 4:(cg + 1) * 4, :].rearrange("p h t -> p (h t)"), bo.ap[:, :], rec[:], ALU.mult),
                 reads=[bo.res, r_rec], writes=[r_ON])

        def stage_b7(qt):
            qs = slice(qt * 128, (qt + 1) * 128)
            P.dma("sp", xs[:], x_d[qs, :], writes=[r_xs])
            for hf in range(2):
                bank = C.bank(6 + hf)
                for jj in range(4):
                    j = hf * 4 + jj
                    for u in range(2):
                        h = 2 * j + u
                        P.op("pe", lambda e, bank=bank, jj=jj, h=h, u=u: e.matmul(bank.ap[:, jj * 128:(jj + 1) * 128], Wuv2[:, h, :], ON[:, h, :], start=(u == 0), stop=(u == 1)),
                             reads=[r_w, r_ON], writes=[bank.res])
                P.op("act", lambda e, bank=bank, hf=hf: e.copy(OV[:, hf * 4:(hf + 1) * 4, :], bank.ap[:, :].rearrange("p (j t) -> p j t", j=4)),
                     reads=[bank.res], writes=[r_OV])
            for half in range(2):
                bank = C.bank(6 + half)
                for j in range(8):
                    P.op("pe", lambda e, bank=bank, j=j, half=half: e.matmul(bank.ap[:, :], OV[:, j, :], Wo[:, j, half * 512:(half + 1) * 512], start=(j == 0), stop=(j == 7)),
                         reads=[r_OV, r_w], writes=[bank.res])
                hs = slice(half * 512, (half + 1) * 512)
                P.op("dve", lambda e, bank=bank, hs=hs: e.tensor_tensor(ys[:, hs], bank.ap[:, :], C.gate[:, hs], ALU.mult), reads=[bank.res, C.r_gate], writes=[r_ys])
                P.op("pool", lambda e, hs=hs: e.tensor_tensor(ys[:, hs], ys[:, hs], xs[:, hs], ALU.add), reads=[r_ys, r_xs], writes=[r_ys])
            tok = P.dma("sp", xout_d[qs, :], ys[:], reads=[r_ys], writes=[C.r_xout[qt]])
            C.final.append(tok)

        nq = NT
        if DBG["stop"] == "dsa_qt":
            nq = DBG.get("nqt", 3) + 1
        stage_b1(0)
        stage_b2(0)
        stage_b4(0, 1, 17)
        stage_b5(0)
        for qt in range(nq):
            nx = qt + 1
            has = nx < nq
            if has:
                stage_b1(nx)
                stage_b2(nx)
            for cg in range(4):
                stage_b6_cg(qt, cg)
                if has:
                    stage_b4(nx, 1 + 4 * cg, 5 + 4 * cg)
            stage_b7(qt)
            if has:
                stage_b5(nx)
        P.barrier()


def nsa_stage(C, x_d, xout_d):
    P, Dm = C.P, C.D
    with ExitStack() as st:
        KS_T = P.sbuf("n_ksT", [128, 2, T], BF16, st)
        KW_T = P.sbuf("n_kwT", [128, 2, T], BF16, st)
        VS = P.sbuf("n_vs", [128, 32, 4, 65], BF16, st)
        VW = P.sbuf("n_vw", [128, 32, 4, 65], BF16, st)
        KCM = P.sbuf("n_kcm", [128, 2, 256], BF16, st)
        VCM = P.sbuf("n_vcm", [128, 2, 4, 65], BF16, st)
        GT = P.sbuf("n_gt", [128, 32, 48], F32, st)
        r_w, r_K, r_V, r_G, r_cm = Res(), Res(), Res(), Res(), Res()
        P.op("pool", lambda e: e.memset(VS[:, :, :, 64:65], 1.0), writes=[r_V])
        P.op("pool", lambda e: e.memset(VW[:, :, :, 64:65], 1.0), writes=[r_V])
        P.op("pool", lambda e: e.memset(VCM[:], 0.0), writes=[r_cm])
        P.op("pool", lambda e: e.memset(VCM[:, :, :, 64:65], 1.0), writes=[r_cm])
        P.op("pool", lambda e: e.memset(KCM[:], 0.0), writes=[r_cm])
        hTv = Dm["hT"].rearrange("kc p t -> p kc t")
        qTv = Dm["qT"].rearrange("b p t -> p b t")
        r_qT = [Res() for _ in range(8)]

        with ExitStack() as sa:
            KC_T = P.sbuf("na_kcT", [128, 2, T], BF16, sa)
            VC_T = P.sbuf("na_vcT", [128, 2, T], BF16, sa)
            r_kc = Res()
            cnt = 0
            for ppass in range(2):
              with ExitStack() as sp_:
                if ppass == 0:
                    Wk = P.sbuf("na_wk", [128, 4, 8, 256], BF16, sp_)
                    Wks = P.sbuf("na_wks", [128, 3, 8, 256], BF16, sp_)
                    Wvg = P.sbuf("na_wvg", [128, 8, 560], BF16, sp_)
                else:
                    Wq = P.sbuf("na_wq", [128, 8, 1024], BF16, sp_)
                    Wqs = P.sbuf("na_wqs", [128, 8, 1024], BF16, sp_)
                    qg = P.sbuf("na_qg", [128, 8, 512], BF16, sp_)
                hg = [P.sbuf("na_hg%d_%d" % (i, ppass), [128, 8, 512], BF16, sp_) for i in range(2)]
                Cg = P.sbuf("na_cg%d" % ppass, [128, 512], F32, sp_)
                Sg = P.sbuf("na_sg%d" % ppass, [128, 512], F32, sp_)
                t0 = [P.sbuf("na_t0%d_%d" % (i, ppass), [128, 512], F32, sp_) for i in range(2)]
                t1 = [P.sbuf("na_t1%d_%d" % (i, ppass), [128, 512], F32, sp_) for i in range(2)]
                r_wa = Res()
                r_hg = [Res(), Res()]
                r_cs = Res()
                r_t0 = [Res(), Res()]
                r_t1 = [Res(), Res()]
                r_qg = Res()
                for kc in range(8):
                    if ppass == 0:
                        for t in range(4):
                            C.wload(Wk[:, t, kc, :], Dm["nsa_wk"][t, kc * 128:(kc + 1) * 128, :], r_wa)
                        for t in range(3):
                            C.wload(Wks[:, t, kc, :], Dm["nsa_wk_sw"][t, kc * 128:(kc + 1) * 128, :], r_wa)
                        C.wload(Wvg[:, kc, :], Dm["nsa_wvg"][kc * 128:(kc + 1) * 128, :], r_wa)
                    else:
                        C.wload(Wq[:, kc, :], Dm["nsa_wq"][kc * 128:(kc + 1) * 128, :], r_wa)
                        C.wload(Wqs[:, kc, :], Dm["nsa_wq_sw"][kc * 128:(kc + 1) * 128, :], r_wa)
                for tg in range(8):
                    g2 = tg % 2
                    ts = slice(tg * 512, (tg + 1) * 512)
                    P.dma("sp", hg[g2][:], hTv[:, :, ts], reads=[C.r_hT[tg]], writes=[r_hg[g2]])
                    P.dma("sp", Cg[:], Dm["rope_cfm"][:, ts], writes=[r_cs])
                    P.dma("sp", Sg[:], Dm["rope_sfm"][:, ts], writes=[r_cs])

                    hgt, r_hgt = hg[g2], r_hg[g2]

                    def proj_pair(wp, ws, dst, dres, rope, hgt=hgt, r_hgt=r_hgt, Cg=Cg, Sg=Sg, t0=t0, t1=t1, r_t0=r_t0, r_t1=r_t1, r_cs=r_cs, r_wa=r_wa):
                        nonlocal cnt
                        k = cnt % 2
                        cnt += 1
                        bp, bsw = C.bank(k), C.bank(2 + k)
                        for kc in range(8):
                            P.op("pe", lambda e, hgt=hgt, bp=bp, wp=wp, kc=kc: e.matmul(bp.ap[:, :], wp(kc), hgt[:, kc, :], start=(kc == 0), stop=(kc == 7)),
                                 reads=[r_wa, r_hgt], writes=[bp.res])
                        if not rope:
                            P.op("act", lambda e, bp=bp, dst=dst: e.copy(dst, bp.ap[:, :]), reads=[bp.res], writes=[dres])
                            return
                        for kc in range(8):
                            P.op("pe", lambda e, hgt=hgt, bsw=bsw, ws=ws, kc=kc: e.matmul(bsw.ap[:, :], ws(kc), hgt[:, kc, :], start=(kc == 0), stop=(kc == 7)),
                                 reads=[r_wa, r_hgt], writes=[bsw.res])
                        P.op("dve", lambda e, bp=bp, k=k: e.tensor_tensor(t0[k][:], bp.ap[:, :], Cg[:], ALU.mult), reads=[bp.res, r_cs], writes=[r_t0[k]])
                        P.op("dve", lambda e, bsw=bsw, k=k: e.tensor_tensor(t1[k][:], bsw.ap[:, :], Sg[:], ALU.mult), reads=[bsw.res, r_cs], writes=[r_t1[k]])
                        P.op("pool", lambda e, k=k, dst=dst: e.tensor_tensor(dst, t0[k][:], t1[k][:], ALU.add), reads=[r_t0[k], r_t1[k]], writes=[dres])

                    for a in (range(2) if ppass == 0 else []):
                        acs = slice(a * 128, (a + 1) * 128)
                        for t, dstT, dres in [(0, KC_T, r_kc), (1, KS_T, r_K), (2, KW_T, r_K)]:
                            proj_pair(lambda kc, t=t, acs=acs: Wk[:, t, kc, acs], lambda kc, t=t, acs=acs: Wks[:, t, kc, acs], dstT[:, a, ts], dres, True)
                        proj_pair(lambda kc, acs=acs: Wk[:, 3, kc, acs], None, VC_T[:, a, ts], r_kc, False)
                    for blk in (range(8) if ppass == 1 else []):
                        bcs = slice(blk * 128, (blk + 1) * 128)
                        proj_pair(lambda kc, bcs=bcs: Wq[:, kc, bcs], lambda kc, bcs=bcs: Wqs[:, kc, bcs], qg[:, blk, :], r_qg, True)
                    if ppass == 1:
                        P.dma("sp", qTv[:, :, ts], qg[:], reads=[r_qg], writes=[r_qT[tg]])
                    for j in (range(4) if ppass == 0 else []):
                        i = tg * 4 + j
                        b1, b2 = C.bank(4 + (j % 2)), C.bank(6 + (j % 2))
                        for kc in range(8):
                            P.op("pe", lambda e, hgt=hgt, b1=b1, kc=kc, j=j: e.matmul(b1.ap[:, :], hgt[:, kc, j * 128:(j + 1) * 128], Wvg[:, kc, 0:512], start=(kc == 0), stop=(kc == 7)),
                                 reads=[r_wa, r_hgt], writes=[b1.res])
                        for kc in range(8):
                            P.op("pe", lambda e, hgt=hgt, b2=b2, kc=kc, j=j: e.matmul(b2.ap[:, 0:48], hgt[:, kc, j * 128:(j + 1) * 128], Wvg[:, kc, 512:560], start=(kc == 0), stop=(kc == 7)),
                                 reads=[r_wa, r_hgt], writes=[b2.res])
                        P.op("act", lambda e, b1=b1, i=i: e.copy(VS[:, i, :, 0:64], b1.ap[:, 0:256].rearrange("p (g d) -> p g d", g=4)), reads=[b1.res], writes=[r_V])
                        P.op("dve", lambda e, b1=b1, i=i: e.tensor_copy(VW[:, i, :, 0:64], b1.ap[:, 256:512].rearrange("p (g d) -> p g d", g=4)), reads=[b1.res], writes=[r_V])
                        P.op("act", lambda e, b2=b2, i=i: e.activation(GT[:, i, :], b2.ap[:, 0:48], AF.Sigmoid), reads=[b2.res], writes=[r_G])
                P.barrier()
            if DBG["stop"] == "nsa_A":
                C.final.append(P.dma("sp", xout_d[0:128, :], x_d[0:128, :], writes=[Res()]))
                P.barrier()
                return
            W1s = [P.sbuf("na_w1s%d" % i, [128, 32, 256], BF16, sa) for i in range(2)]
            peT = P.sbuf("na_peT", [128, 32], BF16, sa)
            W2k2 = P.sbuf("na_w2k2", [128, 2, 2, 128], BF16, sa)
            W2v = P.sbuf("na_w2v", [128, 2, 64], BF16, sa)
            cb = P.sbuf("na_cb", [128, 4], F32, sa)
            hidT = [P.sbuf("na_hid%d" % i, [128, 2, 256], BF16, sa) for i in range(2)]
            r_cw, r_cb = Res(), Res()
            r_hid = [Res(), Res()]
            P.dma("pool", W1s[0][:], Dm["nsa_w1k"], writes=[r_cw])
            P.dma("pool", W1s[1][:], Dm["nsa_w1v"], writes=[r_cw])
            P.dma("pool", peT[:], Dm["nsa_peT"], writes=[r_cw])
            P.dma("pool", W2k2[:], Dm["nsa_w2k2"].rearrange("(c p) s m -> p c s m", p=128), writes=[r_cw])
            P.dma("pool", W2v[:], Dm["nsa_w2v"].rearrange("(c p) m -> p c m", p=128), writes=[r_cw])
            for kv in range(2):
                for ch in range(2):
                    bank = C.bank(6 + ch)
                    for l in range(32):
                        P.op("pe", lambda e, bank=bank, kv=kv, ch=ch, l=l: e.matmul(bank.ap[:, 0:1], W1s[kv][0:64, l, ch * 128:(ch + 1) * 128], peT[0:64, l:l + 1],
                                                                                    start=(l == 0), stop=(l == 31)),
                             reads=[r_cw], writes=[bank.res])
                    P.op("act", lambda e, bank=bank, kv=kv, ch=ch: e.copy(cb[:, kv * 2 + ch:kv * 2 + ch + 1], bank.ap[:, 0:1]), reads=[bank.res], writes=[r_cb])
            hc = 0
            for kv in range(2):
                srcT = KC_T if kv == 0 else VC_T
                for g in range(4):
                    a, s = g // 2, g % 2
                    pb = s * 64
                    hb = hc % 2
                    hc += 1
                    for ch in range(2):
                        bank = C.bank(4 + ch)
                        for l in range(32):
                            base = srcT[pb:pb + 64, a, 0:16]
                            rhs = bass.AP(base.tensor, base.offset + l, [list(base.ap[0]), [16, 255]])
                            P.op("pe", lambda e, bank=bank, kv=kv, ch=ch, l=l, pb=pb, rhs=rhs: e.matmul(
                                bank.ap[:, 0:255], W1s[kv][pb:pb + 64, l, ch * 128:(ch + 1) * 128], rhs, start=(l == 0), stop=(l == 31)),
                                reads=[r_cw, r_kc], writes=[bank.res])
                        P.op("act", lambda e, bank=bank, kv=kv, ch=ch, hb=hb: e.activation(hidT[hb][:, ch, 0:255], bank.ap[:, 0:255], AF.Silu,
                                                                                            bias=cb[:, kv * 2 + ch:kv * 2 + ch + 1]),
                             reads=[bank.res, r_cb], writes=[r_hid[hb]])
                    if kv == 0:
                        bank = C.bank(6)
                        for ch in range(2):
                            P.op("pe", lambda e, bank=bank, ch=ch, s=s, hb=hb: e.matmul(bank.ap[:, 0:255], W2k2[:, ch, s, :], hidT[hb][:, ch, 0:255], start=(ch == 0), stop=(ch == 1)),
                                 reads=[r_cw, r_hid[hb]], writes=[bank.res])
                        P.op("dve", lambda e, bank=bank, pb=pb, a=a: e.tensor_copy(KCM[pb:pb + 64, a, 0:255], bank.ap[pb:pb + 64, 0:255]), reads=[bank.res], writes=[r_cm])
                    else:
                        for nt in range(2):
                            nn = 128 if nt == 0 else 127
                            bank = C.bank(6 + nt)
                            for ch in range(2):
                                P.op("pe", lambda e, bank=bank, ch=ch, nt=nt, nn=nn, hb=hb: e.matmul(bank.ap[0:nn, 0:64], hidT[hb][:, ch, nt * 128:nt * 128 + nn], W2v[:, ch, :],
                                                                                                   start=(ch == 0), stop=(ch == 1)),
                                     reads=[r_cw, r_hid[hb]], writes=[bank.res])
                            P.op("dve", lambda e, bank=bank, nt=nt, nn=nn, g=g: e.tensor_copy(VCM[0:nn, nt, g, 0:64], bank.ap[0:nn, 0:64]), reads=[bank.res], writes=[r_cm])
            P.barrier()

        if DBG["stop"] == "nsa_C":
            C.final.append(P.dma("sp", xout_d[0:128, :], x_d[0:128, :], writes=[Res()]))
            P.barrier()
            return
        Wo = P.sbuf("n_wo", [128, 8, 1024], BF16, st)
        esel = P.sbuf("n_esel", [128, 32, 128], BF16, st)
        ntri = P.sbuf("n_ntri", [128, 2, 128], BF16, st)
        ncmp = P.sbuf("n_ncmp", [128, NCMP_N, 128], BF16, st)
        fa = P.sbuf("n_fa", [128, 32, 64], F32, st)
        cmast = P.sbuf("n_cmast", [128, 512], F32, st)
        for kc in range(8):
            C.wload(Wo[:, kc, :], Dm["nsa_w_o"][kc * 128:(kc + 1) * 128, :], r_w)
        P.dma("sp", esel[:], Dm["nsa_esel"], writes=[r_w])
        P.dma("sp", ntri[:], Dm["nsa_ntri"], writes=[r_w])
        P.dma("sp", ncmp[:], Dm["nsa_ncmp"], writes=[r_w])
        P.dma("sp", fa[:], Dm["nsa_fa"], writes=[r_w])
        P.dma("sp", cmast[:], Dm["nsa_cmast"], writes=[r_w])
        qgB = [P.sbuf("n_qg%d" % i, [128, 8, 512], BF16, st) for i in range(2)]
        NPM = 8
        PM = [P.sbuf("n_pm%d" % i, [128, 4, 128], BF16, st) for i in range(NPM)]
        ee = [P.sbuf("n_ee%d" % i, [128, 2, 256], F32, st) for i in range(2)]
        em = [P.sbuf("n_em%d" % i, [128, 256], F32, st) for i in range(2)]
        pcs = P.sbuf("n_pcs", [128, 4, 256], F32, st)
        imp = P.sbuf("n_imp", [128, 4, 64], F32, st)
        imp3 = P.sbuf("n_imp3", [128, 64], F32, st)
        m8 = P.sbuf("n_m8", [128, 4, 16], F32, st)
        sm = P.sbuf("n_sm", [128, 64], F32, st)
        nsel = P.sbuf("n_nsel", [128, 2, 128], BF16, st)
        nselT = P.sbuf("n_nselT", [128, 2, 128], BF16, st)
        oacc = P.sbuf("n_oacc", [128, 16, 64], F32, st)
        otmp = P.sbuf("n_otmp", [128, 4, 64], F32, st)
        fac = P.sbuf("n_fac", [128, 16], F32, st)
        otmp2 = [P.sbuf("n_otmp2_%d" % i, [128, 4, 64], F32, st) for i in range(2)]
        obf = P.sbuf("n_obf", [128, 1024], BF16, st)
        OT = P.sbuf("n_oT", [128, 8, 128], BF16, st)
        xs = P.sbuf("n_xs", [128, 1024], F32, st)
        ys = P.sbuf("n_ys", [128, 1024], F32, st)
        r_qg = [Res(), Res()]
        r_PM = [Res() for _ in range(8)]
        r_ee = [Res(), Res()]
        r_em = [Res(), Res()]
        r_pcs, r_imp, r_imp3, r_m8, r_sm, r_nsel, r_nselT, r_oacc, r_otmp, r_fac, r_obf, r_OT, r_xs, r_ys = [Res() for _ in range(14)]
        P.op("pool", lambda e: e.memset(pcs[:], 0.0), writes=[r_pcs])
        SB = [C.bank(0), C.bank(1), C.bank(4), C.bank(5)]
        cst = {"pm": 0, "sb": 0, "ob": 0}

        def qbuf(qt):
            return (qt // 4) % 2

        def load_q(qt):
            tg = qt // 4
            qb = qbuf(qt)
            P.dma("sp", qgB[qb][:], qTv[:, :, tg * 512:(tg + 1) * 512], reads=[r_qT[tg]], writes=[r_qg[qb]])

        def sel_scores(qt, hps):
            qb = qbuf(qt)
            ql = slice((qt % 4) * 128, (qt % 4 + 1) * 128)
            u0 = 248 - 8 * qt
            for hp in hps:
                a, u = hp // 4, hp % 4
                eb = hp % 2
                for s in range(2):
                    bank = C.bank(4 + s)
                    qap = qgB[qb][s * 64:(s + 1) * 64, a * 4 + u, ql]
                    P.op("pe", lambda e, bank=bank, s=s, a=a, qap=qap: e.matmul(bank.ap[:, 0:255], qap,
                                                                                KCM[s * 64:(s + 1) * 64, a, 0:255], start=True, stop=True),
                         reads=[r_qg[qb], r_cm], writes=[bank.res])
                    P.op("act", lambda e, bank=bank, eb=eb, s=s: e.activation(ee[eb][:, s, 0:255], bank.ap[:, 0:255], AF.Exp, scale=0.125),
                         reads=[bank.res], writes=[r_ee[eb]])
                for s in range(2):
                    g = 2 * a + s
                    k = s
                    c0 = 2 * (hp * 2 + s)
                    P.op("dve", lambda e, eb=eb, s=s, k=k, u0=u0, c0=c0: e.scalar_tensor_tensor(em[k][:, 0:255], ee[eb][:, s, 0:255], 1.0, cmast[:, u0:u0 + 255], ALU.mult, ALU.mult,
                                                                                                accum_out=sm[:, c0:c0 + 1]),
                         reads=[r_ee[eb], r_w], writes=[r_em[k], r_sm])
                    P.op("dve", lambda e, c0=c0: e.tensor_scalar(sm[:, c0 + 1:c0 + 2], sm[:, c0:c0 + 1], 1e-30, None, ALU.max), reads=[r_sm], writes=[r_sm])
                    P.op("dve", lambda e, c0=c0: e.reciprocal(sm[:, c0:c0 + 1], sm[:, c0 + 1:c0 + 2]), reads=[r_sm], writes=[r_sm])
                    if u == 0:
                        P.op("dve", lambda e, k=k, g=g, c0=c0: e.tensor_scalar(pcs[:, g, 0:255], em[k][:, 0:255], sm[:, c0:c0 + 1], None, ALU.mult),
                             reads=[r_em[k], r_sm], writes=[r_pcs])
                    else:
                        P.op("dve", lambda e, k=k, g=g, c0=c0: e.scalar_tensor_tensor(pcs[:, g, 0:255], em[k][:, 0:255], sm[:, c0:c0 + 1], pcs[:, g, 0:255], ALU.mult, ALU.add),
                             reads=[r_em[k], r_sm, r_pcs], writes=[r_pcs])

        def sel_top(qt):
            P.op("dve", lambda e: e.tensor_reduce(imp[:], pcs[:].rearrange("p g (j r) -> p g j r", r=4), AX.X, ALU.add), reads=[r_pcs], writes=[r_imp])
            sh = pcs[:, :, 0:252]
            shv = bass.AP(sh.tensor, sh.offset + 3, [list(sh.ap[0]), list(sh.ap[1]), [4, 63]])
            P.op("dve", lambda e, shv=shv: e.tensor_tensor(imp[:, :, 1:64], imp[:, :, 1:64], shv, ALU.add), reads=[r_pcs, r_imp], writes=[r_imp])
            P.op("dve", lambda e, qt=qt: e.tensor_tensor(imp[:], imp[:], fbc(fa[:, qt, :], 0, 4), ALU.add), reads=[r_imp, r_w], writes=[r_imp])
            for g in range(4):
                P.op("dve", lambda e, g=g: e.max(m8[:, g, 0:8], imp[:, g, :]), reads=[r_imp], writes=[r_m8])
                P.op("dve", lambda e, g=g: e.match_replace(imp3[:], m8[:, g, 0:8], imp[:, g, :], -1e30), reads=[r_imp, r_m8], writes=[r_imp3])
                P.op("dve", lambda e, g=g: e.max(m8[:, g, 8:16], imp3[:]), reads=[r_imp3], writes=[r_m8])
                P.op("dve", lambda e, g=g: e.tensor_scalar(m8[:, g, 0:1], m8[:, g, 15:16], -1.0, None, ALU.max), reads=[r_m8], writes=[r_m8])
                P.op("dve", lambda e, g=g: e.tensor_scalar(nsel[:, g // 2, (g % 2) * 64:(g % 2) * 64 + 64], imp[:, g, :], m8[:, g, 0:1], 1.0, ALU.is_ge, ALU.subtract),
                     reads=[r_imp, r_m8], writes=[r_nsel])
            bank = C.bank(4)
            pv = bank.ap.bitcast(BF16)
            for a2 in range(2):
                P.op("pe", lambda e, pv=pv, a2=a2: e.transpose(pv[:, a2 * 128:(a2 + 1) * 128], nsel[:, a2, :], C.identb[:]), reads=[r_nsel, C.r_ident], writes=[bank.res])
            P.op("act", lambda e, pv=pv: e.copy(nselT[:], pv[:, 0:256].rearrange("p (g t) -> p g t", g=2)), reads=[bank.res], writes=[r_nselT])

        OB = [C.bank(2), C.bank(3), C.bank(6), C.bank(7)]

        def branches_pair(qt, a):
            qb = qbuf(qt)
            ql = slice((qt % 4) * 128, (qt % 4 + 1) * 128)
            nvalid = min(255, 8 * qt + 7)
            work = []
            for br in range(3):
                if br == 0:
                    items = [("c", nt, 128 if nt == 0 else 127) for nt in range(2) if nt * 128 < nvalid]
                elif br == 1:
                    items = [("s", kt, 128) for kt in range(qt + 1)]
                else:
                    items = [("w", kt, 128) for kt in range(max(0, qt - 4), qt + 1)]
                for ii, it in enumerate(items):
                    work.append((br, ii, len(items)) + it)
            obanks = {}
            for br in range(3):
                k = cst["ob"] % 2
                cst["ob"] += 1
                obanks[br] = (OB[2 * k], OB[2 * k + 1])
            slots = {}

            def stage1(w):
                br, ii, nitems, kind, kt, nn = work[w]
                sbs, pis, mms = [], [], []
                for s in range(2):
                    pb = s * 64
                    g = 2 * a + s
                    qrh = qgB[qb][pb:pb + 64, a * 4:(a + 1) * 4, ql]
                    sb = SB[cst["sb"] % 4]
                    cst["sb"] += 1
                    pi = cst["pm"] % NPM
                    cst["pm"] += 1
                    sbs.append(sb)
                    pis.append(pi)
                    mm = []
                    if kind == "c":
                        mm.append((KCM[pb:pb + 64, a, kt * 128:kt * 128 + nn], qrh, [r_cm, r_qg[qb]]))
                        ci = NCMP_IDX.get((qt, kt))
                        if ci is not None:
                            mm.append((C.identb[0:nn, 0:nn], fbc(ncmp[0:nn, ci, :], 0, 4), [C.r_ident, r_w]))
                    elif kind == "s":
                        mm.append((KS_T[pb:pb + 64, a, kt * 128:(kt + 1) * 128], qrh, [r_K, r_qg[qb]]))
                        mm.append((esel[pb:pb + 64, kt, :], fbc(nselT[pb:pb + 64, a, :], 0, 4), [r_w, r_nselT]))
                        if kt == qt:
                            mm.append((C.identb[:], fbc(ntri[:, 0, :], 0, 4), [C.r_ident, r_w]))
                    else:
                        mm.append((KW_T[pb:pb + 64, a, kt * 128:(kt + 1) * 128], qrh, [r_K, r_qg[qb]]))
                        if kt == qt:
                            mm.append((C.identb[:], fbc(ntri[:, 0, :], 0, 4), [C.r_ident, r_w]))
                        if kt == qt - 4:
                            mm.append((C.identb[:], fbc(ntri[:, 1, :], 0, 4), [C.r_ident, r_w]))
                    mms.append(mm)
                slots[w] = pis
                nm = len(mms[0])
                for mi in range(nm):
                    for s in range(2):
                        l_, r_, rs_ = mms[s][mi]
                        sb = sbs[s]
                        P.op("pe", lambda e, sb=sb, nn=nn, l_=l_, r_=r_, mi=mi, last=(mi == nm - 1): e.matmul(
                            sb.ap[0:nn, :].rearrange("p (h t) -> p h t", h=4), l_, r_, start=(mi == 0), stop=last),
                            reads=rs_, writes=[sb.res])
                for s in range(2):
                    sb, pi = sbs[s], pis[s]
                    P.op("act", lambda e, sb=sb, nn=nn, pi=pi: e.activation(PM[pi][0:nn, :, :], sb.ap[0:nn, :].rearrange("p (h t) -> p h t", h=4), AF.Exp, scale=0.125),
                         reads=[sb.res], writes=[r_PM[pi]])

            def stage2(w):
                br, ii, nitems, kind, kt, nn = work[w]
                for s in range(2):
                    g = 2 * a + s
                    pi = slots[w][s]
                    ob = obanks[br][s]
                    oview = ob.ap[:, 0:260].rearrange("p (h c) -> p h c", h=4)
                    if kind == "c":
                        vsrc, vres = VCM[0:nn, kt, g, :], r_cm
                    elif kind == "s":
                        vsrc, vres = VS[:, kt, g, :], r_V
                    else:
                        vsrc, vres = VW[:, kt, g, :], r_V
                    for hh in range(4):
                        P.op("pe", lambda e, oview=oview, hh=hh, pi=pi, nn=nn, vsrc=vsrc, first=(ii == 0 and hh == 0), last=(ii == nitems - 1 and hh == 3): e.matmul(
                            oview[:, hh, :], PM[pi][0:nn, hh, :], vsrc, start=first, stop=last),
                            reads=[r_PM[pi], vres], writes=[ob.res])
                    if ii == nitems - 1:
                        fs = fac[:, s * 8:(s + 1) * 8]
                        rsum = oview[:, :, 64]
                        P.op("dve", lambda e, rsum=rsum, fs=fs: e.tensor_scalar(fs[:, 0:4], rsum, 1e-30, None, ALU.max), reads=[ob.res], writes=[r_fac])
                        P.op("dve", lambda e, fs=fs: e.reciprocal(fs[:, 4:8], fs[:, 0:4]), reads=[r_fac], writes=[r_fac])
                        gsl = GT[:, qt, g * 12:(g + 1) * 12]
                        gv = bass.AP(gsl.tensor, gsl.offset + br, [list(gsl.ap[0]), [3, 4]])
                        P.op("dve", lambda e, gv=gv, fs=fs: e.tensor_tensor(fs[:, 0:4], fs[:, 4:8], gv, ALU.mult), reads=[r_fac, r_G], writes=[r_fac])
                        if br == 0:
                            P.op("dve", lambda e, oview=oview, g=g, fs=fs: e.tensor_tensor(oacc[:, g * 4:(g + 1) * 4, :], oview[:, :, 0:64], fbc(fs[:, 0:4], 1, 64), ALU.mult),
                                 reads=[ob.res, r_fac], writes=[r_oacc])
                        else:
                            ot = otmp2[s]
                            P.op("dve", lambda e, oview=oview, fs=fs, ot=ot: e.tensor_tensor(ot[:], oview[:, :, 0:64], fbc(fs[:, 0:4], 1, 64), ALU.mult),
                                 reads=[ob.res, r_fac], writes=[r_otmp])
                            P.op("pool", lambda e, g=g, ot=ot: e.tensor_tensor(oacc[:, g * 4:(g + 1) * 4, :], oacc[:, g * 4:(g + 1) * 4, :], ot[:], ALU.add),
                                 reads=[r_otmp, r_oacc], writes=[r_oacc])

            LA = 2
            nw = len(work)
            for w in range(min(LA, nw)):
                stage1(w)
            for w in range(nw):
                if w + LA < nw:
                    stage1(w + LA)
                stage2(w)

        def outproj(qt):
            qs = slice(qt * 128, (qt + 1) * 128)
            P.dma("sp", xs[:], x_d[qs, :], writes=[r_xs])
            P.op("act", lambda e: e.copy(obf[:], oacc[:].rearrange("p h d -> p (h d)")), reads=[r_oacc], writes=[r_obf])
            bank = C.bank(4)
            pv = bank.ap.bitcast(BF16)
            for j in range(8):
                P.op("pe", lambda e, pv=pv, j=j: e.transpose(pv[:, j * 128:(j + 1) * 128], obf[:, j * 128:(j + 1) * 128], C.identb[:]), reads=[r_obf, C.r_ident], writes=[bank.res])
            P.op("act", lambda e, pv=pv: e.copy(OT[:], pv.rearrange("p (j t) -> p j t", j=8)), reads=[bank.res], writes=[r_OT])
            for half in range(2):
                bank = C.bank(4 + half)
                for j in range(8):
                    P.op("pe", lambda e, bank=bank, j=j, half=half: e.matmul(bank.ap[:, :], OT[:, j, :], Wo[:, j, half * 512:(half + 1) * 512], start=(j == 0), stop=(j == 7)),
                         reads=[r_OT, r_w], writes=[bank.res])
                hs = slice(half * 512, (half + 1) * 512)
                P.op("dve", lambda e, bank=bank, hs=hs: e.tensor_tensor(ys[:, hs], bank.ap[:, :], C.gate[:, hs], ALU.mult), reads=[bank.res, C.r_gate], writes=[r_ys])
                P.op("pool", lambda e, hs=hs: e.tensor_tensor(ys[:, hs], ys[:, hs], xs[:, hs], ALU.add), reads=[r_ys, r_xs], writes=[r_ys])
            tok = P.dma("sp", xout_d[qs, :], ys[:], reads=[r_ys], writes=[C.r_xout[qt]])
            C.final.append(tok)

        nq = NT
        if DBG["stop"] == "nsa_qt":
            nq = DBG.get("nqt", 3) + 1
        load_q(0)
        sel_scores(0, range(8))
        sel_top(0)
        for qt in range(nq):
            nx = qt + 1
            has = nx < nq
            if has and nx % 4 == 0:
                load_q(nx)
            for a in range(2):
                branches_pair(qt, a)
                if has:
                    sel_scores(nx, [4 * a, 4 * a + 1, 4 * a + 2, 4 * a + 3])
            outproj(qt)
            if has:
                sel_top(nx)
        P.barrier()


def build(stages, ncores=8):
    nc = bass.Bass("TRN2", target_bir_lowering=False)
    P = Prog(nc)
    C = Ctx()
    C.P, C.nc = P, nc
    Dm = {}
    C.D = Dm
    C.final = []

    def din(name, shape, dt=F32):
        Dm[name] = nc.dram_tensor(name, list(shape), dt, kind="ExternalInput").ap()
        return Dm[name]

    def dout(name, shape, dt=F32):
        Dm[name] = nc.dram_tensor(name, list(shape), dt, kind="ExternalOutput").ap()
        return Dm[name]

    def dtmp(name, shape, dt=F32):
        Dm[name] = nc.dram_tensor(name, list(shape), dt).ap()
        return Dm[name]

    C.fence_res = Res()
    C.pending_fence = []

    def fence(rl):
        P.barrier()
    C.fence = fence

    banks = [Bank(P.psum("bank%d" % i, [128, 512], F32)) for i in range(8)]
    C.bank = lambda i: banks[i]
    NSTG = 3
    stg = [P.sbuf("stg%d" % i, [128, 1024], F32) for i in range(NSTG)]
    r_stg = [Res() for _ in range(NSTG)]
    stg_i = [0]

    def wload(dst, src, dres):
        p, n = dst.shape[0], dst.shape[-1]
        i = stg_i[0] % NSTG
        stg_i[0] += 1
        P.dma("sp", stg[i][0:p, 0:n], src, writes=[r_stg[i]])
        P.op("pool", lambda e, i=i, p=p, n=n, dst=dst: e.tensor_copy(dst, stg[i][0:p, 0:n]), reads=[r_stg[i]], writes=[dres])
    C.wload = wload

    order = ["dsa", "ffn", "nsa", "moe"]
    xnames = {"dsa": ("x", "x1"), "ffn": ("x1", "x2"), "nsa": ("x2", "x3"), "moe": ("x3", "out")}
    first, last = stages[0], stages[-1]
    for sname in stages:
        a, b = xnames[sname]
        if a not in Dm:
            din(a, [T, D])
        if sname == last:
            dout(b, [T, D])
        else:
            dtmp(b, [T, D])
    din("csil_in", [128, 8])
    din("ada_w", [2, 2, 1024, 3072])
    din("ada_b", [2, 2, 3072])
    din("norm_mix", [2, 1024])
    din("norm_ffn", [2, 1024])
    din("ident_in", [128, 128])
    dtmp("hT", [8, 128, T], BF16)
    dtmp("yacc", [T, D])
    C.r_hT = [Res() for _ in range(8)]
    C.r_yacc = [Res() for _ in range(NT)]
    rx = {n: [Res() for _ in range(NT)] for n in ["x", "x1", "x2", "x3", "out"]}

    C.identb = P.sbuf("identb", [128, 128], BF16)
    C.r_ident = Res()
    P.dma("pool", C.identb[:], Dm["ident_in"], writes=[C.r_ident])
    csl = P.sbuf("csl", [128, 8], F32)
    C.csb = P.sbuf("csb", [128, 8, 128], F32)
    C.r_csb = Res()
    r_csl = Res()
    P.dma("sp", csl[:], Dm["csil_in"], writes=[r_csl])
    P.op("act", lambda e: e.activation(csl[:], csl[:], AF.Silu), reads=[r_csl], writes=[r_csl])
    P.op("dve", lambda e: e.tensor_copy(C.csb[:], fbc(csl[:], 1, 128)), reads=[r_csl], writes=[C.r_csb])
    C.epsc = P.sbuf("epsc", [128, 2], F32)
    C.r_eps = Res()
    P.op("pool", lambda e: e.memset(C.epsc[:], EPS), writes=[C.r_eps])
    C.gs = P.sbuf("gs", [128, 1024], F32)
    C.shift = P.sbuf("shift", [128, 1024], F32)
    C.gate = P.sbuf("gate", [128, 1024], F32)
    C.r_gs, C.r_shift, C.r_gate = Res(), Res(), Res()

    for sname in stages:
        a, b = xnames[sname]
        C.r_xin = rx[a]
        C.r_xout = rx[b]
        C.final = []
        if sname == "dsa":
            for nm, shp in [("dsa_w_in", [1024, 472]), ("dsa_w_o", [1024, 1024]), ("dsa_wuq_nope", [256, 768]), ("dsa_wuq_rope", [256, 256]),
                            ("dsa_wuq_rsw", [256, 256]), ("dsa_wiq", [256, 512]), ("dsa_wiq_sw", [256, 512]), ("dsa_wukT", [48, 16, 128]),
                            ("dsa_wuv2", [128, 16, 128]), ("dsa_g_q", [256]), ("dsa_g_kv", [128]), ("rope_ctm", [T, 16]), ("rope_stm", [T, 16]),
                            ("rope_cfm", [128, T]), ("rope_sfm", [128, T])]:
                if nm not in Dm:
                    din(nm, shp)
            mod_stage(C, 0, 0, Dm["norm_mix"][0])
            dsa_stage(C, Dm[a], Dm[b])
        elif sname == "nsa":
            for ent in [("nsa_w_o", [1024, 1024]), ("nsa_wk", [4, 1024, 256]), ("nsa_wk_sw", [3, 1024, 256]), ("nsa_wq", [1024, 1024]),
                            ("nsa_wq_sw", [1024, 1024]), ("nsa_wvg", [1024, 560]), ("nsa_w1k", [128, 32, 256]), ("nsa_w1v", [128, 32, 256]),
                            ("nsa_peT", [128, 32]), ("nsa_w2k2", [256, 2, 128]), ("nsa_w2v", [256, 64]), ("nsa_esel", [128, 32, 128], BF16),
                            ("nsa_ntri", [128, 2, 128], BF16), ("nsa_ncmp", [128, NCMP_N, 128], BF16), ("nsa_fa", [128, 32, 64]), ("nsa_cmast", [128, 512]),
                            ("rope_cfm", [128, T]), ("rope_sfm", [128, T])]:
                nm, shp = ent[0], ent[1]
                if nm not in Dm:
                    din(nm, shp, ent[2] if len(ent) > 2 else F32)
            dtmp("qT", [8, 128, T], BF16)
            mod_stage(C, 1, 0, Dm["norm_mix"][1])
            prenorm_stage(C, Dm[a], Dm["hT"])
            nsa_stage(C, Dm[a], Dm[b])
        elif sname == "ffn":
            din("ffn_w1", [1024, 2816])
            din("ffn_w3", [1024, 2816])
            din("ffn_w2", [2816, 1024])
            mod_stage(C, 0, 1, Dm["norm_ffn"][0])
            if DBG["stop"] == "mod":
                dout("dbg_mod", [3, 128, 1024])
                for i, t in enumerate([C.gs, C.shift, C.gate]):
                    C.final.append(P.dma("sp", Dm["dbg_mod"][i], t[:], reads=[C.r_gs, C.r_shift, C.r_gate], writes=[Res()]))
                break
            if DBG["stop"] == "prenorm":
                prenorm_stage(C, Dm[a], Dm["hT"])
                dout("dbg_hT", [8, 128, T], BF16)
                hsb = P.sbuf("dbg_hsb", [128, 8, 512], BF16)
                rr = Res()
                for tg in range(8):
                    P.dma("sp", hsb[:], Dm["hT"].rearrange("kc p t -> p kc t")[:, :, tg * 512:(tg + 1) * 512], reads=[C.r_hT[tg]], writes=[rr])
                    C.final.append(P.dma("sp", Dm["dbg_hT"].rearrange("kc p t -> p kc t")[:, :, tg * 512:(tg + 1) * 512], hsb[:], reads=[rr], writes=[Res()]))
                break
            passes = []
            for (f0, F) in [(0, 1024), (1024, 896), (1920, 896)]:
                passes.append((Dm["ffn_w1"][:, f0:f0 + F], Dm["ffn_w3"][:, f0:f0 + F], Dm["ffn_w2"][f0:f0 + F, :], None))
            ffn_passes(C, Dm["hT"], passes, Dm["yacc"], pn_args=(Dm[a], None))
            combine_stage(C, Dm[a], Dm["yacc"], Dm[b])
        elif sname == "moe":
            din("moe_router", [1024, 8])
            din("moe_w1", [8, 1024, 3584])
            din("moe_w3", [8, 1024, 3584])
            din("moe_w2", [8, 3584, 1024])
            din("final_norm", [1024])
            mod_stage(C, 1, 1, Dm["norm_ffn"][1])
            wrT = P.sbuf("moe_wrT", [128, 8, 8], F32)
            tokgate = P.sbuf("moe_tokgate", [128, 32, 8], F32)
            r_wr = Res()
            r_tgl = [Res() for _ in range(8)]
            P.dma("sp", wrT[:], Dm["moe_router"].rearrange("(kc p) e -> p kc e", p=128), writes=[r_wr])
            passes = []
            for ex in range(8):
                for qf in range(4):
                    f0 = qf * 896
                    passes.append((Dm["moe_w1"][ex, :, f0:f0 + 896], Dm["moe_w3"][ex, :, f0:f0 + 896],
                                   Dm["moe_w2"][ex, f0:f0 + 896, :], ex))
            ffn_passes(C, Dm["hT"], passes, Dm["yacc"], tokgate=(tokgate, r_tgl),
                       pn_args=(Dm[a], dict(wrT=(wrT, r_wr), tokgate=(tokgate, r_tgl))), FM=896)
            combine_stage(C, Dm[a], Dm["yacc"], Dm[b], final_g=Dm["final_norm"])
    P.finish(C.final)
    return nc


def host_prep(inputs, b, stages):
    m = {}
    m["csil_in"] = np.ascontiguousarray(inputs["c"][b].reshape(8, 128).T)
    m["ada_w"] = inputs["ada_w"]
    m["ada_b"] = inputs["ada_b"]
    m["norm_mix"] = inputs["norm_mix"]
    m["norm_ffn"] = inputs["norm_ffn"]
    m["ident_in"] = np.eye(128, dtype=np.float32)
    if "dsa" in stages or "nsa" in stages:
        inv = (500000.0 ** (-np.arange(0, 16, 2, dtype=np.float32) / 16)).astype(np.float32)
        ang = np.arange(T, dtype=np.float32)[:, None] * inv[None, :]
        co, si = np.cos(ang).astype(np.float32), np.sin(ang).astype(np.float32)
        m["rope_ctm"] = np.ascontiguousarray(np.concatenate([co, co], 1))
        m["rope_stm"] = np.ascontiguousarray(np.concatenate([-si, si], 1))
        cf = np.ones((64, T), np.float32)
        sf = np.zeros((64, T), np.float32)
        cf[0:8] = co.T
        cf[8:16] = co.T
        sf[0:8] = -si.T
        sf[8:16] = si.T
        m["rope_cfm"] = np.ascontiguousarray(np.concatenate([cf, cf], 0))
        m["rope_sfm"] = np.ascontiguousarray(np.concatenate([sf, sf], 0))
    if "dsa" in stages:
        m["dsa_w_in"] = inputs["dsa_w_in"][0]
        m["dsa_w_o"] = inputs["dsa_w_o"][0]
        wuq = inputs["dsa_w_uq"][0].reshape(256, 16, 64)
        m["dsa_wuq_nope"] = np.ascontiguousarray(wuq[:, :, 16:64].reshape(256, 768))
        m["dsa_wuq_rope"] = np.ascontiguousarray(wuq[:, :, 0:16].reshape(256, 256))
        m["dsa_wuq_rsw"] = np.ascontiguousarray(np.concatenate([wuq[:, :, 8:16], wuq[:, :, 0:8]], 2).reshape(256, 256))
        wiq = inputs["dsa_w_iq"][0].reshape(256, 8, 64)
        m["dsa_wiq"] = np.ascontiguousarray(wiq.reshape(256, 512))
        m["dsa_wiq_sw"] = np.ascontiguousarray(np.concatenate([wiq[:, :, 8:16], wiq[:, :, 0:8], wiq[:, :, 16:64]], 2).reshape(256, 512))
        m["dsa_wukT"] = np.ascontiguousarray(inputs["dsa_w_uk"][0].transpose(2, 0, 1))
        wuv2 = np.zeros((128, 16, 128), np.float32)
        for h in range(16):
            wuv2[:, h, (h % 2) * 64:(h % 2) * 64 + 64] = inputs["dsa_w_uv"][0][h]
        m["dsa_wuv2"] = wuv2
        m["dsa_g_q"] = inputs["dsa_g_q"][0]
        m["dsa_g_kv"] = inputs["dsa_g_kv"][0]
    if "nsa" in stages:
        w = inputs["nsa_w_in"][0]

        def sw(wc, nh):
            x = wc.reshape(1024, nh, 64)
            return np.concatenate([x[:, :, 8:16], x[:, :, 0:8], x[:, :, 16:64]], 2).reshape(1024, nh * 64)

        def blk(wc):
            x = wc.reshape(1024, 16, 64)
            cols = []
            for a_ in range(2):
                for u_ in range(4):
                    cols += [x[:, 8 * a_ + u_], x[:, 8 * a_ + 4 + u_]]
            return np.ascontiguousarray(np.concatenate(cols, 1))
        wq = w[:, 0:1024]
        m["nsa_wq"] = blk(wq)
        m["nsa_wq_sw"] = blk(sw(wq, 16))
        kc, vc, ks, vs, kw, vw = [w[:, 1024 + 256 * i_:1280 + 256 * i_] for i_ in range(6)]
        m["nsa_wk"] = np.ascontiguousarray(np.stack([kc, ks, kw, vc], 0))
        m["nsa_wk_sw"] = np.ascontiguousarray(np.stack([sw(kc, 4), sw(ks, 4), sw(kw, 4)], 0))
        m["nsa_wvg"] = np.ascontiguousarray(np.concatenate([vs, vw, w[:, 2560:2608]], 1))
        m["nsa_w_o"] = inputs["nsa_w_o"][0]
        for nm, src in [("nsa_w1k", "nsa_cmp_k1"), ("nsa_w1v", "nsa_cmp_v1")]:
            x = inputs[src][0].reshape(32, 64, 256).transpose(1, 0, 2)
            m[nm] = np.ascontiguousarray(np.concatenate([x, x], 0))
        pe = inputs["nsa_cmp_pe"][0].T
        m["nsa_peT"] = np.ascontiguousarray(np.concatenate([pe, pe], 0))
        w2k2 = np.zeros((256, 2, 128), np.float32)
        w2k2[:, 0, 0:64] = inputs["nsa_cmp_k2"][0]
        w2k2[:, 1, 64:128] = inputs["nsa_cmp_k2"][0]
        m["nsa_w2k2"] = w2k2
        m["nsa_w2v"] = inputs["nsa_cmp_v2"][0]
        m["nsa_esel"], m["nsa_ntri"], m["nsa_ncmp"] = [np.asarray(a_).astype(ml_dtypes.bfloat16) for a_ in _NC[0:3]]
        m["nsa_fa"], m["nsa_cmast"] = _NC[4], _NC[5]
    if "ffn" in stages:
        m["ffn_w1"] = inputs["ffn_w1"][0]
        m["ffn_w3"] = inputs["ffn_w3"][0]
        m["ffn_w2"] = inputs["ffn_w2"][0]
    if "moe" in stages:
        m["moe_router"] = inputs["moe_router"][0]
        m["moe_w1"] = inputs["moe_w1"][0]
        m["moe_w3"] = inputs["moe_w3"][0]
        m["moe_w2"] = inputs["moe_w2"][0]
        m["final_norm"] = inputs["final_norm"]
    return m


_CACHE = {}


def kernel(**inputs):
    inputs = {k: np.asarray(v) for k, v in inputs.items()}
    stages = ["dsa", "ffn", "nsa", "moe"]
    if "nc" not in _CACHE:
        _CACHE["nc"] = build(stages)
    nc = _CACHE["nc"]
    shared = host_prep(inputs, 0, stages)
    in_maps = []
    for b in range(8):
        m = dict(shared)
        m["csil_in"] = np.ascontiguousarray(inputs["c"][b].reshape(8, 128).T.astype(np.float32))
        m["x"] = np.ascontiguousarray(inputs["x"][b].astype(np.float32))
        in_maps.append(m)
    res = run_bass_kernel_spmd(nc, in_maps, core_ids=list(range(8)))
    out = np.stack([np.asarray(res.results[b]["out"]) for b in range(8)], 0).astype(np.float32)
    return out
```

```python
import numpy as np
import ml_dtypes
from contextlib import ExitStack
import concourse.bass as bass
import concourse.mybir as mybir
from concourse.bass_utils import run_bass_kernel_spmd

F32 = mybir.dt.float32
BF16 = mybir.dt.bfloat16
I32 = mybir.dt.int32
AF = mybir.ActivationFunctionType
ALU = mybir.AluOpType
AX = mybir.AxisListType

T = 4096
D = 1024
NT = 32
EPS = 1e-6
NEG = -30000.0


class Res:
    __slots__ = ("w", "r", "name")

    def __init__(self, name=""):
        self.w = {}
        self.r = []
        self.name = name


class Prog:
    CENG = ["pe", "act", "dve", "pool"]
    NPOOL = 16
    EPOCH = 30000

    def __init__(self, nc):
        self.nc = nc
        self.es = ExitStack()
        self.q = {e: [] for e in ["pe", "act", "dve", "pool", "sp"]}
        self.sems = {}
        self.cnt = {}
        self.seen = {e: {} for e in self.q}
        self.nsem = 0
        self.epoch = {e: 0 for e in self.CENG}
        self.ecount = {e: 0 for e in self.CENG}
        self.cur = {}
        for e in self.CENG:
            self._new_epoch(e)
        self.dpool = {}
        self.dpos = {}
        self.ninstr = 0
        self.pending = {e: [] for e in self.q}
        self.lasttok = {}

    def barrier(self):
        toks = list(self.lasttok.values())
        for k, v in self.cnt.items():
            if k[0] == "dma" and v > 0:
                toks.append((k, v))
        for e in self.q:
            self.pending[e].extend(toks)

    def _mksem(self, key):
        s = self.es.enter_context(self.nc.semaphore("s%d" % self.nsem))
        self.nsem += 1
        self.sems[key] = s
        self.cnt[key] = 0
        return s

    def _new_epoch(self, e):
        key = (e, self.epoch[e])
        self.epoch[e] += 1
        self._mksem(key)
        self.cur[e] = key
        self.ecount[e] = 0

    def sbuf(self, name, shape, dt, stack=None):
        self.nsem += 1
        return (stack or self.es).enter_context(self.nc.sbuf_tensor("%s_u%d" % (name, self.nsem), shape, dt))

    def psum(self, name, shape, dt):
        return self.es.enter_context(self.nc.psum_tensor(name, shape, dt))

    def _waits_for(self, eng, reads, writes):
        toks = self.pending[eng]
        self.pending[eng] = []
        for r in reads:
            toks.extend(r.w.items())
        for w in writes:
            toks.extend(w.w.items())
            toks.extend(w.r)
        need = {}
        seen = self.seen[eng]
        for (k, v) in toks:
            if eng == "pe" and k[0] == "pe":
                continue
            if seen.get(k, 0) >= v:
                continue
            if need.get(k, 0) < v:
                need[k] = v
        for k, v in need.items():
            seen[k] = v
        return list(need.items())

    def _commit(self, tok, reads, writes):
        for r in reads:
            r.r.append(tok)
            if len(r.r) > 16:
                d = {}
                for (k, v) in r.r:
                    if d.get(k, 0) < v:
                        d[k] = v
                r.r = list(d.items())
        for w in writes:
            if w.w.get(tok[0], 0) < tok[1]:
                w.w[tok[0]] = tok[1]
            w.r = []

    def op(self, eng, fn, reads=(), writes=()):
        if self.ecount[eng] >= self.EPOCH:
            self._new_epoch(eng)
        waits = self._waits_for(eng, reads, writes)
        key = self.cur[eng]
        self.cnt[key] += 1
        self.ecount[eng] += 1
        tok = (key, self.cnt[key])
        self.lasttok[eng] = tok
        self.q[eng].append((waits, fn, key, 1))
        self._commit(tok, reads, writes)
        self.ninstr += 1
        return tok

    def op_raw(self, eng, fn, inc, reads=(), writes=()):
        if eng not in self.dpool:
            self.dpool[eng] = []
            for i in range(self.NPOOL):
                self._mksem(("dma", eng, i))
                self.dpool[eng].append(("dma", eng, i))
            self.dpos[eng] = 0
        key = self.dpool[eng][self.dpos[eng] % self.NPOOL]
        self.dpos[eng] += 1
        waits = self._waits_for(eng, reads, writes)
        prev = self.cnt[key]
        if prev > 0 and self.seen[eng].get(key, 0) < prev:
            waits.append((key, prev))
            self.seen[eng][key] = prev
        self.cnt[key] += inc
        tok = (key, self.cnt[key])
        self.q[eng].append((waits, fn, key, inc))
        self._commit(tok, reads, writes)
        self.ninstr += 1
        return tok

    def dma(self, eng, out, in_, reads=(), writes=(), **kw):
        if eng not in self.dpool:
            self.dpool[eng] = []
            for i in range(self.NPOOL):
                self._mksem(("dma", eng, i))
                self.dpool[eng].append(("dma", eng, i))
            self.dpos[eng] = 0
        key = self.dpool[eng][self.dpos[eng] % self.NPOOL]
        self.dpos[eng] += 1
        waits = self._waits_for(eng, reads, writes)
        prev = self.cnt[key]
        if prev > 0 and self.seen[eng].get(key, 0) < prev:
            waits.append((key, prev))
            self.seen[eng][key] = prev
        self.cnt[key] += 16
        tok = (key, self.cnt[key])

        def fn(e, out=out, in_=in_, kw=kw):
            return e.dma_start(out=out, in_=in_, **kw)
        self.q[eng].append((waits, fn, key, 16))
        self._commit(tok, reads, writes)
        self.ninstr += 1
        return tok

    def finish(self, final_tokens):
        nc = self.nc
        sems = self.sems
        q = self.q
        fw = {}
        for (k, v) in final_tokens:
            if fw.get(k, 0) < v:
                fw[k] = v

        def emit(e, lst):
            for (waits, fn, key, inc) in lst:
                for (k, v) in waits:
                    e.wait_ge(sems[k], v)
                fn(e).then_inc(sems[key], inc)

        with nc.Block() as block:
            @block.tensor
            def _(e):
                emit(e, q["pe"])

            @block.scalar
            def _(e):
                emit(e, q["act"])

            @block.vector
            def _(e):
                emit(e, q["dve"])

            @block.gpsimd
            def _(e):
                emit(e, q["pool"])

            @block.sync
            def _(e):
                emit(e, q["sp"])
                for k, v in fw.items():
                    e.wait_ge(sems[k], v)
        self.es.close()


def fbc(ap, pos, n):
    l = [list(x) for x in ap.ap]
    l.insert(1 + pos, [0, n])
    return bass.AP(ap.tensor, ap.offset, l)


def _nsa_consts():
    n = np.arange(128)
    esel = np.zeros((128, 32, 128), np.float32)
    for kt in range(32):
        for s_ in range(2):
            esel[64 * s_ + 2 * kt, kt, 0:64] = 30000.0
            esel[64 * s_ + 2 * kt + 1, kt, 64:128] = 30000.0
    ntri = np.zeros((128, 2, 128), np.float32)
    ntri[:, 0, :] = np.where(n[:, None] > n[None, :], NEG, 0.0)
    ntri[:, 1, :] = np.where(n[:, None] <= n[None, :], NEG, 0.0)
    idx = {}
    tiles = []
    for qt in range(32):
        nvalid = min(255, 8 * qt + 7)
        for nt in range(2):
            if nt * 128 >= nvalid:
                continue
            nn = nt * 128 + n
            tq = qt * 128 + n
            mk = np.where(16 * nn[:, None] + 31 > tq[None, :], NEG, 0.0).astype(np.float32)
            if np.any(mk != 0):
                idx[(qt, nt)] = len(tiles)
                tiles.append(mk)
    ncmp = np.ascontiguousarray(np.stack(tiles, 1))
    fa = np.zeros((128, 32, 64), np.float32)
    j = np.arange(64)
    for qt in range(32):
        cur = 2 * qt + (n >= 64).astype(np.int64)
        forced = (j[None, :] == 0) | (j[None, :] == cur[:, None]) | (j[None, :] == cur[:, None] - 1)
        valid = j[None, :] <= cur[:, None]
        fa[:, qt, :] = np.where(valid, np.where(forced, 1e4, 0.0), -1e30)
    u = np.arange(512)
    cmast = (16 * (u[None, :] - 248) + 31 <= n[:, None]).astype(np.float32)
    return esel, ntri, ncmp, idx, fa, cmast


_NC = _nsa_consts()
NCMP_IDX = _NC[3]
NCMP_N = _NC[2].shape[1]


class Ctx:
    pass


DBG = {"stop": None}


def mod_stage(C, l, s, normg_d):
    P, nc, Dm = C.P, C.nc, C.D
    with ExitStack() as st:
        bb = P.sbuf("mod_bb", [128, 3072], F32, st)
        gb = P.sbuf("mod_gb", [128, 1024], F32, st)
        modt = P.sbuf("mod_t", [128, 2048], F32, st)
        wch = [P.sbuf("mod_w%d" % i, [128, 8, 512], F32, st) for i in range(2)]
        r_bb, r_gb, r_mod = Res(), Res(), Res()
        r_w = [Res(), Res()]
        P.dma("sp", bb[:], bass.AP(Dm["ada_b"].tensor, Dm["ada_b"][l, s, :].offset, [[0, 128], [1, 3072]]), writes=[r_bb])
        P.dma("sp", gb[:], bass.AP(normg_d.tensor, normg_d.offset, [[0, 128], [1, 1024]]), writes=[r_gb])
        wv = Dm["ada_w"][l, s].rearrange("(kc p) n -> p kc n", p=128)
        for n in range(6):
            w = wch[n % 2]
            P.dma("sp", w[:], wv[:, :, n * 512:(n + 1) * 512], writes=[r_w[n % 2]])
            bank = C.bank(6 + (n % 2))
            for kc in range(8):
                P.op("pe", lambda e, w=w, kc=kc, bank=bank: e.matmul(bank.ap[:, :], C.csb[:, kc, :], w[:, kc, :],
                                                                     start=(kc == 0), stop=(kc == 7)),
                     reads=[C.r_csb, r_w[n % 2]], writes=[bank.res])
            if n < 2:
                dst, dres = C.shift[:, n * 512:(n + 1) * 512], C.r_shift
            elif n < 4:
                dst, dres = modt[:, (n - 2) * 512:(n - 1) * 512], r_mod
            else:
                dst, dres = C.gate[:, (n - 4) * 512:(n - 3) * 512], C.r_gate
            P.op("dve", lambda e, dst=dst, bank=bank, n=n: e.tensor_tensor(dst, bank.ap[:, :], bb[:, n * 512:(n + 1) * 512], ALU.add),
                 reads=[bank.res, r_bb], writes=[dres])
        P.op("dve", lambda e: e.scalar_tensor_tensor(C.gs[:], modt[:, 0:1024], 1.0, gb[:], ALU.add, ALU.mult),
             reads=[r_mod, r_gb], writes=[C.r_gs])
        C.fence([r_bb, r_gb, r_mod] + r_w)


class Bank:
    def __init__(self, ap):
        self.ap = ap
        self.res = Res()


class Prenorm:
    def __init__(self, C, x_d, hT_d, st, router=None):
        P = C.P
        self.C, self.x_d, self.router = C, x_d, router
        self.hTv = hT_d.rearrange("kc p t -> p kc t")
        self.xt = P.sbuf("pn_x", [128, 1024], F32, st)
        self.sq = P.sbuf("pn_sq", [128, 1024], BF16, st)
        self.t1 = [P.sbuf("pn_t%d" % i, [128, 1024], F32, st) for i in range(4 if router else 2)]
        self.hb = [P.sbuf("pn_hb%d" % i, [128, 1024], BF16, st) for i in range(4)]
        self.hgp = P.sbuf("pn_hg", [128, 8, 512], BF16, st)
        self.ss = P.sbuf("pn_ss", [128, 64], F32, st)
        self.r_x, self.r_sq, self.r_hgp = Res(), Res(), Res()
        self.r_t1 = [Res() for _ in self.t1]
        self.r_hb = [Res() for _ in self.hb]
        self.r_ss = [Res() for _ in range(32)]
        if router:
            self.identf = P.sbuf("pn_idf", [128, 128], F32, st)
            self.r_idf = Res()
            P.dma("sp", self.identf[:], C.D["ident_in"], writes=[self.r_idf])
            self.hTf = P.sbuf("pn_hTf", [128, 8, 128], F32, st)
            self.r_hTf = Res()
            self.lg = P.sbuf("pn_lg", [128, 32, 8], F32, st)
            self.r_lg = [Res() for _ in range(8)]
            self.m1 = P.sbuf("pn_m1", [128, 4, 8], F32, st)
            self.l2 = P.sbuf("pn_l2", [128, 4, 8], F32, st)
            self.ex = P.sbuf("pn_ex", [128, 4, 8], F32, st)
            self.mx = P.sbuf("pn_mx", [128, 4, 4], F32, st)
            self.r_a = Res()

    def front(self, tg):
        C, P = self.C, self.C.P
        xt, sq, ss = self.xt, self.sq, self.ss
        for j in range(4):
            i = tg * 4 + j
            t1, r_t1 = (self.t1[j], self.r_t1[j]) if self.router else (self.t1[j % 2], self.r_t1[j % 2])
            hb, r_hb = self.hb[j], self.r_hb[j]
            P.dma("sp", xt[:], self.x_d[i * 128:(i + 1) * 128, :], reads=[C.r_xin[i]] if C.r_xin else [], writes=[self.r_x])
            P.op("act", lambda e, i=i: e.activation(sq[:], xt[:], AF.Square, accum_out=ss[:, 2 * i:2 * i + 1]),
                 reads=[self.r_x], writes=[self.r_sq, self.r_ss[i]])
            P.op("act", lambda e, i=i: e.activation(ss[:, 2 * i + 1:2 * i + 2], ss[:, 2 * i:2 * i + 1], AF.Sqrt, bias=C.epsc[:, 0:1], scale=1.0 / D),
                 reads=[self.r_ss[i], C.r_eps], writes=[self.r_ss[i]])
            P.op("dve", lambda e, i=i: e.reciprocal(ss[:, 2 * i:2 * i + 1], ss[:, 2 * i + 1:2 * i + 2]),
                 reads=[self.r_ss[i]], writes=[self.r_ss[i]])
            P.op("dve", lambda e, i=i, t1=t1: e.scalar_tensor_tensor(t1[:], xt[:], ss[:, 2 * i:2 * i + 1], C.gs[:], ALU.mult, ALU.mult),
                 reads=[self.r_x, self.r_ss[i], C.r_gs], writes=[r_t1])
            if not self.router:
                P.op("pool", lambda e, t1=t1, hb=hb: e.tensor_tensor(hb[:], t1[:], C.shift[:], ALU.add),
                     reads=[r_t1, C.r_shift], writes=[r_hb])
            else:
                P.op("pool", lambda e, t1=t1: e.tensor_tensor(t1[:], t1[:], C.shift[:], ALU.add),
                     reads=[r_t1, C.r_shift], writes=[r_t1])
                P.op("act", lambda e, t1=t1, hb=hb: e.copy(hb[:], t1[:]), reads=[r_t1], writes=[r_hb])

    def back(self, tg):
        C, P = self.C, self.C.P
        hgp = self.hgp
        for j in range(4):
            i = tg * 4 + j
            hb, r_hb = self.hb[j], self.r_hb[j]
            bank = C.bank(6) if self.router else C.bank(6 + (j % 2))
            pv = bank.ap.bitcast(BF16)
            for kc in range(8):
                P.op("pe", lambda e, hb=hb, kc=kc, pv=pv: e.transpose(pv[:, kc * 128:(kc + 1) * 128], hb[:, kc * 128:(kc + 1) * 128], C.identb[:]),
                     reads=[r_hb, C.r_ident], writes=[bank.res])
            P.op("act", lambda e, j=j, pv=pv: e.copy(hgp[:, :, j * 128:(j + 1) * 128], pv.rearrange("p (kc t) -> p kc t", kc=8)),
                 reads=[bank.res], writes=[self.r_hgp])
            if self.router:
                t1, r_t1 = self.t1[j], self.r_t1[j]
                wrT, r_wr = self.router["wrT"]
                b7 = C.bank(7)
                for half in range(2):
                    for k in range(4):
                        kc = half * 4 + k
                        P.op("pe", lambda e, t1=t1, kc=kc, k=k, b7=b7: e.transpose(b7.ap[:, k * 128:(k + 1) * 128], t1[:, kc * 128:(kc + 1) * 128], self.identf[:]),
                             reads=[r_t1, self.r_idf], writes=[b7.res])
                    P.op("dve", lambda e, half=half, b7=b7: e.tensor_copy(self.hTf[:, half * 4:(half + 1) * 4, :], b7.ap[:, :].rearrange("p (k t) -> p k t", k=4)),
                         reads=[b7.res], writes=[self.r_hTf])
                for kc in range(8):
                    P.op("pe", lambda e, kc=kc, bank=bank: e.matmul(bank.ap[:, 0:8], self.hTf[:, kc, :], wrT[:, kc, :], start=(kc == 0), stop=(kc == 7)),
                         reads=[self.r_hTf, r_wr], writes=[bank.res])
                P.op("dve", lambda e, i=i, bank=bank: e.tensor_copy(self.lg[:, i, :], bank.ap[:, 0:8]), reads=[bank.res], writes=[self.r_lg[tg]])
        P.dma("sp", self.hTv[:, :, tg * 512:(tg + 1) * 512], hgp[:], reads=[self.r_hgp], writes=[C.r_hT[tg]])
        if self.router:
            tokgate, r_tgl = self.router["tokgate"]
            lg = self.lg[:, tg * 4:(tg + 1) * 4, :]
            m1, l2, ex, mx, r_a = self.m1, self.l2, self.ex, self.mx, self.r_a
            P.op("dve", lambda e, lg=lg: e.tensor_reduce(mx[:, :, 0], lg, AX.X, ALU.max), reads=[self.r_lg[tg]], writes=[r_a])
            P.op("dve", lambda e, lg=lg: e.tensor_tensor(m1[:], lg, fbc(mx[:, :, 0], 1, 8), ALU.is_equal), reads=[r_a, self.r_lg[tg]], writes=[r_a])
            P.op("dve", lambda e, lg=lg: e.scalar_tensor_tensor(l2[:], m1[:], -1e30, lg, ALU.mult, ALU.add), reads=[r_a], writes=[r_a])
            P.op("dve", lambda e: e.tensor_reduce(mx[:, :, 1], l2[:], AX.X, ALU.max), reads=[r_a], writes=[r_a])
            P.op("dve", lambda e, lg=lg: e.tensor_tensor(m1[:], lg, fbc(mx[:, :, 1], 1, 8), ALU.is_ge), reads=[r_a], writes=[r_a])
            P.op("dve", lambda e, lg=lg: e.tensor_tensor(l2[:], lg, fbc(mx[:, :, 0], 1, 8), ALU.subtract), reads=[r_a], writes=[r_a])
            P.op("act", lambda e: e.activation(ex[:], l2[:], AF.Exp), reads=[r_a], writes=[r_a])
            P.op("dve", lambda e: e.tensor_tensor(ex[:], ex[:], m1[:], ALU.mult), reads=[r_a], writes=[r_a])
            P.op("dve", lambda e: e.tensor_reduce(mx[:, :, 2], ex[:], AX.X, ALU.add), reads=[r_a], writes=[r_a])
            P.op("dve", lambda e: e.reciprocal(mx[:, :, 3], mx[:, :, 2]), reads=[r_a], writes=[r_a])
            P.op("dve", lambda e, tg=tg: e.tensor_tensor(tokgate[:, tg * 4:(tg + 1) * 4, :], ex[:], fbc(mx[:, :, 3], 1, 8), ALU.mult), reads=[r_a], writes=[r_tgl[tg]])


def prenorm_stage(C, x_d, hT_d, router=None):
    P = C.P
    with ExitStack() as st:
        xt = [P.sbuf("pn_x%d" % i, [128, 1024], F32, st) for i in range(2)]
        sq = P.sbuf("pn_sq", [128, 1024], F32, st)
        t1 = [P.sbuf("pn_t%d" % i, [128, 1024], F32, st) for i in range(2)]
        hb = [P.sbuf("pn_hb%d" % i, [128, 1024], BF16, st) for i in range(2)]
        hg = [P.sbuf("pn_hg%d" % i, [128, 8, 512], BF16, st) for i in range(2)]
        ss = P.sbuf("pn_ss", [128, 64], F32, st)
        r_x = [Res(), Res()]
        r_sq = Res()
        r_t1 = [Res(), Res()]
        r_hb = [Res(), Res()]
        r_hg = [Res(), Res()]
        r_ss = [Res() for _ in range(32)]
        if router is not None:
            wr, r_wr, tokgate, r_tg = router
            lg = P.sbuf("pn_lg", [128, 32, 8], F32, st)
            junk = P.sbuf("pn_junk", [128, 1024], F32, st)
            r_lg = [Res() for _ in range(32)]
            r_junk = Res()
        hTv = hT_d.rearrange("kc p t -> p kc t")
        for i in range(NT):
            b = i % 2
            g = (i // 4) % 2
            P.dma("sp", xt[b][:], x_d[i * 128:(i + 1) * 128, :], writes=[r_x[b]])
            P.op("act", lambda e, b=b, i=i: e.activation(sq[:], xt[b][:], AF.Square, accum_out=ss[:, 2 * i:2 * i + 1]),
                 reads=[r_x[b]], writes=[r_sq, r_ss[i]])
            P.op("act", lambda e, i=i: e.activation(ss[:, 2 * i + 1:2 * i + 2], ss[:, 2 * i:2 * i + 1], AF.Sqrt, bias=C.epsc[:, 0:1], scale=1.0 / D),
                 reads=[r_ss[i], C.r_eps], writes=[r_ss[i]])
            P.op("dve", lambda e, i=i: e.reciprocal(ss[:, 2 * i:2 * i + 1], ss[:, 2 * i + 1:2 * i + 2]),
                 reads=[r_ss[i]], writes=[r_ss[i]])
            P.op("dve", lambda e, b=b, i=i: e.scalar_tensor_tensor(t1[b][:], xt[b][:], ss[:, 2 * i:2 * i + 1], C.gs[:], ALU.mult, ALU.mult),
                 reads=[r_x[b], r_ss[i], C.r_gs], writes=[r_t1[b]])
            if router is None:
                P.op("pool", lambda e, b=b: e.tensor_tensor(hb[b][:], t1[b][:], C.shift[:], ALU.add),
                     reads=[r_t1[b], C.r_shift], writes=[r_hb[b]])
            else:
                P.op("pool", lambda e, b=b: e.tensor_tensor(t1[b][:], t1[b][:], C.shift[:], ALU.add),
                     reads=[r_t1[b], C.r_shift], writes=[r_t1[b]])
                P.op("act", lambda e, b=b: e.copy(hb[b][:], t1[b][:]), reads=[r_t1[b]], writes=[r_hb[b]])
                for ex in range(8):
                    P.op("dve", lambda e, b=b, ex=ex, i=i: e.scalar_tensor_tensor(
                        junk[:], t1[b][:], 1.0, wr[:, ex, :], ALU.mult, ALU.mult, accum_out=lg[:, i, ex:ex + 1]),
                        reads=[r_t1[b], r_wr], writes=[r_junk, r_lg[i]])
            bank = C.bank(6 + (i % 2))
            pv = bank.ap.bitcast(BF16)
            for kc in range(8):
                P.op("pe", lambda e, b=b, kc=kc, pv=pv: e.transpose(pv[:, kc * 128:(kc + 1) * 128], hb[b][:, kc * 128:(kc + 1) * 128], C.identb[:]),
                     reads=[r_hb[b], C.r_ident], writes=[bank.res])
            j = i % 4
            P.op("act", lambda e, g=g, j=j, pv=pv: e.copy(hg[g][:, :, j * 128:(j + 1) * 128], pv.rearrange("p (kc t) -> p kc t", kc=8)),
                 reads=[bank.res], writes=[r_hg[g]])
            if j == 3:
                tg = i // 4
                P.dma("sp", hTv[:, :, tg * 512:(tg + 1) * 512], hg[g][:], reads=[r_hg[g]], writes=[C.r_hT[tg]])
        if router is not None:
            m1 = P.sbuf("pn_m1", [128, 32, 8], F32, st)
            l2 = P.sbuf("pn_l2", [128, 32, 8], F32, st)
            ex = P.sbuf("pn_ex", [128, 32, 8], F32, st)
            mx = P.sbuf("pn_mx", [128, 32, 4], F32, st)
            r_a = Res()
            allr = r_lg
            P.op("dve", lambda e: e.tensor_reduce(mx[:, :, 0], lg[:], AX.X, ALU.max), reads=allr, writes=[r_a])
            P.op("dve", lambda e: e.tensor_tensor(m1[:], lg[:], fbc(mx[:, :, 0], 1, 8), ALU.is_equal), reads=[r_a] + allr, writes=[r_a])
            P.op("dve", lambda e: e.scalar_tensor_tensor(l2[:], m1[:], -1e30, lg[:], ALU.mult, ALU.add), reads=[r_a], writes=[r_a])
            P.op("dve", lambda e: e.tensor_reduce(mx[:, :, 1], l2[:], AX.X, ALU.max), reads=[r_a], writes=[r_a])
            P.op("dve", lambda e: e.tensor_tensor(m1[:], lg[:], fbc(mx[:, :, 1], 1, 8), ALU.is_ge), reads=[r_a], writes=[r_a])
            P.op("dve", lambda e: e.tensor_tensor(l2[:], lg[:], fbc(mx[:, :, 0], 1, 8), ALU.subtract), reads=[r_a], writes=[r_a])
            P.op("act", lambda e: e.activation(ex[:], l2[:], AF.Exp), reads=[r_a], writes=[r_a])
            P.op("dve", lambda e: e.tensor_tensor(ex[:], ex[:], m1[:], ALU.mult), reads=[r_a], writes=[r_a])
            P.op("dve", lambda e: e.tensor_reduce(mx[:, :, 2], ex[:], AX.X, ALU.add), reads=[r_a], writes=[r_a])
            P.op("dve", lambda e: e.reciprocal(mx[:, :, 3], mx[:, :, 2]), reads=[r_a], writes=[r_a])
            P.op("dve", lambda e: e.tensor_tensor(tokgate[:], ex[:], fbc(mx[:, :, 3], 1, 8), ALU.mult), reads=[r_a], writes=[r_tg])
            C.fence([r_a, r_junk] + r_lg)
        C.fence(r_x + [r_sq] + r_t1 + r_hb + r_hg + r_ss)


def ffn_passes(C, hT_d, passes, yacc_d, tokgate=None, pn_args=None, FM=1024):
    P = C.P
    with ExitStack() as st:
        pn = Prenorm(C, pn_args[0], hT_d, st, router=pn_args[1]) if pn_args is not None else None
        W1 = [P.sbuf("ff_w1_%d" % i, [128, 8, FM], BF16, st) for i in range(2)]
        W3 = [P.sbuf("ff_w3_%d" % i, [128, 8, FM], BF16, st) for i in range(2)]
        W2 = [P.sbuf("ff_w2_%d" % i, [128, FM // 128, 1024], BF16, st) for i in range(2)]
        hg = [P.sbuf("ff_hg%d" % i, [128, 8, 512], BF16, st) for i in range(2)]
        gT = [P.sbuf("ff_gT%d" % i, [128, FM // 128, 512], BF16, st) for i in range(2)]
        sS = [P.sbuf("ff_s%d" % i, [128, 512], F32, st) for i in range(2)]
        yS = [P.sbuf("ff_y%d" % i, [128, 1024], F32, st) for i in range(2)]
        r_W = [[Res(), Res(), Res()] for _ in range(2)]
        r_hg = [Res(), Res()]
        r_gT = [Res(), Res()]
        r_s = [Res(), Res()]
        r_y = [Res(), Res()]
        hTv = hT_d.rearrange("kc p t -> p kc t")
        def w_tasks(pi):
            w1a, w3a, w2a, ex = passes[pi]
            F = w1a.shape[1]
            s = pi % 2
            tasks = []
            for kc in range(8):
                tasks.append((W1[s][:, kc, 0:F], w1a[kc * 128:(kc + 1) * 128, :], r_W[s][0]))
            for kc in range(8):
                tasks.append((W3[s][:, kc, 0:F], w3a[kc * 128:(kc + 1) * 128, :], r_W[s][1]))
            for fc in range(F // 128):
                tasks.append((W2[s][:, fc, :], w2a[fc * 128:(fc + 1) * 128, :], r_W[s][2]))
            return tasks

        for t_ in w_tasks(0):
            C.wload(*t_)
        state = {}
        cn = {"cnt": 0, "ycnt": 0, "gcnt": 0}

        def p1(pi, tg):
            w1a, w3a, w2a, ex = passes[pi]
            nf = w1a.shape[1] // 128
            s = pi % 2
            hb = cn["gcnt"] % 2
            cn["gcnt"] += 1
            state[(pi, tg)] = hb
            P.dma("sp", hg[hb][:], hTv[:, :, tg * 512:(tg + 1) * 512], reads=[C.r_hT[tg]], writes=[r_hg[hb]])
            if tg >= 1 and pi + 1 < len(passes):
                for t_ in w_tasks(pi + 1)[(tg - 1) * 4:tg * 4]:
                    C.wload(*t_)
            for fc in range(nf):
                k = cn["cnt"] % 2
                cn["cnt"] += 1
                ba, bb_ = C.bank(k), C.bank(2 + k)
                for kc in range(8):
                    P.op("pe", lambda e, ba=ba, s=s, kc=kc, fc=fc, hb=hb: e.matmul(
                        ba.ap[:, :], W1[s][:, kc, fc * 128:(fc + 1) * 128], hg[hb][:, kc, :], start=(kc == 0), stop=(kc == 7)),
                        reads=[r_W[s][0], r_hg[hb]], writes=[ba.res])
                for kc in range(8):
                    P.op("pe", lambda e, bb_=bb_, s=s, kc=kc, fc=fc, hb=hb: e.matmul(
                        bb_.ap[:, :], W3[s][:, kc, fc * 128:(fc + 1) * 128], hg[hb][:, kc, :], start=(kc == 0), stop=(kc == 7)),
                        reads=[r_W[s][1], r_hg[hb]], writes=[bb_.res])
                P.op("act", lambda e, k=k, ba=ba: e.activation(sS[k][:], ba.ap[:, :], AF.Silu), reads=[ba.res], writes=[r_s[k]])
                P.op("dve", lambda e, k=k, bb_=bb_, hb=hb, fc=fc: e.tensor_tensor(gT[hb][:, fc, :], sS[k][:], bb_.ap[:, :], ALU.mult),
                     reads=[r_s[k], bb_.res], writes=[r_gT[hb]])

        def p2(pi, tg):
            w1a, w3a, w2a, ex = passes[pi]
            nf = w1a.shape[1] // 128
            s = pi % 2
            hb = state[(pi, tg)]
            for j in range(4):
                tile = tg * 4 + j
                yb = cn["ycnt"] % 2
                cn["ycnt"] += 1
                for half in range(2):
                    bo = C.bank(4 + half)
                    for fc in range(nf):
                        P.op("pe", lambda e, bo=bo, hb=hb, fc=fc, j=j, s=s, half=half, nf=nf: e.matmul(
                            bo.ap[:, :], gT[hb][:, fc, j * 128:(j + 1) * 128], W2[s][:, fc, half * 512:(half + 1) * 512],
                            start=(fc == 0), stop=(fc == nf - 1)),
                            reads=[r_gT[hb], r_W[s][2]], writes=[bo.res])
                    if ex is None:
                        P.op("act", lambda e, yb=yb, bo=bo, half=half: e.copy(yS[yb][:, half * 512:(half + 1) * 512], bo.ap[:, :]),
                             reads=[bo.res], writes=[r_y[yb]])
                    else:
                        P.op("act", lambda e, yb=yb, bo=bo, half=half, tile=tile, ex=ex: e.activation(
                            yS[yb][:, half * 512:(half + 1) * 512], bo.ap[:, :], AF.Copy, scale=tokgate[0][:, tile, ex:ex + 1]),
                            reads=[bo.res, tokgate[1][tile // 4]], writes=[r_y[yb]])
                if pi == 0:
                    P.dma("sp", yacc_d[tile * 128:(tile + 1) * 128, :], yS[yb][:], reads=[r_y[yb]], writes=[C.r_yacc[tile]])
                else:
                    P.dma("pool", yacc_d[tile * 128:(tile + 1) * 128, :], yS[yb][:], reads=[r_y[yb]], writes=[C.r_yacc[tile]],
                          accum_op=ALU.add)

        seq = [(pi, tg) for pi in range(len(passes)) for tg in range(8)]
        if pn is not None:
            pn.front(0)
            pn.back(0)
            pn.front(1)
        p1(*seq[0])
        if pn is not None:
            pn.back(1)
        for k, cur in enumerate(seq):
            nx = seq[k + 1] if k + 1 < len(seq) else None
            pnx = pn is not None and nx is not None and nx[0] == 0 and nx[1] + 1 < 8
            if pnx:
                pn.front(nx[1] + 1)
            if nx is not None:
                p1(*nx)
            p2(*cur)
            if pnx:
                pn.back(nx[1] + 1)
        fl = r_hg + r_gT + r_s + r_y
        for a in r_W:
            fl += a
        C.fence(fl)


def combine_stage(C, xin_d, yacc_d, xout_d, final_g=None):
    P = C.P
    with ExitStack() as st:
        xt = [P.sbuf("cb_x%d" % i, [128, 1024], F32, st) for i in range(2)]
        yt = [P.sbuf("cb_y%d" % i, [128, 1024], F32, st) for i in range(2)]
        r_x = [Res(), Res()]
        r_y = [Res(), Res()]
        if final_g is not None:
            fg = P.sbuf("cb_fg", [128, 1024], F32, st)
            sq = P.sbuf("cb_sq", [128, 1024], F32, st)
            ss = P.sbuf("cb_ss", [128, 64], F32, st)
            r_fg, r_sq = Res(), Res()
            r_ss = [Res() for _ in range(32)]
            P.dma("sp", fg[:], bass.AP(final_g.tensor, final_g.offset, [[0, 128], [1, 1024]]), writes=[r_fg])
        for i in range(NT):
            b = i % 2
            P.dma("sp", xt[b][:], xin_d[i * 128:(i + 1) * 128, :], reads=[C.r_xin[i]] if C.r_xin else [], writes=[r_x[b]])
            P.dma("sp", yt[b][:], yacc_d[i * 128:(i + 1) * 128, :], reads=[C.r_yacc[i]], writes=[r_y[b]])
            P.op("pool", lambda e, b=b: e.tensor_tensor(yt[b][:], yt[b][:], C.gate[:], ALU.mult), reads=[r_y[b], C.r_gate], writes=[r_y[b]])
            P.op("dve", lambda e, b=b: e.tensor_tensor(xt[b][:], xt[b][:], yt[b][:], ALU.add), reads=[r_y[b], r_x[b]], writes=[r_x[b]])
            if final_g is not None:
                P.op("act", lambda e, b=b, i=i: e.activation(sq[:], xt[b][:], AF.Square, accum_out=ss[:, 2 * i:2 * i + 1]),
                     reads=[r_x[b]], writes=[r_sq, r_ss[i]])
                P.op("act", lambda e, i=i: e.activation(ss[:, 2 * i + 1:2 * i + 2], ss[:, 2 * i:2 * i + 1], AF.Sqrt, bias=C.epsc[:, 0:1], scale=1.0 / D),
                     reads=[r_ss[i], C.r_eps], writes=[r_ss[i]])
                P.op("dve", lambda e, i=i: e.reciprocal(ss[:, 2 * i:2 * i + 1], ss[:, 2 * i + 1:2 * i + 2]),
                     reads=[r_ss[i]], writes=[r_ss[i]])
                P.op("dve", lambda e, b=b, i=i: e.scalar_tensor_tensor(xt[b][:], xt[b][:], ss[:, 2 * i:2 * i + 1], fg[:], ALU.mult, ALU.mult),
                     reads=[r_x[b], r_ss[i], r_fg], writes=[r_x[b]])
            tok = P.dma("sp", xout_d[i * 128:(i + 1) * 128, :], xt[b][:], reads=[r_x[b]], writes=[C.r_xout[i]])
            C.final.append(tok)
        fl = r_x + r_y
        if final_g is not None:
            fl += [r_fg, r_sq] + r_ss
        C.fence(fl)


def dsa_stage(C, x_d, xout_d):
    P, Dm = C.P, C.D
    KSC = 0.125 / (8.0 ** 0.5)
    with ExitStack() as st:
        CKV_tm = P.sbuf("d_ckvtm", [128, 32, 128], BF16, st)
        CKV_T = P.sbuf("d_ckvT", [128, T], BF16, st)
        IK_T2 = P.sbuf("d_ikT2", [128, T], BF16, st)
        KR_T = P.sbuf("d_krT", [16, T], BF16, st)
        QLN_T = P.sbuf("d_qlnT", [128, 2, T], BF16, st)
        iwabs = P.sbuf("d_iwabs", [128, 32, 8], F32, st)
        iwsgn = P.sbuf("d_iwsgn", [128, 32, 8], F32, st)
        Win = P.sbuf("d_win", [128, 8, 472], BF16, st)
        Wnope = P.sbuf("d_wnope", [128, 2, 768], BF16, st)
        Wrope = P.sbuf("d_wrope", [128, 2, 256], BF16, st)
        Wrsw = P.sbuf("d_wrsw", [128, 2, 256], BF16, st)
        WukT = P.sbuf("d_wukT", [48, 16, 128], BF16, st)
        Wuv2 = P.sbuf("d_wuv2", [128, 16, 128], BF16, st)
        Wiq = P.sbuf("d_wiq", [128, 2, 512], BF16, st)
        Wiqs = P.sbuf("d_wiqs", [128, 2, 512], BF16, st)
        Wo = P.sbuf("d_wo", [128, 8, 1024], BF16, st)
        Ctm = P.sbuf("d_ctm", [128, 32, 16], F32, st)
        Stm = P.sbuf("d_stm", [128, 32, 16], F32, st)
        gq = P.sbuf("d_gq", [128, 256], F32, st)
        gkv = P.sbuf("d_gkv", [128, 128], F32, st)
        onesb = P.sbuf("d_ones", [128, 128], BF16, st)
        r_w = Res()
        r_K = Res()
        r_iw = Res()
        P.op("pool", lambda e: e.memset(onesb[:], 1.0), writes=[r_w])
        for kc in range(8):
            C.wload(Win[:, kc, :], Dm["dsa_w_in"][kc * 128:(kc + 1) * 128, :], r_w)
            C.wload(Wo[:, kc, :], Dm["dsa_w_o"][kc * 128:(kc + 1) * 128, :], r_w)
        for k2 in range(2):
            C.wload(Wnope[:, k2, :], Dm["dsa_wuq_nope"][k2 * 128:(k2 + 1) * 128, :], r_w)
            C.wload(Wrope[:, k2, :], Dm["dsa_wuq_rope"][k2 * 128:(k2 + 1) * 128, :], r_w)
            C.wload(Wrsw[:, k2, :], Dm["dsa_wuq_rsw"][k2 * 128:(k2 + 1) * 128, :], r_w)
            C.wload(Wiq[:, k2, :], Dm["dsa_wiq"][k2 * 128:(k2 + 1) * 128, :], r_w)
            C.wload(Wiqs[:, k2, :], Dm["dsa_wiq_sw"][k2 * 128:(k2 + 1) * 128, :], r_w)
        P.dma("pool", WukT[:], Dm["dsa_wukT"], writes=[r_w])
        P.dma("pool", Wuv2[:], Dm["dsa_wuv2"], writes=[r_w])
        P.dma("sp", Ctm[:], Dm["rope_ctm"].rearrange("(i p) r -> p i r", p=128), writes=[r_w])
        P.dma("sp", Stm[:], Dm["rope_stm"].rearrange("(i p) r -> p i r", p=128), writes=[r_w])
        P.dma("sp", gq[:], bass.AP(Dm["dsa_g_q"].tensor, 0, [[0, 128], [1, 256]]), writes=[r_w])
        P.dma("sp", gkv[:], bass.AP(Dm["dsa_g_kv"].tensor, 0, [[0, 128], [1, 128]]), writes=[r_w])

        with ExitStack() as sa:
            hg = [P.sbuf("da_hg%d" % i, [128, 8, 512], BF16, sa) for i in range(2)]
            pj = [P.sbuf("da_pj%d" % i, [128, 472], F32, sa) for i in range(2)]
            jk = P.sbuf("da_jk", [128, 256], F32, sa)
            stt = P.sbuf("da_st", [128, 32, 4], F32, sa)
            qln = [P.sbuf("da_qln%d" % i, [128, 256], BF16, sa) for i in range(2)]
            ik2 = [P.sbuf("da_ik2%d" % i, [128, 128], BF16, sa) for i in range(2)]
            krb = [P.sbuf("da_kr%d" % i, [128, 16], BF16, sa) for i in range(2)]
            tr = [P.sbuf("da_tr%d" % i, [128, 4, 16], F32, sa) for i in range(2)]
            r_hg = [Res(), Res()]
            r_pj = [Res(), Res()]
            r_jk = Res()
            r_st = [Res() for _ in range(32)]
            r_q = [Res(), Res()]
            r_ik = [Res(), Res()]
            r_kr = [Res(), Res()]
            r_tr = [Res(), Res()]
            hTv = Dm["hT"].rearrange("kc p t -> p kc t")
            pn = Prenorm(C, x_d, Dm["hT"], sa)
            pn.front(0)
            pn.back(0)
            for i in range(NT):
                b = i % 2
                tg, j = i // 4, i % 4
                g = tg % 2
                if j == 0:
                    if tg + 1 < 8:
                        pn.front(tg + 1)
                    P.dma("sp", hg[g][:], hTv[:, :, tg * 512:(tg + 1) * 512], reads=[C.r_hT[tg]], writes=[r_hg[g]])
                bank = C.bank(6 + b)
                for kc in range(8):
                    P.op("pe", lambda e, bank=bank, g=g, kc=kc, j=j: e.matmul(bank.ap[:, 0:472], hg[g][:, kc, j * 128:(j + 1) * 128], Win[:, kc, :],
                                                                           start=(kc == 0), stop=(kc == 7)),
                         reads=[r_hg[g], r_w], writes=[bank.res])
                P.op("act", lambda e, b=b, bank=bank: e.copy(pj[b][:], bank.ap[:, 0:472]), reads=[bank.res], writes=[r_pj[b]])
                P.op("act", lambda e, b=b, i=i: e.activation(jk[:, 0:256], pj[b][:, 0:256], AF.Square, accum_out=stt[:, i, 0:1]),
                     reads=[r_pj[b]], writes=[r_jk, r_st[i]])
                P.op("act", lambda e, b=b, i=i: e.activation(jk[:, 0:128], pj[b][:, 256:384], AF.Square, accum_out=stt[:, i, 1:2]),
                     reads=[r_pj[b]], writes=[r_jk, r_st[i]])
                P.op("act", lambda e, i=i: e.activation(stt[:, i, 2:3], stt[:, i, 0:1], AF.Sqrt, bias=C.epsc[:, 0:1], scale=1.0 / 256),
                     reads=[r_st[i], C.r_eps], writes=[r_st[i]])
                P.op("act", lambda e, i=i: e.activation(stt[:, i, 3:4], stt[:, i, 1:2], AF.Sqrt, bias=C.epsc[:, 0:1], scale=1.0 / 128),
                     reads=[r_st[i], C.r_eps], writes=[r_st[i]])
                P.op("dve", lambda e, i=i: e.reciprocal(stt[:, i, 0:2], stt[:, i, 2:4]), reads=[r_st[i]], writes=[r_st[i]])
                P.op("dve", lambda e, b=b, i=i: e.scalar_tensor_tensor(qln[b][:], pj[b][:, 0:256], stt[:, i, 0:1], gq[:], ALU.mult, ALU.mult),
                     reads=[r_pj[b], r_st[i], r_w], writes=[r_q[b]])
                P.op("dve", lambda e, b=b, i=i: e.scalar_tensor_tensor(CKV_tm[:, i, :], pj[b][:, 256:384], stt[:, i, 1:2], gkv[:], ALU.mult, ALU.mult),
                     reads=[r_pj[b], r_st[i], r_w], writes=[r_K])
                for (c0, which) in [(384, 0), (400, 1)]:
                    P.op("pool", lambda e, b=b, i=i, c0=c0, which=which: e.tensor_tensor(tr[b][:, 2 * which, :], pj[b][:, c0:c0 + 16], Ctm[:, i, :], ALU.mult),
                         reads=[r_pj[b], r_w], writes=[r_tr[b]])
                    P.op("pool", lambda e, b=b, i=i, c0=c0, which=which: e.tensor_tensor(tr[b][:, 2 * which + 1, 0:8], pj[b][:, c0 + 8:c0 + 16], Stm[:, i, 0:8], ALU.mult),
                         reads=[r_pj[b], r_w], writes=[r_tr[b]])
                    P.op("pool", lambda e, b=b, i=i, c0=c0, which=which: e.tensor_tensor(tr[b][:, 2 * which + 1, 8:16], pj[b][:, c0:c0 + 8], Stm[:, i, 8:16], ALU.mult),
                         reads=[r_pj[b], r_w], writes=[r_tr[b]])
                P.op("dve", lambda e, b=b: e.tensor_tensor(krb[b][:], tr[b][:, 0, :], tr[b][:, 1, :], ALU.add), reads=[r_tr[b]], writes=[r_kr[b]])
                P.op("dve", lambda e, b=b: e.tensor_tensor(ik2[b][:, 0:16], tr[b][:, 2, :], tr[b][:, 3, :], ALU.add), reads=[r_tr[b]], writes=[r_ik[b]])
                P.op("act", lambda e, b=b: e.copy(ik2[b][:, 16:64], pj[b][:, 416:464]), reads=[r_pj[b]], writes=[r_ik[b]])
                P.op("pool", lambda e, b=b: e.tensor_copy(ik2[b][:, 64:128], ik2[b][:, 0:64]), reads=[r_ik[b]], writes=[r_ik[b]])
                P.op("act", lambda e, b=b, i=i: e.activation(iwsgn[:, i, :], pj[b][:, 464:472], AF.Sign), reads=[r_pj[b]], writes=[r_iw])
                P.op("dve", lambda e, b=b, i=i: e.scalar_tensor_tensor(iwabs[:, i, :], pj[b][:, 464:472], KSC, iwsgn[:, i, :], ALU.mult, ALU.mult),
                     reads=[r_pj[b], r_iw], writes=[r_iw])
                bank2 = C.bank(4 + b)
                pv = bank2.ap.bitcast(BF16)
                srcs = [(qln[b][:, 0:128], r_q[b], 128), (qln[b][:, 128:256], r_q[b], 128), (CKV_tm[:, i, :], r_K, 128),
                        (ik2[b][:], r_ik[b], 128), (krb[b][:], r_kr[b], 16)]
                for k, (src, rs, n) in enumerate(srcs):
                    P.op("pe", lambda e, pv=pv, k=k, src=src, n=n: e.transpose(pv[0:n, k * 128:(k + 1) * 128], src, C.identb[:]),
                         reads=[rs, C.r_ident], writes=[bank2.res])
                cs = slice(i * 128, (i + 1) * 128)
                P.op("act", lambda e, pv=pv, cs=cs: e.copy(QLN_T[:, :, cs], pv[:, 0:256].rearrange("p (k t) -> p k t", k=2)), reads=[bank2.res], writes=[r_K])
                P.op("dve", lambda e, pv=pv, cs=cs: e.tensor_copy(CKV_T[:, cs], pv[:, 256:384]), reads=[bank2.res], writes=[r_K])
                P.op("act", lambda e, pv=pv, cs=cs: e.copy(IK_T2[:, cs], pv[:, 384:512]), reads=[bank2.res], writes=[r_K])
                P.op("dve", lambda e, pv=pv, cs=cs: e.tensor_copy(KR_T[:, cs], pv[0:16, 512:640]), reads=[bank2.res], writes=[r_K])
                if j == 3 and tg + 1 < 8:
                    pn.back(tg + 1)
            P.barrier()

        SC = P.sbuf("d_sc", [128, T], F32, st)
        MK = P.sbuf("d_mk", [128, T], BF16, st)
        MT = P.sbuf("d_mt", [128, 32, 128], BF16, st)
        Rr = [P.sbuf("d_r%d" % i, [128, 512], F32, st) for i in range(2)]
        NE = 4
        Eb = [P.sbuf("d_e%d" % i, [128, 512], BF16, st) for i in range(NE)]
        rec = P.sbuf("d_rec", [128, 512], F32, st)
        ON = P.sbuf("d_on", [128, 16, 128], BF16, st)
        QABS = [P.sbuf("d_qabs%d" % i, [128, 16, 128], BF16, st) for i in range(2)]
        QN = P.sbuf("d_qn", [48, 16, 128], BF16, st)
        QR = [P.sbuf("d_qr%d" % i, [16, 16, 128], BF16, st) for i in range(2)]
        IQ = P.sbuf("d_iq", [128, 4, 128], BF16, st)
        OV = P.sbuf("d_ov", [128, 8, 128], BF16, st)
        Cq = P.sbuf("d_cq", [128, 128], F32, st)
        Sq = P.sbuf("d_sq", [128, 128], F32, st)
        tq = [P.sbuf("d_tq%d" % i, [128, 512], F32, st) for i in range(2)]
        xs = P.sbuf("d_xs", [128, 1024], F32, st)
        ys = P.sbuf("d_ys", [128, 1024], F32, st)
        bs = P.sbuf("d_bs", [128, 8], F32, st)
        thrneg = P.sbuf("d_thrneg", [128, 1], F32, st)
        identN = P.sbuf("d_identN", [128, 128], BF16, st)
        r_SC, r_MK, r_MT, r_rec, r_ON, r_QN, r_IQ, r_OV, r_cs, r_xs, r_ys, r_bs = [Res() for _ in range(12)]
        r_QABS = [Res(), Res()]
        r_QR = [Res(), Res()]
        r_R = [Res(), Res()]
        r_E = [Res() for _ in range(NE)]
        r_tq = [Res(), Res()]
        r_thrneg = Res()
        r_idn = Res()
        P.op("pool", lambda e: e.memset(thrneg[:], -1e29), writes=[r_thrneg])
        P.op("act", lambda e: e.activation(identN[:], C.identb[:], AF.Copy, scale=30000.0), reads=[C.r_ident], writes=[r_idn])
        SB = [C.bank(0), C.bank(1), C.bank(4), C.bank(5)]
        st_ = {"ecnt": 0, "scnt": 0}

        def stage_b1(qt):
            qs = slice(qt * 128, (qt + 1) * 128)
            qp = qt % 2
            P.dma("sp", Cq[:], Dm["rope_cfm"][:, qs], writes=[r_cs])
            P.dma("sp", Sq[:], Dm["rope_sfm"][:, qs], writes=[r_cs])
            BB = [C.bank(0), C.bank(1), C.bank(4), C.bank(5)]
            for hq in range(4):
                bank = BB[hq]
                for hh in range(4):
                    h = hq * 4 + hh
                    for k2 in range(2):
                        P.op("pe", lambda e, bank=bank, hh=hh, h=h, k2=k2, qs=qs: e.matmul(
                            bank.ap[0:48, hh * 128:(hh + 1) * 128], Wnope[:, k2, h * 48:(h + 1) * 48], QLN_T[:, k2, qs], start=(k2 == 0), stop=(k2 == 1)),
                            reads=[r_w, r_K], writes=[bank.res])
                P.op("act", lambda e, bank=bank, hq=hq: e.copy(QN[:, hq * 4:(hq + 1) * 4, :], bank.ap[0:48, :].rearrange("p (h t) -> p h t", h=4)),
                     reads=[bank.res], writes=[r_QN])
            for hq in range(4):
                bank = BB[(hq + 2) % 4]
                for hh in range(4):
                    h = hq * 4 + hh
                    P.op("pe", lambda e, bank=bank, hh=hh, h=h: e.matmul(bank.ap[:, hh * 128:(hh + 1) * 128], WukT[:, h, :], QN[:, h, :], start=True, stop=True),
                         reads=[r_w, r_QN], writes=[bank.res])
                P.op("act", lambda e, bank=bank, hq=hq, qp=qp: e.copy(QABS[qp][:, hq * 4:(hq + 1) * 4, :], bank.ap[:, :].rearrange("p (h t) -> p h t", h=4)),
                     reads=[bank.res], writes=[r_QABS[qp]])
            for hq in range(4):
                b0, b1 = (C.bank(6), C.bank(7)) if hq % 2 == 0 else (C.bank(0), C.bank(1))
                for (bank, Wt) in [(b0, Wrope), (b1, Wrsw)]:
                    for hh in range(4):
                        h = hq * 4 + hh
                        for k2 in range(2):
                            P.op("pe", lambda e, bank=bank, Wt=Wt, hh=hh, h=h, k2=k2, qs=qs: e.matmul(
                                bank.ap[0:16, hh * 128:(hh + 1) * 128], Wt[:, k2, h * 16:(h + 1) * 16], QLN_T[:, k2, qs], start=(k2 == 0), stop=(k2 == 1)),
                                reads=[r_w, r_K], writes=[bank.res])
                P.op("dve", lambda e, b0=b0: e.tensor_tensor(tq[0][0:16, :].rearrange("p (h t) -> p h t", h=4), b0.ap[0:16, :].rearrange("p (h t) -> p h t", h=4),
                                                             fbc(Cq[0:16, :], 0, 4), ALU.mult), reads=[b0.res, r_cs], writes=[r_tq[0]])
                P.op("dve", lambda e, b1=b1: e.tensor_tensor(tq[1][0:16, :].rearrange("p (h t) -> p h t", h=4), b1.ap[0:16, :].rearrange("p (h t) -> p h t", h=4),
                                                             fbc(Sq[0:16, :], 0, 4), ALU.mult), reads=[b1.res, r_cs], writes=[r_tq[1]])
                P.op("pool", lambda e, hq=hq, qp=qp: e.tensor_tensor(QR[qp][:, hq * 4:(hq + 1) * 4, :], tq[0][0:16, :].rearrange("p (h t) -> p h t", h=4),
                                                                     tq[1][0:16, :].rearrange("p (h t) -> p h t", h=4), ALU.add),
                     reads=r_tq, writes=[r_QR[qp]])
            b0, b1 = C.bank(4), C.bank(5)
            for (bank, Wt) in [(b0, Wiq), (b1, Wiqs)]:
                for ch in range(4):
                    for k2 in range(2):
                        P.op("pe", lambda e, bank=bank, Wt=Wt, ch=ch, k2=k2, qs=qs: e.matmul(
                            bank.ap[:, ch * 128:(ch + 1) * 128], Wt[:, k2, ch * 128:(ch + 1) * 128], QLN_T[:, k2, qs], start=(k2 == 0), stop=(k2 == 1)),
                            reads=[r_w, r_K], writes=[bank.res])
            P.op("dve", lambda e, b0=b0: e.tensor_tensor(tq[0][:].rearrange("p (h t) -> p h t", h=4), b0.ap[:, :].rearrange("p (h t) -> p h t", h=4),
                                                         fbc(Cq[:], 0, 4), ALU.mult), reads=[b0.res, r_cs], writes=[r_tq[0]])
            P.op("dve", lambda e, b1=b1: e.tensor_tensor(tq[1][:].rearrange("p (h t) -> p h t", h=4), b1.ap[:, :].rearrange("p (h t) -> p h t", h=4),
                                                         fbc(Sq[:], 0, 4), ALU.mult), reads=[b1.res, r_cs], writes=[r_tq[1]])
            P.op("pool", lambda e: e.tensor_tensor(IQ[:].rearrange("p h t -> p (h t)"), tq[0][:], tq[1][:], ALU.add), reads=r_tq, writes=[r_IQ])

        def stage_b2(qt):
            qs = slice(qt * 128, (qt + 1) * 128)
            nk = (qt + 1) * 128
            ng = (nk + 511) // 512
            for kg in range(ng):
                ncol = min(512, nk - kg * 512)
                ks = slice(kg * 512, kg * 512 + ncol)
                for h in range(8):
                    bank = C.bank(6 + (h % 2))
                    rb = st_["ecnt"] % 2
                    st_["ecnt"] += 1
                    pb = (h % 2) * 64
                    P.op("pe", lambda e, bank=bank, pb=pb, h=h, ks=ks, ncol=ncol: e.matmul(
                        bank.ap[:, 0:ncol], IQ[pb:pb + 64, h // 2, :], IK_T2[pb:pb + 64, ks], start=True, stop=True),
                        reads=[r_IQ, r_K], writes=[bank.res])
                    P.op("act", lambda e, bank=bank, rb=rb, ncol=ncol, qt=qt, h=h: e.activation(
                        Rr[rb][:, 0:ncol], bank.ap[:, 0:ncol], AF.Relu, scale=iwabs[:, qt, h:h + 1]),
                        reads=[bank.res, r_iw], writes=[r_R[rb]])
                    if h == 0:
                        P.op("dve", lambda e, rb=rb, ncol=ncol, ks=ks, qt=qt, h=h: e.tensor_scalar(
                            SC[:, ks], Rr[rb][:, 0:ncol], iwsgn[:, qt, h:h + 1], None, ALU.mult),
                            reads=[r_R[rb], r_iw], writes=[r_SC])
                    else:
                        P.op("dve", lambda e, rb=rb, ncol=ncol, ks=ks, qt=qt, h=h: e.scalar_tensor_tensor(
                            SC[:, ks], Rr[rb][:, 0:ncol], iwsgn[:, qt, h:h + 1], SC[:, ks], ALU.mult, ALU.add),
                            reads=[r_R[rb], r_iw, r_SC], writes=[r_SC])
            P.op("pool", lambda e, qs=qs: e.affine_select(SC[:, qs], SC[:, qs], [[-1, 128]], ALU.is_ge, -1e30, base=0, channel_multiplier=1),
                 reads=[r_SC], writes=[r_SC])
            if qt >= 2:
                P.op("dve", lambda e, nk=nk: e.tensor_reduce(bs[:, 0:1], SC[:, 0:nk], AX.X, ALU.max), reads=[r_SC], writes=[r_bs])
                P.op("dve", lambda e, qt=qt: e.tensor_reduce(bs[:, 1:2], SC[:, 0:qt * 128], AX.X, ALU.min), reads=[r_SC], writes=[r_bs])
                P.op("dve", lambda e: e.tensor_tensor(bs[:, 2:3], bs[:, 0:1], bs[:, 1:2], ALU.subtract), reads=[r_bs], writes=[r_bs])

        def stage_b4(qt, k0, k1):
            nk = (qt + 1) * 128
            if qt < 2:
                return
            for k in range(k0, k1):
                f = 2.0 ** (-k)
                P.op("dve", lambda e, f=f: e.scalar_tensor_tensor(bs[:, 3:4], bs[:, 2:3], f, bs[:, 1:2], ALU.mult, ALU.add), reads=[r_bs], writes=[r_bs])
                P.op("dve", lambda e, nk=nk: e.tensor_scalar(MK[:, 0:nk], SC[:, 0:nk], bs[:, 3:4], 0.0, ALU.is_ge, ALU.add, accum_out=bs[:, 4:5]),
                     reads=[r_SC, r_bs], writes=[r_MK, r_bs])
                P.op("dve", lambda e: e.scalar_tensor_tensor(bs[:, 5:6], bs[:, 4:5], 255.5, bs[:, 2:3], ALU.is_ge, ALU.mult), reads=[r_bs], writes=[r_bs])
                P.op("dve", lambda e, f=f: e.scalar_tensor_tensor(bs[:, 1:2], bs[:, 5:6], f, bs[:, 1:2], ALU.mult, ALU.add), reads=[r_bs], writes=[r_bs])

        def stage_b5(qt):
            nk = (qt + 1) * 128
            if qt >= 2:
                thr_ap, thr_res = bs[:, 1:2], r_bs
            else:
                thr_ap, thr_res = thrneg[:, 0:1], r_thrneg
            P.op("dve", lambda e, nk=nk, thr_ap=thr_ap: e.tensor_scalar(MK[:, 0:nk], SC[:, 0:nk], thr_ap, 1.0, ALU.is_ge, ALU.subtract),
                 reads=[r_SC, thr_res], writes=[r_MK])
            for k4 in range((qt + 4) // 4):
                bank = C.bank(6 + (k4 % 2))
                pv = bank.ap.bitcast(BF16)
                nn = min(4, qt + 1 - k4 * 4)
                for kk in range(nn):
                    kt = k4 * 4 + kk
                    P.op("pe", lambda e, pv=pv, kk=kk, kt=kt: e.transpose(pv[:, kk * 128:(kk + 1) * 128], MK[:, kt * 128:(kt + 1) * 128], C.identb[:]),
                         reads=[r_MK, C.r_ident], writes=[bank.res])
                P.op("act", lambda e, pv=pv, k4=k4, nn=nn: e.copy(MT[:, k4 * 4:k4 * 4 + nn, :], pv[:, 0:nn * 128].rearrange("p (k t) -> p k t", k=nn)),
                     reads=[bank.res], writes=[r_MT])

        def stage_b6_cg(qt, cg):
            qp = qt % 2
            bo, br = C.bank(2 + 4 * (cg % 2)), C.bank(3 + 4 * (cg % 2))
            qa = QABS[qp][:, cg * 4:(cg + 1) * 4, :].rearrange("p h t -> p (h t)")
            qr = QR[qp][:, cg * 4:(cg + 1) * 4, :].rearrange("p h t -> p (h t)")
            LA = 3
            slots = {}

            def emit_s(kt):
                i = st_["scnt"]
                st_["scnt"] += 1
                sb = SB[i % 4]
                eb = i % NE
                slots[kt] = (sb, eb)
                kss = slice(kt * 128, (kt + 1) * 128)
                P.op("pe", lambda e, sb=sb, kss=kss, qa=qa: e.matmul(sb.ap[:, :], CKV_T[:, kss], qa, start=True, stop=False),
                     reads=[r_K, r_QABS[qp]], writes=[sb.res])
                P.op("pe", lambda e, sb=sb, kss=kss, qr=qr: e.matmul(sb.ap[:, :], KR_T[:, kss], qr, start=False, stop=False),
                     reads=[r_K, r_QR[qp]], writes=[sb.res])
                P.op("pe", lambda e, sb=sb, kt=kt: e.matmul(sb.ap[:, :].rearrange("p (h t) -> p h t", h=4), identN[:], fbc(MT[:, kt, :], 0, 4), start=False, stop=True),
                     reads=[r_idn, r_MT], writes=[sb.res])
                P.op("act", lambda e, sb=sb, eb=eb: e.activation(Eb[eb][:], sb.ap[:, :], AF.Exp, scale=0.125), reads=[sb.res], writes=[r_E[eb]])

            for kt in range(min(LA, qt + 1)):
                emit_s(kt)
            for kt in range(qt + 1):
                sb, eb = slots[kt]
                P.op("pe", lambda e, bo=bo, kt=kt, eb=eb, qt=qt: e.matmul(bo.ap[:, :], CKV_tm[:, kt, :], Eb[eb][:], start=(kt == 0), stop=(kt == qt)),
                     reads=[r_K, r_E[eb]], writes=[bo.res])
                P.op("pe", lambda e, br=br, kt=kt, eb=eb, qt=qt: e.matmul(br.ap[:, :], onesb[:], Eb[eb][:], start=(kt == 0), stop=(kt == qt)),
                     reads=[r_w, r_E[eb]], writes=[br.res])
                if kt + LA <= qt:
                    emit_s(kt + LA)
            P.op("dve", lambda e, br=br: e.reciprocal(rec[:], br.ap[:, :]), reads=[br.res], writes=[r_rec])
            P.op("dve", lambda e, bo=bo, cg=cg: e.tensor_tensor(ON[:, cg * 4:(cg + 1) * 4, :].rearrange("p h t -> p (h t)"), bo.ap[:, :], rec[:], ALU.mult),
                 reads=[bo.res, r_rec], writes=[r_ON])

        def stage_b7(qt):
            qs = slice(qt * 128, (qt + 1) * 128)
            P.dma("sp", xs[:], x_d[qs, :], writes=[r_xs])
            for hf in range(2):
                bank = C.bank(6 + hf)
                for jj in range(4):
                    j = hf * 4 + jj
                    for u in range(2):
                        h = 2 * j + u
                        P.op("pe", lambda e, bank=bank, jj=jj, h=h, u=u: e.matmul(bank.ap[:, jj * 128:(jj + 1) * 128], Wuv2[:, h, :], ON[:, h, :], start=(u == 0), stop=(u == 1)),
                             reads=[r_w, r_ON], writes=[bank.res])
                P.op("act", lambda e, bank=bank, hf=hf: e.copy(OV[:, hf * 4:(hf + 1) * 4, :], bank.ap[:, :].rearrange("p (j t) -> p j t", j=4)),
                     reads=[bank.res], writes=[r_OV])
            for half in range(2):
                bank = C.bank(6 + half)
                for j in range(8):
                    P.op("pe", lambda e, bank=bank, j=j, half=half: e.matmul(bank.ap[:, :], OV[:, j, :], Wo[:, j, half * 512:(half + 1) * 512], start=(j == 0), stop=(j == 7)),
                         reads=[r_OV, r_w], writes=[bank.res])
                hs = slice(half * 512, (half + 1) * 512)
                P.op("dve", lambda e, bank=bank, hs=hs: e.tensor_tensor(ys[:, hs], bank.ap[:, :], C.gate[:, hs], ALU.mult), reads=[bank.res, C.r_gate], writes=[r_ys])
                P.op("pool", lambda e, hs=hs: e.tensor_tensor(ys[:, hs], ys[:, hs], xs[:, hs], ALU.add), reads=[r_ys, r_xs], writes=[r_ys])
            tok = P.dma("sp", xout_d[qs, :], ys[:], reads=[r_ys], writes=[C.r_xout[qt]])
            C.final.append(tok)

        nq = NT
        if DBG["stop"] == "dsa_qt":
            nq = DBG.get("nqt", 3) + 1
        stage_b1(0)
        stage_b2(0)
        stage_b4(0, 1, 17)
        stage_b5(0)
        for qt in range(nq):
            nx = qt + 1
            has = nx < nq
            if has:
                stage_b1(nx)
                stage_b2(nx)
            for cg in range(4):
                stage_b6_cg(qt, cg)
                if has:
                    stage_b4(nx, 1 + 4 * cg, 5 + 4 * cg)
            stage_b7(qt)
            if has:
                stage_b5(nx)
        P.barrier()


def nsa_stage(C, x_d, xout_d):
    P, Dm = C.P, C.D
    with ExitStack() as st:
        KS_T = P.sbuf("n_ksT", [128, 2, T], BF16, st)
        KW_T = P.sbuf("n_kwT", [128, 2, T], BF16, st)
        VS = P.sbuf("n_vs", [128, 32, 4, 65], BF16, st)
        VW = P.sbuf("n_vw", [128, 32, 4, 65], BF16, st)
        KCM = P.sbuf("n_kcm", [128, 2, 256], BF16, st)
        VCM = P.sbuf("n_vcm", [128, 2, 4, 65], BF16, st)
        GT = P.sbuf("n_gt", [128, 32, 48], F32, st)
        r_w, r_K, r_V, r_G, r_cm = Res(), Res(), Res(), Res(), Res()
        P.op("pool", lambda e: e.memset(VS[:, :, :, 64:65], 1.0), writes=[r_V])
        P.op("pool", lambda e: e.memset(VW[:, :, :, 64:65], 1.0), writes=[r_V])
        P.op("pool", lambda e: e.memset(VCM[:], 0.0), writes=[r_cm])
        P.op("pool", lambda e: e.memset(VCM[:, :, :, 64:65], 1.0), writes=[r_cm])
        P.op("pool", lambda e: e.memset(KCM[:], 0.0), writes=[r_cm])
        hTv = Dm["hT"].rearrange("kc p t -> p kc t")
        qTv = Dm["qT"].rearrange("b p t -> p b t")
        r_qT = [Res() for _ in range(8)]

        with ExitStack() as sa:
            KC_T = P.sbuf("na_kcT", [128, 2, T], BF16, sa)
            VC_T = P.sbuf("na_vcT", [128, 2, T], BF16, sa)
            r_kc = Res()
            cnt = 0
            for ppass in range(2):
              with ExitStack() as sp_:
                if ppass == 0:
                    Wk = P.sbuf("na_wk", [128, 4, 8, 256], BF16, sp_)
                    Wks = P.sbuf("na_wks", [128, 3, 8, 256], BF16, sp_)
                    Wvg = P.sbuf("na_wvg", [128, 8, 560], BF16, sp_)
                else:
                    Wq = P.sbuf("na_wq", [128, 8, 1024], BF16, sp_)
                    Wqs = P.sbuf("na_wqs", [128, 8, 1024], BF16, sp_)
                    qg = P.sbuf("na_qg", [128, 8, 512], BF16, sp_)
                hg = [P.sbuf("na_hg%d_%d" % (i, ppass), [128, 8, 512], BF16, sp_) for i in range(2)]
                Cg = P.sbuf("na_cg%d" % ppass, [128, 512], F32, sp_)
                Sg = P.sbuf("na_sg%d" % ppass, [128, 512], F32, sp_)
                t0 = [P.sbuf("na_t0%d_%d" % (i, ppass), [128, 512], F32, sp_) for i in range(2)]
                t1 = [P.sbuf("na_t1%d_%d" % (i, ppass), [128, 512], F32, sp_) for i in range(2)]
                r_wa = Res()
                r_hg = [Res(), Res()]
                r_cs = Res()
                r_t0 = [Res(), Res()]
                r_t1 = [Res(), Res()]
                r_qg = Res()
                for kc in range(8):
                    if ppass == 0:
                        for t in range(4):
                            C.wload(Wk[:, t, kc, :], Dm["nsa_wk"][t, kc * 128:(kc + 1) * 128, :], r_wa)
                        for t in range(3):
                            C.wload(Wks[:, t, kc, :], Dm["nsa_wk_sw"][t, kc * 128:(kc + 1) * 128, :], r_wa)
                        C.wload(Wvg[:, kc, :], Dm["nsa_wvg"][kc * 128:(kc + 1) * 128, :], r_wa)
                    else:
                        C.wload(Wq[:, kc, :], Dm["nsa_wq"][kc * 128:(kc + 1) * 128, :], r_wa)
                        C.wload(Wqs[:, kc, :], Dm["nsa_wq_sw"][kc * 128:(kc + 1) * 128, :], r_wa)
                for tg in range(8):
                    g2 = tg % 2
                    ts = slice(tg * 512, (tg + 1) * 512)
                    P.dma("sp", hg[g2][:], hTv[:, :, ts], reads=[C.r_hT[tg]], writes=[r_hg[g2]])
                    P.dma("sp", Cg[:], Dm["rope_cfm"][:, ts], writes=[r_cs])
                    P.dma("sp", Sg[:], Dm["rope_sfm"][:, ts], writes=[r_cs])

                    hgt, r_hgt = hg[g2], r_hg[g2]

                    def proj_pair(wp, ws, dst, dres, rope, hgt=hgt, r_hgt=r_hgt, Cg=Cg, Sg=Sg, t0=t0, t1=t1, r_t0=r_t0, r_t1=r_t1, r_cs=r_cs, r_wa=r_wa):
                        nonlocal cnt
                        k = cnt % 2
                        cnt += 1
                        bp, bsw = C.bank(k), C.bank(2 + k)
                        for kc in range(8):
                            P.op("pe", lambda e, hgt=hgt, bp=bp, wp=wp, kc=kc: e.matmul(bp.ap[:, :], wp(kc), hgt[:, kc, :], start=(kc == 0), stop=(kc == 7)),
                                 reads=[r_wa, r_hgt], writes=[bp.res])
                        if not rope:
                            P.op("act", lambda e, bp=bp, dst=dst: e.copy(dst, bp.ap[:, :]), reads=[bp.res], writes=[dres])
                            return
                        for kc in range(8):
                            P.op("pe", lambda e, hgt=hgt, bsw=bsw, ws=ws, kc=kc: e.matmul(bsw.ap[:, :], ws(kc), hgt[:, kc, :], start=(kc == 0), stop=(kc == 7)),
                                 reads=[r_wa, r_hgt], writes=[bsw.res])
                        P.op("dve", lambda e, bp=bp, k=k: e.tensor_tensor(t0[k][:], bp.ap[:, :], Cg[:], ALU.mult), reads=[bp.res, r_cs], writes=[r_t0[k]])
                        P.op("dve", lambda e, bsw=bsw, k=k: e.tensor_tensor(t1[k][:], bsw.ap[:, :], Sg[:], ALU.mult), reads=[bsw.res, r_cs], writes=[r_t1[k]])
                        P.op("pool", lambda e, k=k, dst=dst: e.tensor_tensor(dst, t0[k][:], t1[k][:], ALU.add), reads=[r_t0[k], r_t1[k]], writes=[dres])

                    for a in (range(2) if ppass == 0 else []):
                        acs = slice(a * 128, (a + 1) * 128)
                        for t, dstT, dres in [(0, KC_T, r_kc), (1, KS_T, r_K), (2, KW_T, r_K)]:
                            proj_pair(lambda kc, t=t, acs=acs: Wk[:, t, kc, acs], lambda kc, t=t, acs=acs: Wks[:, t, kc, acs], dstT[:, a, ts], dres, True)
                        proj_pair(lambda kc, acs=acs: Wk[:, 3, kc, acs], None, VC_T[:, a, ts], r_kc, False)
                    for blk in (range(8) if ppass == 1 else []):
                        bcs = slice(blk * 128, (blk + 1) * 128)
                        proj_pair(lambda kc, bcs=bcs: Wq[:, kc, bcs], lambda kc, bcs=bcs: Wqs[:, kc, bcs], qg[:, blk, :], r_qg, True)
                    if ppass == 1:
                        P.dma("sp", qTv[:, :, ts], qg[:], reads=[r_qg], writes=[r_qT[tg]])
                    for j in (range(4) if ppass == 0 else []):
                        i = tg * 4 + j
                        b1, b2 = C.bank(4 + (j % 2)), C.bank(6 + (j % 2))
                        for kc in range(8):
                            P.op("pe", lambda e, hgt=hgt, b1=b1, kc=kc, j=j: e.matmul(b1.ap[:, :], hgt[:, kc, j * 128:(j + 1) * 128], Wvg[:, kc, 0:512], start=(kc == 0), stop=(kc == 7)),
                                 reads=[r_wa, r_hgt], writes=[b1.res])
                        for kc in range(8):
                            P.op("pe", lambda e, hgt=hgt, b2=b2, kc=kc, j=j: e.matmul(b2.ap[:, 0:48], hgt[:, kc, j * 128:(j + 1) * 128], Wvg[:, kc, 512:560], start=(kc == 0), stop=(kc == 7)),
                                 reads=[r_wa, r_hgt], writes=[b2.res])
                        P.op("act", lambda e, b1=b1, i=i: e.copy(VS[:, i, :, 0:64], b1.ap[:, 0:256].rearrange("p (g d) -> p g d", g=4)), reads=[b1.res], writes=[r_V])
                        P.op("dve", lambda e, b1=b1, i=i: e.tensor_copy(VW[:, i, :, 0:64], b1.ap[:, 256:512].rearrange("p (g d) -> p g d", g=4)), reads=[b1.res], writes=[r_V])
                        P.op("act", lambda e, b2=b2, i=i: e.activation(GT[:, i, :], b2.ap[:, 0:48], AF.Sigmoid), reads=[b2.res], writes=[r_G])
                P.barrier()
            if DBG["stop"] == "nsa_A":
                C.final.append(P.dma("sp", xout_d[0:128, :], x_d[0:128, :], writes=[Res()]))
                P.barrier()
                return
            W1s = [P.sbuf("na_w1s%d" % i, [128, 32, 256], BF16, sa) for i in range(2)]
            peT = P.sbuf("na_peT", [128, 32], BF16, sa)
            W2k2 = P.sbuf("na_w2k2", [128, 2, 2, 128], BF16, sa)
            W2v = P.sbuf("na_w2v", [128, 2, 64], BF16, sa)
            cb = P.sbuf("na_cb", [128, 4], F32, sa)
            hidT = [P.sbuf("na_hid%d" % i, [128, 2, 256], BF16, sa) for i in range(2)]
            r_cw, r_cb = Res(), Res()
            r_hid = [Res(), Res()]
            P.dma("pool", W1s[0][:], Dm["nsa_w1k"], writes=[r_cw])
            P.dma("pool", W1s[1][:], Dm["nsa_w1v"], writes=[r_cw])
            P.dma("pool", peT[:], Dm["nsa_peT"], writes=[r_cw])
            P.dma("pool", W2k2[:], Dm["nsa_w2k2"].rearrange("(c p) s m -> p c s m", p=128), writes=[r_cw])
            P.dma("pool", W2v[:], Dm["nsa_w2v"].rearrange("(c p) m -> p c m", p=128), writes=[r_cw])
            for kv in range(2):
                for ch in range(2):
                    bank = C.bank(6 + ch)
                    for l in range(32):
                        P.op("pe", lambda e, bank=bank, kv=kv, ch=ch, l=l: e.matmul(bank.ap[:, 0:1], W1s[kv][0:64, l, ch * 128:(ch + 1) * 128], peT[0:64, l:l + 1],
                                                                                    start=(l == 0), stop=(l == 31)),
                             reads=[r_cw], writes=[bank.res])
                    P.op("act", lambda e, bank=bank, kv=kv, ch=ch: e.copy(cb[:, kv * 2 + ch:kv * 2 + ch + 1], bank.ap[:, 0:1]), reads=[bank.res], writes=[r_cb])
            hc = 0
            for kv in range(2):
                srcT = KC_T if kv == 0 else VC_T
                for g in range(4):
                    a, s = g // 2, g % 2
                    pb = s * 64
                    hb = hc % 2
                    hc += 1
                    for ch in range(2):
                        bank = C.bank(4 + ch)
                        for l in range(32):
                            base = srcT[pb:pb + 64, a, 0:16]
                            rhs = bass.AP(base.tensor, base.offset + l, [list(base.ap[0]), [16, 255]])
                            P.op("pe", lambda e, bank=bank, kv=kv, ch=ch, l=l, pb=pb, rhs=rhs: e.matmul(
                                bank.ap[:, 0:255], W1s[kv][pb:pb + 64, l, ch * 128:(ch + 1) * 128], rhs, start=(l == 0), stop=(l == 31)),
                                reads=[r_cw, r_kc], writes=[bank.res])
                        P.op("act", lambda e, bank=bank, kv=kv, ch=ch, hb=hb: e.activation(hidT[hb][:, ch, 0:255], bank.ap[:, 0:255], AF.Silu,
                                                                                            bias=cb[:, kv * 2 + ch:kv * 2 + ch + 1]),
                             reads=[bank.res, r_cb], writes=[r_hid[hb]])
                    if kv == 0:
                        bank = C.bank(6)
                        for ch in range(2):
                            P.op("pe", lambda e, bank=bank, ch=ch, s=s, hb=hb: e.matmul(bank.ap[:, 0:255], W2k2[:, ch, s, :], hidT[hb][:, ch, 0:255], start=(ch == 0), stop=(ch == 1)),
                                 reads=[r_cw, r_hid[hb]], writes=[bank.res])
                        P.op("dve", lambda e, bank=bank, pb=pb, a=a: e.tensor_copy(KCM[pb:pb + 64, a, 0:255], bank.ap[pb:pb + 64, 0:255]), reads=[bank.res], writes=[r_cm])
                    else:
                        for nt in range(2):
                            nn = 128 if nt == 0 else 127
                            bank = C.bank(6 + nt)
                            for ch in range(2):
                                P.op("pe", lambda e, bank=bank, ch=ch, nt=nt, nn=nn, hb=hb: e.matmul(bank.ap[0:nn, 0:64], hidT[hb][:, ch, nt * 128:nt * 128 + nn], W2v[:, ch, :],
                                                                                                   start=(ch == 0), stop=(ch == 1)),
                                     reads=[r_cw, r_hid[hb]], writes=[bank.res])
                            P.op("dve", lambda e, bank=bank, nt=nt, nn=nn, g=g: e.tensor_copy(VCM[0:nn, nt, g, 0:64], bank.ap[0:nn, 0:64]), reads=[bank.res], writes=[r_cm])
            P.barrier()

        if DBG["stop"] == "nsa_C":
            C.final.append(P.dma("sp", xout_d[0:128, :], x_d[0:128, :], writes=[Res()]))
            P.barrier()
            return
        Wo = P.sbuf("n_wo", [128, 8, 1024], BF16, st)
        esel = P.sbuf("n_esel", [128, 32, 128], BF16, st)
        ntri = P.sbuf("n_ntri", [128, 2, 128], BF16, st)
        ncmp = P.sbuf("n_ncmp", [128, NCMP_N, 128], BF16, st)
        fa = P.sbuf("n_fa", [128, 32, 64], F32, st)
        cmast = P.sbuf("n_cmast", [128, 512], F32, st)
        for kc in range(8):
            C.wload(Wo[:, kc, :], Dm["nsa_w_o"][kc * 128:(kc + 1) * 128, :], r_w)
        P.dma("sp", esel[:], Dm["nsa_esel"], writes=[r_w])
        P.dma("sp", ntri[:], Dm["nsa_ntri"], writes=[r_w])
        P.dma("sp", ncmp[:], Dm["nsa_ncmp"], writes=[r_w])
        P.dma("sp", fa[:], Dm["nsa_fa"], writes=[r_w])
        P.dma("sp", cmast[:], Dm["nsa_cmast"], writes=[r_w])
        qgB = [P.sbuf("n_qg%d" % i, [128, 8, 512], BF16, st) for i in range(2)]
        NPM = 8
        PM = [P.sbuf("n_pm%d" % i, [128, 4, 128], BF16, st) for i in range(NPM)]
        ee = [P.sbuf("n_ee%d" % i, [128, 2, 256], F32, st) for i in range(2)]
        em = [P.sbuf("n_em%d" % i, [128, 256], F32, st) for i in range(2)]
        pcs = P.sbuf("n_pcs", [128, 4, 256], F32, st)
        imp = P.sbuf("n_imp", [128, 4, 64], F32, st)
        imp3 = P.sbuf("n_imp3", [128, 64], F32, st)
        m8 = P.sbuf("n_m8", [128, 4, 16], F32, st)
        sm = P.sbuf("n_sm", [128, 64], F32, st)
        nsel = P.sbuf("n_nsel", [128, 2, 128], BF16, st)
        nselT = P.sbuf("n_nselT", [128, 2, 128], BF16, st)
        oacc = P.sbuf("n_oacc", [128, 16, 64], F32, st)
        otmp = P.sbuf("n_otmp", [128, 4, 64], F32, st)
        fac = P.sbuf("n_fac", [128, 16], F32, st)
        otmp2 = [P.sbuf("n_otmp2_%d" % i, [128, 4, 64], F32, st) for i in range(2)]
        obf = P.sbuf("n_obf", [128, 1024], BF16, st)
        OT = P.sbuf("n_oT", [128, 8, 128], BF16, st)
        xs = P.sbuf("n_xs", [128, 1024], F32, st)
        ys = P.sbuf("n_ys", [128, 1024], F32, st)
        r_qg = [Res(), Res()]
        r_PM = [Res() for _ in range(8)]
        r_ee = [Res(), Res()]
        r_em = [Res(), Res()]
        r_pcs, r_imp, r_imp3, r_m8, r_sm, r_nsel, r_nselT, r_oacc, r_otmp, r_fac, r_obf, r_OT, r_xs, r_ys = [Res() for _ in range(14)]
        P.op("pool", lambda e: e.memset(pcs[:], 0.0), writes=[r_pcs])
        SB = [C.bank(0), C.bank(1), C.bank(4), C.bank(5)]
        cst = {"pm": 0, "sb": 0, "ob": 0}

        def qbuf(qt):
            return (qt // 4) % 2

        def load_q(qt):
            tg = qt // 4
            qb = qbuf(qt)
            P.dma("sp", qgB[qb][:], qTv[:, :, tg * 512:(tg + 1) * 512], reads=[r_qT[tg]], writes=[r_qg[qb]])

        def sel_scores(qt, hps):
            qb = qbuf(qt)
            ql = slice((qt % 4) * 128, (qt % 4 + 1) * 128)
            u0 = 248 - 8 * qt
            for hp in hps:
                a, u = hp // 4, hp % 4
                eb = hp % 2
                for s in range(2):
                    bank = C.bank(4 + s)
                    qap = qgB[qb][s * 64:(s + 1) * 64, a * 4 + u, ql]
                    P.op("pe", lambda e, bank=bank, s=s, a=a, qap=qap: e.matmul(bank.ap[:, 0:255], qap,
                                                                                KCM[s * 64:(s + 1) * 64, a, 0:255], start=True, stop=True),
                         reads=[r_qg[qb], r_cm], writes=[bank.res])
                    P.op("act", lambda e, bank=bank, eb=eb, s=s: e.activation(ee[eb][:, s, 0:255], bank.ap[:, 0:255], AF.Exp, scale=0.125),
                         reads=[bank.res], writes=[r_ee[eb]])
                for s in range(2):
                    g = 2 * a + s
                    k = s
                    c0 = 2 * (hp * 2 + s)
                    P.op("dve", lambda e, eb=eb, s=s, k=k, u0=u0, c0=c0: e.scalar_tensor_tensor(em[k][:, 0:255], ee[eb][:, s, 0:255], 1.0, cmast[:, u0:u0 + 255], ALU.mult, ALU.mult,
                                                                                                accum_out=sm[:, c0:c0 + 1]),
                         reads=[r_ee[eb], r_w], writes=[r_em[k], r_sm])
                    P.op("dve", lambda e, c0=c0: e.tensor_scalar(sm[:, c0 + 1:c0 + 2], sm[:, c0:c0 + 1], 1e-30, None, ALU.max), reads=[r_sm], writes=[r_sm])
                    P.op("dve", lambda e, c0=c0: e.reciprocal(sm[:, c0:c0 + 1], sm[:, c0 + 1:c0 + 2]), reads=[r_sm], writes=[r_sm])
                    if u == 0:
                        P.op("dve", lambda e, k=k, g=g, c0=c0: e.tensor_scalar(pcs[:, g, 0:255], em[k][:, 0:255], sm[:, c0:c0 + 1], None, ALU.mult),
                             reads=[r_em[k], r_sm], writes=[r_pcs])
                    else:
                        P.op("dve", lambda e, k=k, g=g, c0=c0: e.scalar_tensor_tensor(pcs[:, g, 0:255], em[k][:, 0:255], sm[:, c0:c0 + 1], pcs[:, g, 0:255], ALU.mult, ALU.add),
                             reads=[r_em[k], r_sm, r_pcs], writes=[r_pcs])

        def sel_top(qt):
            P.op("dve", lambda e: e.tensor_reduce(imp[:], pcs[:].rearrange("p g (j r) -> p g j r", r=4), AX.X, ALU.add), reads=[r_pcs], writes=[r_imp])
            sh = pcs[:, :, 0:252]
            shv = bass.AP(sh.tensor, sh.offset + 3, [list(sh.ap[0]), list(sh.ap[1]), [4, 63]])
            P.op("dve", lambda e, shv=shv: e.tensor_tensor(imp[:, :, 1:64], imp[:, :, 1:64], shv, ALU.add), reads=[r_pcs, r_imp], writes=[r_imp])
            P.op("dve", lambda e, qt=qt: e.tensor_tensor(imp[:], imp[:], fbc(fa[:, qt, :], 0, 4), ALU.add), reads=[r_imp, r_w], writes=[r_imp])
            for g in range(4):
                P.op("dve", lambda e, g=g: e.max(m8[:, g, 0:8], imp[:, g, :]), reads=[r_imp], writes=[r_m8])
                P.op("dve", lambda e, g=g: e.match_replace(imp3[:], m8[:, g, 0:8], imp[:, g, :], -1e30), reads=[r_imp, r_m8], writes=[r_imp3])
                P.op("dve", lambda e, g=g: e.max(m8[:, g, 8:16], imp3[:]), reads=[r_imp3], writes=[r_m8])
                P.op("dve", lambda e, g=g: e.tensor_scalar(m8[:, g, 0:1], m8[:, g, 15:16], -1.0, None, ALU.max), reads=[r_m8], writes=[r_m8])
                P.op("dve", lambda e, g=g: e.tensor_scalar(nsel[:, g // 2, (g % 2) * 64:(g % 2) * 64 + 64], imp[:, g, :], m8[:, g, 0:1], 1.0, ALU.is_ge, ALU.subtract),
                     reads=[r_imp, r_m8], writes=[r_nsel])
            bank = C.bank(4)
            pv = bank.ap.bitcast(BF16)
            for a2 in range(2):
                P.op("pe", lambda e, pv=pv, a2=a2: e.transpose(pv[:, a2 * 128:(a2 + 1) * 128], nsel[:, a2, :], C.identb[:]), reads=[r_nsel, C.r_ident], writes=[bank.res])
            P.op("act", lambda e, pv=pv: e.copy(nselT[:], pv[:, 0:256].rearrange("p (g t) -> p g t", g=2)), reads=[bank.res], writes=[r_nselT])

        OB = [C.bank(2), C.bank(3), C.bank(6), C.bank(7)]

        def branches_pair(qt, a):
            qb = qbuf(qt)
            ql = slice((qt % 4) * 128, (qt % 4 + 1) * 128)
            nvalid = min(255, 8 * qt + 7)
            work = []
            for br in range(3):
                if br == 0:
                    items = [("c", nt, 128 if nt == 0 else 127) for nt in range(2) if nt * 128 < nvalid]
                elif br == 1:
                    items = [("s", kt, 128) for kt in range(qt + 1)]
                else:
                    items = [("w", kt, 128) for kt in range(max(0, qt - 4), qt + 1)]
                for ii, it in enumerate(items):
                    work.append((br, ii, len(items)) + it)
            obanks = {}
            for br in range(3):
                k = cst["ob"] % 2
                cst["ob"] += 1
                obanks[br] = (OB[2 * k], OB[2 * k + 1])
            slots = {}

            def stage1(w):
                br, ii, nitems, kind, kt, nn = work[w]
                sbs, pis, mms = [], [], []
                for s in range(2):
                    pb = s * 64
                    g = 2 * a + s
                    qrh = qgB[qb][pb:pb + 64, a * 4:(a + 1) * 4, ql]
                    sb = SB[cst["sb"] % 4]
                    cst["sb"] += 1
                    pi = cst["pm"] % NPM
                    cst["pm"] += 1
                    sbs.append(sb)
                    pis.append(pi)
                    mm = []
                    if kind == "c":
                        mm.append((KCM[pb:pb + 64, a, kt * 128:kt * 128 + nn], qrh, [r_cm, r_qg[qb]]))
                        ci = NCMP_IDX.get((qt, kt))
                        if ci is not None:
                            mm.append((C.identb[0:nn, 0:nn], fbc(ncmp[0:nn, ci, :], 0, 4), [C.r_ident, r_w]))
                    elif kind == "s":
                        mm.append((KS_T[pb:pb + 64, a, kt * 128:(kt + 1) * 128], qrh, [r_K, r_qg[qb]]))
                        mm.append((esel[pb:pb + 64, kt, :], fbc(nselT[pb:pb + 64, a, :], 0, 4), [r_w, r_nselT]))
                        if kt == qt:
                            mm.append((C.identb[:], fbc(ntri[:, 0, :], 0, 4), [C.r_ident, r_w]))
                    else:
                        mm.append((KW_T[pb:pb + 64, a, kt * 128:(kt + 1) * 128], qrh, [r_K, r_qg[qb]]))
                        if kt == qt:
                            mm.append((C.identb[:], fbc(ntri[:, 0, :], 0, 4), [C.r_ident, r_w]))
                        if kt == qt - 4:
                            mm.append((C.identb[:], fbc(ntri[:, 1, :], 0, 4), [C.r_ident, r_w]))
                    mms.append(mm)
                slots[w] = pis
                nm = len(mms[0])
                for mi in range(nm):
                    for s in range(2):
                        l_, r_, rs_ = mms[s][mi]
                        sb = sbs[s]
                        P.op("pe", lambda e, sb=sb, nn=nn, l_=l_, r_=r_, mi=mi, last=(mi == nm - 1): e.matmul(
                            sb.ap[0:nn, :].rearrange("p (h t) -> p h t", h=4), l_, r_, start=(mi == 0), stop=last),
                            reads=rs_, writes=[sb.res])
                for s in range(2):
                    sb, pi = sbs[s], pis[s]
                    P.op("act", lambda e, sb=sb, nn=nn, pi=pi: e.activation(PM[pi][0:nn, :, :], sb.ap[0:nn, :].rearrange("p (h t) -> p h t", h=4), AF.Exp, scale=0.125),
                         reads=[sb.res], writes=[r_PM[pi]])

            def stage2(w):
                br, ii, nitems, kind, kt, nn = work[w]
                for s in range(2):
                    g = 2 * a + s
                    pi = slots[w][s]
                    ob = obanks[br][s]
                    oview = ob.ap[:, 0:260].rearrange("p (h c) -> p h c", h=4)
                    if kind == "c":
                        vsrc, vres = VCM[0:nn, kt, g, :], r_cm
                    elif kind == "s":
                        vsrc, vres = VS[:, kt, g, :], r_V
                    else:
                        vsrc, vres = VW[:, kt, g, :], r_V
                    for hh in range(4):
                        P.op("pe", lambda e, oview=oview, hh=hh, pi=pi, nn=nn, vsrc=vsrc, first=(ii == 0 and hh == 0), last=(ii == nitems - 1 and hh == 3): e.matmul(
                            oview[:, hh, :], PM[pi][0:nn, hh, :], vsrc, start=first, stop=last),
                            reads=[r_PM[pi], vres], writes=[ob.res])
                    if ii == nitems - 1:
                        fs = fac[:, s * 8:(s + 1) * 8]
                        rsum = oview[:, :, 64]
                        P.op("dve", lambda e, rsum=rsum, fs=fs: e.tensor_scalar(fs[:, 0:4], rsum, 1e-30, None, ALU.max), reads=[ob.res], writes=[r_fac])
                        P.op("dve", lambda e, fs=fs: e.reciprocal(fs[:, 4:8], fs[:, 0:4]), reads=[r_fac], writes=[r_fac])
                        gsl = GT[:, qt, g * 12:(g + 1) * 12]
                        gv = bass.AP(gsl.tensor, gsl.offset + br, [list(gsl.ap[0]), [3, 4]])
                        P.op("dve", lambda e, gv=gv, fs=fs: e.tensor_tensor(fs[:, 0:4], fs[:, 4:8], gv, ALU.mult), reads=[r_fac, r_G], writes=[r_fac])
                        if br == 0:
                            P.op("dve", lambda e, oview=oview, g=g, fs=fs: e.tensor_tensor(oacc[:, g * 4:(g + 1) * 4, :], oview[:, :, 0:64], fbc(fs[:, 0:4], 1, 64), ALU.mult),
                                 reads=[ob.res, r_fac], writes=[r_oacc])
                        else:
                            ot = otmp2[s]
                            P.op("dve", lambda e, oview=oview, fs=fs, ot=ot: e.tensor_tensor(ot[:], oview[:, :, 0:64], fbc(fs[:, 0:4], 1, 64), ALU.mult),
                                 reads=[ob.res, r_fac], writes=[r_otmp])
                            P.op("pool", lambda e, g=g, ot=ot: e.tensor_tensor(oacc[:, g * 4:(g + 1) * 4, :], oacc[:, g * 4:(g + 1) * 4, :], ot[:], ALU.add),
                                 reads=[r_otmp, r_oacc], writes=[r_oacc])

            LA = 2
            nw = len(work)
            for w in range(min(LA, nw)):
                stage1(w)
            for w in range(nw):
                if w + LA < nw:
                    stage1(w + LA)
                stage2(w)

        def outproj(qt):
            qs = slice(qt * 128, (qt + 1) * 128)
            P.dma("sp", xs[:], x_d[qs, :], writes=[r_xs])
            P.op("act", lambda e: e.copy(obf[:], oacc[:].rearrange("p h d -> p (h d)")), reads=[r_oacc], writes=[r_obf])
            bank = C.bank(4)
            pv = bank.ap.bitcast(BF16)
            for j in range(8):
                P.op("pe", lambda e, pv=pv, j=j: e.transpose(pv[:, j * 128:(j + 1) * 128], obf[:, j * 128:(j + 1) * 128], C.identb[:]), reads=[r_obf, C.r_ident], writes=[bank.res])
            P.op("act", lambda e, pv=pv: e.copy(OT[:], pv.rearrange("p (j t) -> p j t", j=8)), reads=[bank.res], writes=[r_OT])
            for half in range(2):
                bank = C.bank(4 + half)
                for j in range(8):
                    P.op("pe", lambda e, bank=bank, j=j, half=half: e.matmul(bank.ap[:, :], OT[:, j, :], Wo[:, j, half * 512:(half + 1) * 512], start=(j == 0), stop=(j == 7)),
                         reads=[r_OT, r_w], writes=[bank.res])
                hs = slice(half * 512, (half + 1) * 512)
                P.op("dve", lambda e, bank=bank, hs=hs: e.tensor_tensor(ys[:, hs], bank.ap[:, :], C.gate[:, hs], ALU.mult), reads=[bank.res, C.r_gate], writes=[r_ys])
                P.op("pool", lambda e, hs=hs: e.tensor_tensor(ys[:, hs], ys[:, hs], xs[:, hs], ALU.add), reads=[r_ys, r_xs], writes=[r_ys])
            tok = P.dma("sp", xout_d[qs, :], ys[:], reads=[r_ys], writes=[C.r_xout[qt]])
            C.final.append(tok)

        nq = NT
        if DBG["stop"] == "nsa_qt":
            nq = DBG.get("nqt", 3) + 1
        load_q(0)
        sel_scores(0, range(8))
        sel_top(0)
        for qt in range(nq):
            nx = qt + 1
            has = nx < nq
            if has and nx % 4 == 0:
                load_q(nx)
            for a in range(2):
                branches_pair(qt, a)
                if has:
                    sel_scores(nx, [4 * a, 4 * a + 1, 4 * a + 2, 4 * a + 3])
            outproj(qt)
            if has:
                sel_top(nx)
        P.barrier()


def build(stages, ncores=8):
    nc = bass.Bass("TRN2", target_bir_lowering=False)
    P = Prog(nc)
    C = Ctx()
    C.P, C.nc = P, nc
    Dm = {}
    C.D = Dm
    C.final = []

    def din(name, shape, dt=F32):
        Dm[name] = nc.dram_tensor(name, list(shape), dt, kind="ExternalInput").ap()
        return Dm[name]

    def dout(name, shape, dt=F32):
        Dm[name] = nc.dram_tensor(name, list(shape), dt, kind="ExternalOutput").ap()
        return Dm[name]

    def dtmp(name, shape, dt=F32):
        Dm[name] = nc.dram_tensor(name, list(shape), dt).ap()
        return Dm[name]

    C.fence_res = Res()
    C.pending_fence = []

    def fence(rl):
        P.barrier()
    C.fence = fence

    banks = [Bank(P.psum("bank%d" % i, [128, 512], F32)) for i in range(8)]
    C.bank = lambda i: banks[i]
    NSTG = 3
    stg = [P.sbuf("stg%d" % i, [128, 1024], F32) for i in range(NSTG)]
    r_stg = [Res() for _ in range(NSTG)]
    stg_i = [0]

    def wload(dst, src, dres):
        p, n = dst.shape[0], dst.shape[-1]
        i = stg_i[0] % NSTG
        stg_i[0] += 1
        P.dma("sp", stg[i][0:p, 0:n], src, writes=[r_stg[i]])
        P.op("pool", lambda e, i=i, p=p, n=n, dst=dst: e.tensor_copy(dst, stg[i][0:p, 0:n]), reads=[r_stg[i]], writes=[dres])
    C.wload = wload

    order = ["dsa", "ffn", "nsa", "moe"]
    xnames = {"dsa": ("x", "x1"), "ffn": ("x1", "x2"), "nsa": ("x2", "x3"), "moe": ("x3", "out")}
    first, last = stages[0], stages[-1]
    for sname in stages:
        a, b = xnames[sname]
        if a not in Dm:
            din(a, [T, D])
        if sname == last:
            dout(b, [T, D])
        else:
            dtmp(b, [T, D])
    din("csil_in", [128, 8])
    din("ada_w", [2, 2, 1024, 3072])
    din("ada_b", [2, 2, 3072])
    din("norm_mix", [2, 1024])
    din("norm_ffn", [2, 1024])
    din("ident_in", [128, 128])
    dtmp("hT", [8, 128, T], BF16)
    dtmp("yacc", [T, D])
    C.r_hT = [Res() for _ in range(8)]
    C.r_yacc = [Res() for _ in range(NT)]
    rx = {n: [Res() for _ in range(NT)] for n in ["x", "x1", "x2", "x3", "out"]}

    C.identb = P.sbuf("identb", [128, 128], BF16)
    C.r_ident = Res()
    P.dma("pool", C.identb[:], Dm["ident_in"], writes=[C.r_ident])
    csl = P.sbuf("csl", [128, 8], F32)
    C.csb = P.sbuf("csb", [128, 8, 128], F32)
    C.r_csb = Res()
    r_csl = Res()
    P.dma("sp", csl[:], Dm["csil_in"], writes=[r_csl])
    P.op("act", lambda e: e.activation(csl[:], csl[:], AF.Silu), reads=[r_csl], writes=[r_csl])
    P.op("dve", lambda e: e.tensor_copy(C.csb[:], fbc(csl[:], 1, 128)), reads=[r_csl], writes=[C.r_csb])
    C.epsc = P.sbuf("epsc", [128, 2], F32)
    C.r_eps = Res()
    P.op("pool", lambda e: e.memset(C.epsc[:], EPS), writes=[C.r_eps])
    C.gs = P.sbuf("gs", [128, 1024], F32)
    C.shift = P.sbuf("shift", [128, 1024], F32)
    C.gate = P.sbuf("gate", [128, 1024], F32)
    C.r_gs, C.r_shift, C.r_gate = Res(), Res(), Res()

    for sname in stages:
        a, b = xnames[sname]
        C.r_xin = rx[a]
        C.r_xout = rx[b]
        C.final = []
        if sname == "dsa":
            for nm, shp in [("dsa_w_in", [1024, 472]), ("dsa_w_o", [1024, 1024]), ("dsa_wuq_nope", [256, 768]), ("dsa_wuq_rope", [256, 256]),
                            ("dsa_wuq_rsw", [256, 256]), ("dsa_wiq", [256, 512]), ("dsa_wiq_sw", [256, 512]), ("dsa_wukT", [48, 16, 128]),
                            ("dsa_wuv2", [128, 16, 128]), ("dsa_g_q", [256]), ("dsa_g_kv", [128]), ("rope_ctm", [T, 16]), ("rope_stm", [T, 16]),
                            ("rope_cfm", [128, T]), ("rope_sfm", [128, T])]:
                if nm not in Dm:
                    din(nm, shp)
            mod_stage(C, 0, 0, Dm["norm_mix"][0])
            dsa_stage(C, Dm[a], Dm[b])
        elif sname == "nsa":
            for ent in [("nsa_w_o", [1024, 1024]), ("nsa_wk", [4, 1024, 256]), ("nsa_wk_sw", [3, 1024, 256]), ("nsa_wq", [1024, 1024]),
                            ("nsa_wq_sw", [1024, 1024]), ("nsa_wvg", [1024, 560]), ("nsa_w1k", [128, 32, 256]), ("nsa_w1v", [128, 32, 256]),
                            ("nsa_peT", [128, 32]), ("nsa_w2k2", [256, 2, 128]), ("nsa_w2v", [256, 64]), ("nsa_esel", [128, 32, 128], BF16),
                            ("nsa_ntri", [128, 2, 128], BF16), ("nsa_ncmp", [128, NCMP_N, 128], BF16), ("nsa_fa", [128, 32, 64]), ("nsa_cmast", [128, 512]),
                            ("rope_cfm", [128, T]), ("rope_sfm", [128, T])]:
                nm, shp = ent[0], ent[1]
                if nm not in Dm:
                    din(nm, shp, ent[2] if len(ent) > 2 else F32)
            dtmp("qT", [8, 128, T], BF16)
            mod_stage(C, 1, 0, Dm["norm_mix"][1])
            prenorm_stage(C, Dm[a], Dm["hT"])
            nsa_stage(C, Dm[a], Dm[b])
        elif sname == "ffn":
            din("ffn_w1", [1024, 2816])
            din("ffn_w3", [1024, 2816])
            din("ffn_w2", [2816, 1024])
            mod_stage(C, 0, 1, Dm["norm_ffn"][0])
            if DBG["stop"] == "mod":
                dout("dbg_mod", [3, 128, 1024])
                for i, t in enumerate([C.gs, C.shift, C.gate]):
                    C.final.append(P.dma("sp", Dm["dbg_mod"][i], t[:], reads=[C.r_gs, C.r_shift, C.r_gate], writes=[Res()]))
                break
            if DBG["stop"] == "prenorm":
                prenorm_stage(C, Dm[a], Dm["hT"])
                dout("dbg_hT", [8, 128, T], BF16)
                hsb = P.sbuf("dbg_hsb", [128, 8, 512], BF16)
                rr = Res()
                for tg in range(8):
                    P.dma("sp", hsb[:], Dm["hT"].rearrange("kc p t -> p kc t")[:, :, tg * 512:(tg + 1) * 512], reads=[C.r_hT[tg]], writes=[rr])
                    C.final.append(P.dma("sp", Dm["dbg_hT"].rearrange("kc p t -> p kc t")[:, :, tg * 512:(tg + 1) * 512], hsb[:], reads=[rr], writes=[Res()]))
                break
            passes = []
            for (f0, F) in [(0, 1024), (1024, 896), (1920, 896)]:
                passes.append((Dm["ffn_w1"][:, f0:f0 + F], Dm["ffn_w3"][:, f0:f0 + F], Dm["ffn_w2"][f0:f0 + F, :], None))
            ffn_passes(C, Dm["hT"], passes, Dm["yacc"], pn_args=(Dm[a], None))
            combine_stage(C, Dm[a], Dm["yacc"], Dm[b])
        elif sname == "moe":
            din("moe_router", [1024, 8])
            din("moe_w1", [8, 1024, 3584])
            din("moe_w3", [8, 1024, 3584])
            din("moe_w2", [8, 3584, 1024])
            din("final_norm", [1024])
            mod_stage(C, 1, 1, Dm["norm_ffn"][1])
            wrT = P.sbuf("moe_wrT", [128, 8, 8], F32)
            tokgate = P.sbuf("moe_tokgate", [128, 32, 8], F32)
            r_wr = Res()
            r_tgl = [Res() for _ in range(8)]
            P.dma("sp", wrT[:], Dm["moe_router"].rearrange("(kc p) e -> p kc e", p=128), writes=[r_wr])
            passes = []
            for ex in range(8):
                for qf in range(4):
                    f0 = qf * 896
                    passes.append((Dm["moe_w1"][ex, :, f0:f0 + 896], Dm["moe_w3"][ex, :, f0:f0 + 896],
                                   Dm["moe_w2"][ex, f0:f0 + 896, :], ex))
            ffn_passes(C, Dm["hT"], passes, Dm["yacc"], tokgate=(tokgate, r_tgl),
                       pn_args=(Dm[a], dict(wrT=(wrT, r_wr), tokgate=(tokgate, r_tgl))), FM=896)
            combine_stage(C, Dm[a], Dm["yacc"], Dm[b], final_g=Dm["final_norm"])
    P.finish(C.final)
    return nc


def host_prep(inputs, b, stages):
    m = {}
    m["csil_in"] = np.ascontiguousarray(inputs["c"][b].reshape(8, 128).T)
    m["ada_w"] = inputs["ada_w"]
    m["ada_b"] = inputs["ada_b"]
    m["norm_mix"] = inputs["norm_mix"]
    m["norm_ffn"] = inputs["norm_ffn"]
    m["ident_in"] = np.eye(128, dtype=np.float32)
    if "dsa" in stages or "nsa" in stages:
        inv = (500000.0 ** (-np.arange(0, 16, 2, dtype=np.float32) / 16)).astype(np.float32)
        ang = np.arange(T, dtype=np.float32)[:, None] * inv[None, :]
        co, si = np.cos(ang).astype(np.float32), np.sin(ang).astype(np.float32)
        m["rope_ctm"] = np.ascontiguousarray(np.concatenate([co, co], 1))
        m["rope_stm"] = np.ascontiguousarray(np.concatenate([-si, si], 1))
        cf = np.ones((64, T), np.float32)
        sf = np.zeros((64, T), np.float32)
        cf[0:8] = co.T
        cf[8:16] = co.T
        sf[0:8] = -si.T
        sf[8:16] = si.T
        m["rope_cfm"] = np.ascontiguousarray(np.concatenate([cf, cf], 0))
        m["rope_sfm"] = np.ascontiguousarray(np.concatenate([sf, sf], 0))
    if "dsa" in stages:
        m["dsa_w_in"] = inputs["dsa_w_in"][0]
        m["dsa_w_o"] = inputs["dsa_w_o"][0]
        wuq = inputs["dsa_w_uq"][0].reshape(256, 16, 64)
        m["dsa_wuq_nope"] = np.ascontiguousarray(wuq[:, :, 16:64].reshape(256, 768))
        m["dsa_wuq_rope"] = np.ascontiguousarray(wuq[:, :, 0:16].reshape(256, 256))
        m["dsa_wuq_rsw"] = np.ascontiguousarray(np.concatenate([wuq[:, :, 8:16], wuq[:, :, 0:8]], 2).reshape(256, 256))
        wiq = inputs["dsa_w_iq"][0].reshape(256, 8, 64)
        m["dsa_wiq"] = np.ascontiguousarray(wiq.reshape(256, 512))
        m["dsa_wiq_sw"] = np.ascontiguousarray(np.concatenate([wiq[:, :, 8:16], wiq[:, :, 0:8], wiq[:, :, 16:64]], 2).reshape(256, 512))
        m["dsa_wukT"] = np.ascontiguousarray(inputs["dsa_w_uk"][0].transpose(2, 0, 1))
        wuv2 = np.zeros((128, 16, 128), np.float32)
        for h in range(16):
            wuv2[:, h, (h % 2) * 64:(h % 2) * 64 + 64] = inputs["dsa_w_uv"][0][h]
        m["dsa_wuv2"] = wuv2
        m["dsa_g_q"] = inputs["dsa_g_q"][0]
        m["dsa_g_kv"] = inputs["dsa_g_kv"][0]
    if "nsa" in stages:
        w = inputs["nsa_w_in"][0]

        def sw(wc, nh):
            x = wc.reshape(1024, nh, 64)
            return np.concatenate([x[:, :, 8:16], x[:, :, 0:8], x[:, :, 16:64]], 2).reshape(1024, nh * 64)

        def blk(wc):
            x = wc.reshape(1024, 16, 64)
            cols = []
            for a_ in range(2):
                for u_ in range(4):
                    cols += [x[:, 8 * a_ + u_], x[:, 8 * a_ + 4 + u_]]
            return np.ascontiguousarray(np.concatenate(cols, 1))
        wq = w[:, 0:1024]
        m["nsa_wq"] = blk(wq)
        m["nsa_wq_sw"] = blk(sw(wq, 16))
        kc, vc, ks, vs, kw, vw = [w[:, 1024 + 256 * i_:1280 + 256 * i_] for i_ in range(6)]
        m["nsa_wk"] = np.ascontiguousarray(np.stack([kc, ks, kw, vc], 0))
        m["nsa_wk_sw"] = np.ascontiguousarray(np.stack([sw(kc, 4), sw(ks, 4), sw(kw, 4)], 0))
        m["nsa_wvg"] = np.ascontiguousarray(np.concatenate([vs, vw, w[:, 2560:2608]], 1))
        m["nsa_w_o"] = inputs["nsa_w_o"][0]
        for nm, src in [("nsa_w1k", "nsa_cmp_k1"), ("nsa_w1v", "nsa_cmp_v1")]:
            x = inputs[src][0].reshape(32, 64, 256).transpose(1, 0, 2)
            m[nm] = np.ascontiguousarray(np.concatenate([x, x], 0))
        pe = inputs["nsa_cmp_pe"][0].T
        m["nsa_peT"] = np.ascontiguousarray(np.concatenate([pe, pe], 0))
        w2k2 = np.zeros((256, 2, 128), np.float32)
        w2k2[:, 0, 0:64] = inputs["nsa_cmp_k2"][0]
        w2k2[:, 1, 64:128] = inputs["nsa_cmp_k2"][0]
        m["nsa_w2k2"] = w2k2
        m["nsa_w2v"] = inputs["nsa_cmp_v2"][0]
        m["nsa_esel"], m["nsa_ntri"], m["nsa_ncmp"] = [np.asarray(a_).astype(ml_dtypes.bfloat16) for a_ in _NC[0:3]]
        m["nsa_fa"], m["nsa_cmast"] = _NC[4], _NC[5]
    if "ffn" in stages:
        m["ffn_w1"] = inputs["ffn_w1"][0]
        m["ffn_w3"] = inputs["ffn_w3"][0]
        m["ffn_w2"] = inputs["ffn_w2"][0]
    if "moe" in stages:
        m["moe_router"] = inputs["moe_router"][0]
        m["moe_w1"] = inputs["moe_w1"][0]
        m["moe_w3"] = inputs["moe_w3"][0]
        m["moe_w2"] = inputs["moe_w2"][0]
        m["final_norm"] = inputs["final_norm"]
    return m


_CACHE = {}


def kernel(**inputs):
    inputs = {k: np.asarray(v) for k, v in inputs.items()}
    stages = ["dsa", "ffn", "nsa", "moe"]
    if "nc" not in _CACHE:
        _CACHE["nc"] = build(stages)
    nc = _CACHE["nc"]
    shared = host_prep(inputs, 0, stages)
    in_maps = []
    for b in range(8):
        m = dict(shared)
        m["csil_in"] = np.ascontiguousarray(inputs["c"][b].reshape(8, 128).T.astype(np.float32))
        m["x"] = np.ascontiguousarray(inputs["x"][b].astype(np.float32))
        in_maps.append(m)
    res = run_bass_kernel_spmd(nc, in_maps, core_ids=list(range(8)))
    out = np.stack([np.asarray(res.results[b]["out"]) for b in range(8)], 0).astype(np.float32)
    return out
```
